# Optimizing a Trainium2 kernel written in Bass

```python
import jax, jax.numpy as jnp
from jax import lax
import numpy as np

D_MODEL = 1024
BATCH = 16
SEQ = 2048
DEPTH = 1

MEM_LEN = 256
HEAD_DIM = 64
BLOCK_Q = 128
ROPE_THETA = 10000.0
EPS = 1e-6
DIL_PAIRS = ((128, 1), (512, 4), (2048, 16))
N_DIL_GROUPS = 3
DIL_HEADS = 8
DIL_WIDTH = DIL_HEADS * HEAD_DIM
SB_HEADS = 8
SB_WIDTH = SB_HEADS * HEAD_DIM
MEM_HEADS = 4
MEM_HEAD_DIM = 128
MEM_WIDTH = MEM_HEADS * MEM_HEAD_DIM
N_BRANCHES = 3
A_QKV_COLS = 3 * N_DIL_GROUPS * DIL_WIDTH
B_QKV_COLS = 3 * SB_WIDTH
M_Q_COLS = MEM_WIDTH
GATE_COLS = N_BRANCHES * D_MODEL
OFF_B = A_QKV_COLS
OFF_M = OFF_B + B_QKV_COLS
OFF_G = OFF_M + M_Q_COLS
IN_COLS = OFF_G + GATE_COLS
N_GROUPS = 4
EXPERTS_PER_GROUP = 4
N_EXPERTS = N_GROUPS * EXPERTS_PER_GROUP
TOP_K_IN_GROUP = 2
D_EXPERT = 512

kernel_name = 'hybrid_dilated_stickbreak_memory_hmoe'


def _rmsnorm(x, gain):
    xf = x.astype(jnp.float32)
    y = xf * lax.rsqrt(jnp.mean(xf * xf, axis=-1, keepdims=True) + EPS)
    return (y * gain.astype(jnp.float32)).astype(x.dtype)


def _split_heads(t, n_heads, head_dim):
    b, s, _ = t.shape
    return t.reshape(b, s, n_heads, head_dim).transpose(0, 2, 1, 3)


def _merge_heads(t):
    b, h, s, d = t.shape
    return t.transpose(0, 2, 1, 3).reshape(b, s, h * d)


def _rope(t, positions):
    dh = t.shape[-1]
    inv_freq = ROPE_THETA ** (-jnp.arange(0, dh, 2, dtype=jnp.float32) / dh)
    ang = positions.astype(jnp.float32)[:, None, :, None] * inv_freq
    cos, sin = jnp.cos(ang), jnp.sin(ang)
    tf = t.astype(jnp.float32)
    t1, t2 = tf[..., : dh // 2], tf[..., dh // 2:]
    return jnp.concatenate([t1 * cos - t2 * sin, t2 * cos + t1 * sin], axis=-1).astype(t.dtype)


def _banded_attention(q, k, v, span):
    b, h, n, dh = q.shape
    nb = -(-n // BLOCK_Q)
    pad = nb * BLOCK_Q - n
    qb = jnp.pad(q, ((0, 0), (0, 0), (0, pad), (0, 0))).reshape(b, h, nb, BLOCK_Q, dh)

    def key_blocks(t):
        tb = jnp.pad(t, ((0, 0), (0, 0), (BLOCK_Q, pad), (0, 0))).reshape(b, h, nb + 1, BLOCK_Q, dh)
        return jnp.concatenate([tb[:, :, :-1], tb[:, :, 1:]], axis=3)

    kb, vb = key_blocks(k), key_blocks(v)
    scores = jnp.einsum('bhnqd,bhnkd->bhnqk', qb, kb, preferred_element_type=jnp.float32) * (dh ** -0.5)
    qi = jnp.arange(BLOCK_Q)[:, None]
    kj = jnp.arange(2 * BLOCK_Q)[None, :]
    dist = qi + BLOCK_Q - kj
    kpos = jnp.arange(nb)[:, None, None] * BLOCK_Q - BLOCK_Q + kj[None]
    mask = (dist >= 0) & (dist <= span) & (kpos >= 0)
    scores = jnp.where(mask, scores, -jnp.inf)
    m = jnp.max(scores, axis=-1, keepdims=True)
    p = jnp.exp(scores - m)
    den = jnp.sum(p, axis=-1, keepdims=True)
    out = jnp.einsum('bhnqk,bhnkd->bhnqd', p, vb.astype(jnp.float32)) / den
    lse = (m + jnp.log(den))[..., 0]
    out = out.reshape(b, h, nb * BLOCK_Q, dh)[:, :, :n]
    lse = lse.reshape(b, h, nb * BLOCK_Q)[:, :, :n]
    return out, lse


def _dilated_group(q, k, v, window, dilation):
    b, h, s, dh = q.shape
    span = window // dilation
    sub = s // dilation

    def to_sub(t):
        return t.reshape(b, h, sub, dilation, dh).transpose(0, 1, 3, 2, 4).reshape(b, h * dilation, sub, dh)

    o, lse = _banded_attention(to_sub(q), to_sub(k), to_sub(v), span)
    o = o.reshape(b, h, dilation, sub, dh).transpose(0, 1, 3, 2, 4).reshape(b, h, s, dh)
    lse = lse.reshape(b, h, dilation, sub).transpose(0, 1, 3, 2).reshape(b, h, s)
    return o, lse


def _dilated_mixer(cols, positions, qn, kn):
    b, s, _ = cols.shape
    qkv = cols.reshape(b, s, 3, N_DIL_GROUPS, DIL_HEADS, HEAD_DIM)
    outs, lses = [], []
    for g, (window, dilation) in enumerate(DIL_PAIRS):
        q = _rope(_rmsnorm(qkv[:, :, 0, g].transpose(0, 2, 1, 3), qn), positions)
        k = _rope(_rmsnorm(qkv[:, :, 1, g].transpose(0, 2, 1, 3), kn), positions)
        v = qkv[:, :, 2, g].transpose(0, 2, 1, 3)
        o, lse = _dilated_group(q, k, v, window, dilation)
        outs.append(o)
        lses.append(lse)
    wts = jax.nn.softmax(jnp.stack(lses, axis=0), axis=0)
    o = wts[0][..., None] * outs[0] + wts[1][..., None] * outs[1] + wts[2][..., None] * outs[2]
    return _merge_heads(o.astype(cols.dtype))


def _stick_breaking_mixer(cols):
    b, s, _ = cols.shape
    qkv = cols.reshape(b, s, 3, SB_HEADS, HEAD_DIM)
    q = qkv[:, :, 0].transpose(0, 2, 1, 3)
    k = qkv[:, :, 1].transpose(0, 2, 1, 3)
    v = qkv[:, :, 2].transpose(0, 2, 1, 3)
    scale = HEAD_DIM ** -0.5
    outs = []
    for n in range(s // BLOCK_Q):
        start, end = n * BLOCK_Q, (n + 1) * BLOCK_Q
        z = jnp.einsum('bhqd,bhkd->bhqk', q[:, :, start:end], k[:, :, :end],
                       preferred_element_type=jnp.float32) * scale
        tpos = start + jnp.arange(BLOCK_Q)[:, None]
        spos = jnp.arange(end)[None, :]
        causal = spos < tpos
        log_beta = jax.nn.log_sigmoid(z)
        log_1m = jnp.where(causal, jax.nn.log_sigmoid(-z), 0.0)
        suffix = lax.cumsum(log_1m, axis=3, reverse=True) - log_1m
        a = jnp.where(causal, jnp.exp(log_beta + suffix), 0.0)
        outs.append(jnp.einsum('bhqk,bhkd->bhqd', a, v[:, :, :end].astype(jnp.float32)))
    o = jnp.concatenate(outs, axis=2)
    return _merge_heads(o.astype(cols.dtype))


def _memory_mixer(cols, mem, norm_mem, w_mem_kv, qn, kn):
    q = _rmsnorm(_split_heads(cols, MEM_HEADS, MEM_HEAD_DIM), qn)
    kv = _rmsnorm(mem, norm_mem) @ w_mem_kv
    k = _rmsnorm(_split_heads(kv[..., :MEM_WIDTH], MEM_HEADS, MEM_HEAD_DIM), kn)
    v = _split_heads(kv[..., MEM_WIDTH:], MEM_HEADS, MEM_HEAD_DIM)
    scores = jnp.einsum('bhsd,bhmd->bhsm', q, k, preferred_element_type=jnp.float32) * (MEM_HEAD_DIM ** -0.5)
    p = jax.nn.softmax(scores, axis=-1)
    o = jnp.einsum('bhsm,bhmd->bhsd', p, v.astype(jnp.float32))
    return _merge_heads(o.astype(cols.dtype))


def _hier_moe(h, w_router_group, b_router_group, w_router_expert, b_router_expert,
              w_exp_gate, w_exp_up, w_exp_down):
    b, s, d = h.shape
    hf = h.reshape(b * s, d)
    n_tok = hf.shape[0]
    g_logits = (hf @ w_router_group).astype(jnp.float32) + b_router_group.astype(jnp.float32)
    g_prob = jax.nn.softmax(g_logits, axis=-1)
    g_top, g_idx = lax.top_k(g_prob, 1)
    e_logits = ((hf @ w_router_expert).astype(jnp.float32)
                + b_router_expert.astype(jnp.float32)).reshape(n_tok, N_GROUPS, EXPERTS_PER_GROUP)
    e_sel = jnp.einsum('nge,ng->ne', e_logits, jax.nn.one_hot(g_idx[:, 0], N_GROUPS, dtype=jnp.float32))
    e_prob = jax.nn.softmax(e_sel, axis=-1)
    e_top, e_idx = lax.top_k(e_prob, TOP_K_IN_GROUP)
    e_w = e_top / jnp.sum(e_top, axis=-1, keepdims=True) * g_top
    expert_id = g_idx * EXPERTS_PER_GROUP + e_idx
    combine = jnp.einsum('nk,nke->ne', e_w, jax.nn.one_hot(expert_id, N_EXPERTS, dtype=jnp.float32))
    out = jnp.zeros((n_tok, d), jnp.float32)
    for e in range(N_EXPERTS):
        y = (jax.nn.silu(hf @ w_exp_gate[e]) * (hf @ w_exp_up[e])) @ w_exp_down[e]
        out = out + combine[:, e:e + 1] * y.astype(jnp.float32)
    return out.astype(h.dtype).reshape(b, s, d)


def setup_inputs(seed: int = 0) -> dict:
    key = jax.random.key(seed)
    ks = jax.random.split(key, 24)
    f32 = jnp.float32
    L = DEPTH

    def nrm(k, shape, fan_in):
        return jax.random.normal(k, shape, f32) * (fan_in ** -0.5)

    def gain(k, shape):
        return 1.0 + 0.02 * jax.random.normal(k, shape, f32)

    return {
        'x': jax.random.normal(ks[0], (BATCH, SEQ, D_MODEL), f32),
        'mem': jax.random.normal(ks[1], (BATCH, MEM_LEN, D_MODEL), f32),
        'positions': jnp.tile(jnp.arange(SEQ, dtype=jnp.int32)[None, :], (BATCH, 1)),
        'norm_mix': gain(ks[2], (L, D_MODEL)),
        'norm_mem': gain(ks[3], (L, D_MODEL)),
        'w_in': nrm(ks[4], (L, D_MODEL, IN_COLS), D_MODEL),
        'b_gate': 0.02 * jax.random.normal(ks[5], (L, GATE_COLS), f32),
        'qn_dil': gain(ks[6], (L, HEAD_DIM)),
        'kn_dil': gain(ks[7], (L, HEAD_DIM)),
        'qn_mem': gain(ks[8], (L, MEM_HEAD_DIM)),
        'kn_mem': gain(ks[9], (L, MEM_HEAD_DIM)),
        'w_mem_kv': nrm(ks[10], (L, D_MODEL, 2 * MEM_WIDTH), D_MODEL),
        'w_o_dil': nrm(ks[11], (L, DIL_WIDTH, D_MODEL), DIL_WIDTH),
        'w_o_sb': nrm(ks[12], (L, SB_WIDTH, D_MODEL), SB_WIDTH),
        'w_o_mem': nrm(ks[13], (L, MEM_WIDTH, D_MODEL), MEM_WIDTH),
        'w_out': nrm(ks[14], (L, D_MODEL, D_MODEL), D_MODEL),
        'norm_ffn': gain(ks[15], (L, D_MODEL)),
        'w_router_group': nrm(ks[16], (L, D_MODEL, N_GROUPS), D_MODEL),
        'b_router_group': 0.01 * jax.random.normal(ks[17], (L, N_GROUPS), f32),
        'w_router_expert': nrm(ks[18], (L, D_MODEL, N_EXPERTS), D_MODEL),
        'b_router_expert': 0.01 * jax.random.normal(ks[19], (L, N_EXPERTS), f32),
        'w_exp_gate': nrm(ks[20], (L, N_EXPERTS, D_MODEL, D_EXPERT), D_MODEL),
        'w_exp_up': nrm(ks[21], (L, N_EXPERTS, D_MODEL, D_EXPERT), D_MODEL),
        'w_exp_down': nrm(ks[22], (L, N_EXPERTS, D_EXPERT, D_MODEL), D_EXPERT),
    }


def reference(x, mem, positions, norm_mix, norm_mem, w_in, b_gate, qn_dil, kn_dil, qn_mem, kn_mem,
              w_mem_kv, w_o_dil, w_o_sb, w_o_mem, w_out, norm_ffn, w_router_group, b_router_group,
              w_router_expert, b_router_expert, w_exp_gate, w_exp_up, w_exp_down):
    b, s, _ = x.shape
    for l in range(DEPTH):
        h = _rmsnorm(x, norm_mix[l])
        proj = h @ w_in[l]
        o_dil = _dilated_mixer(proj[..., :OFF_B], positions, qn_dil[l], kn_dil[l])
        o_sb = _stick_breaking_mixer(proj[..., OFF_B:OFF_M])
        o_mem = _memory_mixer(proj[..., OFF_M:OFF_G], mem, norm_mem[l], w_mem_kv[l], qn_mem[l], kn_mem[l])
        gates = jax.nn.sigmoid(proj[..., OFF_G:] + b_gate[l]).reshape(b, s, N_BRANCHES, D_MODEL)
        merged = (gates[:, :, 0] * (o_dil @ w_o_dil[l])
                  + gates[:, :, 1] * (o_sb @ w_o_sb[l])
                  + gates[:, :, 2] * (o_mem @ w_o_mem[l]))
        x = x + merged @ w_out[l]
        x = x + _hier_moe(_rmsnorm(x, norm_ffn[l]), w_router_group[l], b_router_group[l],
                          w_router_expert[l], b_router_expert[l],
                          w_exp_gate[l], w_exp_up[l], w_exp_down[l])
    return x
```

```python
import numpy as np
from contextlib import ExitStack
import concourse.bass as bass
import concourse.mybir as mybir
from concourse.alu_op_type import AluOpType as ALU
from concourse.bass_utils import run_bass_kernel_spmd

F32 = mybir.dt.float32
BF16 = mybir.dt.bfloat16
I32 = mybir.dt.int32
AF = mybir.ActivationFunctionType
AX = mybir.AxisListType

ENGS = ("pe", "act", "dve", "pool", "sp")
SEM_LIMIT = 30000
NEG = -1.0e30

D = 1024
S = 2048
NT = 16
NSEQ = 2
OFF_B = 4608
OFF_M = 6144
OFF_G = 6656
EPS = 1e-6
NTILE = 80
NSLOT = NTILE * 128
BIGIDX = 1.0e6


class Prog:
    def __init__(self, nc, stack):
        self.nc = nc
        self.stack = stack
        self.ops = []
        self.last_writer = {}
        self.readers = {}
        self.dma_last = {}
        self.out_dmas = []
        self.last_on = {}
        self.pending = {e: set() for e in ENGS}
        self.all_dmas = []
        self.alias = {}

    def barrier(self):
        s = set(self.last_on.values()) | set(self.all_dmas)
        for e in ENGS:
            self.pending[e] |= s
        self.all_dmas = []

    def op(self, eng, fn, reads=(), writes=(), dma_key=None, is_out=False):
        idx = len(self.ops)
        deps = set()
        if self.alias:
            r2 = []
            for k in reads:
                r2.extend(self.alias.get(k, (k,)))
            reads = r2
        for k in reads:
            w = self.last_writer.get(k)
            if w is not None:
                deps.add(w)
        for k in writes:
            w = self.last_writer.get(k)
            if w is not None:
                deps.add(w)
            for r in self.readers.get(k, {}).values():
                deps.add(r)
        if dma_key is not None:
            p = self.dma_last.get(dma_key)
            if p is not None:
                deps.add(p)
            self.dma_last[dma_key] = idx
            self.all_dmas.append(idx)
        deps |= self.pending[eng]
        self.pending[eng] = set()
        deps.discard(idx)
        self.ops.append(dict(eng=eng, fn=fn, deps=deps, dma_key=dma_key))
        for k in reads:
            d = self.readers.setdefault(k, {})
            rk = eng if dma_key is None else ("dma", dma_key)
            d[rk] = idx
        for k in writes:
            self.last_writer[k] = idx
            self.readers[k] = {}
        if dma_key is None:
            self.last_on[eng] = idx
        if is_out:
            self.out_dmas.append(idx)
        return idx

    def emit(self):
        nc = self.nc
        ops = self.ops
        ops.append(dict(eng="sp", fn=None, deps=set(self.out_dmas), dma_key=None))
        has_dep = [False] * len(ops)
        for o in ops:
            for d in o["deps"]:
                has_dep[d] = True
        eng_sem, eng_cnt, dma_sem, dma_cnt = {}, {}, {}, {}
        nsem = [0]

        def new_sem(name):
            nsem[0] += 1
            return self.stack.enter_context(nc.semaphore(f"{name}_{nsem[0]}"))

        for e in ENGS:
            eng_sem[e] = new_sem(f"s_{e}")
            eng_cnt[e] = 0
        events = [None] * len(ops)
        incs = [None] * len(ops)
        for i, o in enumerate(ops):
            if o["dma_key"] is not None:
                k = o["dma_key"]
                if k not in dma_sem or dma_cnt[k] + 16 > SEM_LIMIT:
                    dma_sem[k] = new_sem("d")
                    dma_cnt[k] = 0
                dma_cnt[k] += 16
                events[i] = (dma_sem[k], dma_cnt[k])
                incs[i] = (dma_sem[k], 16)
            elif has_dep[i]:
                e = o["eng"]
                if eng_cnt[e] + 1 > SEM_LIMIT:
                    eng_sem[e] = new_sem(f"s_{e}")
                    eng_cnt[e] = 0
                eng_cnt[e] += 1
                events[i] = (eng_sem[e], eng_cnt[e])
                incs[i] = (eng_sem[e], 1)
        streams = {e: [] for e in ENGS}
        for i, o in enumerate(ops):
            streams[o["eng"]].append(i)

        def run_stream(e, engobj):
            waited = {}
            for i in streams[e]:
                o = ops[i]
                for d in sorted(o["deps"]):
                    od = ops[d]
                    if od["eng"] == "pe" and e == "pe" and od["dma_key"] is None:
                        continue
                    sem, val = events[d]
                    key = id(sem)
                    if waited.get(key, 0) >= val:
                        continue
                    engobj.wait_ge(sem, val)
                    waited[key] = val
                if o["fn"] is None:
                    continue
                ins = o["fn"](engobj)
                if incs[i] is not None:
                    ins.then_inc(incs[i][0], incs[i][1])

        with nc.Block() as block:

            @block.tensor
            def _(eng):
                run_stream("pe", eng)

            @block.scalar
            def _(eng):
                run_stream("act", eng)

            @block.vector
            def _(eng):
                run_stream("dve", eng)

            @block.gpsimd
            def _(eng):
                run_stream("pool", eng)

            @block.sync
            def _(eng):
                run_stream("sp", eng)


def build(stage="full", nseq=NSEQ):
    nc = bass.Bass("TRN2", target_bir_lowering=False)

    def din(name, shape, dt=F32):
        return nc.dram_tensor(name, list(shape), dt, kind="ExternalInput").ap()

    x_d = din("x", [NSEQ, S, D])
    mem_d = din("mem", [NSEQ, 256, D])
    pos_d = din("pos", [NSEQ, 128, NT], I32)
    w_in_d = din("w_in", [D, 9728])
    nmix_d = din("nmix", [128, 8])
    nmem_d = din("nmem", [128, 8])
    nffn_d = din("nffn", [128, 8])
    bgate_d = din("bgate", [128, 24])
    qk4_d = din("qk4", [128, 4, 64])
    qnm_d = din("qnm", [128, 128])
    knm_d = din("knm", [128, 128])
    wkv_d = din("wkv", [D, 1024])
    wo_d = din("wo", [3, 512, D])
    wout_d = din("wout", [D, D])
    wr_d = din("wr", [D, 20])
    br_d = din("br", [128, 20])
    wg_d = din("wg", [2048, 4096])
    wu_d = din("wu", [2048, 4096])
    wd_d = din("wd", [2048, 4096])
    t128_d = din("t128", [128, 80])
    piota_d = din("piota", [128, 1])
    XN_d = nc.dram_tensor("xn_scratch", [NSEQ * S, D], BF16, kind="Internal").ap()
    XS_d = nc.dram_tensor("xs_scratch", [NSLOT, D], BF16, kind="Internal").ap()
    YS_d = nc.dram_tensor("ys_scratch", [NSLOT, D], F32, kind="Internal").ap()
    WBF_d = [nc.dram_tensor(f"wbf_scratch{m}", [2048, 4096], BF16, kind="Internal").ap() for m in range(3)]
    ident_d = din("ident", [128, 128])
    cst_d = din("cst", [128, 6, 128])
    mask_d = din("masks", [128, 23, 128])
    invf_d = din("invf", [128, 32])
    out_d = nc.dram_tensor("out", [NSEQ, S, D], F32, kind="ExternalOutput").ap()
    dbg_d = None
    if stage != "full":
        dbg_d = nc.dram_tensor("dbg", [128, 4, 2048], F32, kind="ExternalOutput").ap()

    with ExitStack() as st:
        def sb(name, shape, dt):
            return st.enter_context(nc.sbuf_tensor("sb_" + name, list(shape), dt))

        def ps(name, shape, dt):
            return st.enter_context(nc.psum_tensor("ps_" + name, list(shape), dt))

        P = Prog(nc, st)

        identf = sb("identf", [128, 128], F32)
        identb = sb("identb", [128, 128], BF16)
        cstb = sb("cstb", [128, 6, 128], BF16)
        maskb = sb("maskb", [128, 23, 128], BF16)
        invf = sb("invf", [128, 32], F32)
        nmix = sb("nmix", [128, 8], F32)
        nmem = sb("nmem", [128, 8], F32)
        nffn = sb("nffn", [128, 8], F32)
        bgate = sb("bgate", [128, 24], F32)
        qk4 = sb("qk4", [128, 4, 64], F32)
        qnm = sb("qnm", [128, 128], F32)
        knm = sb("knm", [128, 128], F32)
        brt = sb("brt", [128, 20], F32)
        wrb = sb("wrb", [128, 8, 20], BF16)
        t128 = sb("t128", [128, 80], F32)
        piota = sb("piota", [128, 1], F32)

        def ld(dst, src, key, eng="sp"):
            P.op(eng, lambda e: e.dma_start(out=dst, in_=src), writes=[key], dma_key="c_" + key)

        ld(identf[:], ident_d, "identf")
        ld(invf[:], invf_d, "invf")
        ld(nmix[:], nmix_d, "nmix")
        ld(nmem[:], nmem_d, "nmem")
        ld(nffn[:], nffn_d, "nffn")
        ld(bgate[:], bgate_d, "bgate")
        ld(qk4[:], qk4_d, "qk4")
        ld(qnm[:], qnm_d, "qnm")
        ld(knm[:], knm_d, "knm")
        ld(brt[:], br_d, "brt")
        ld(t128[:], t128_d, "t128")
        ld(piota[:], piota_d, "piota")
        ld(cstb[:], cst_d, "cstb", eng="pool")
        ld(maskb[:], mask_d, "maskb", eng="pool")
        ld(wrb[:], wr_d.rearrange("(c p) n -> p c n", p=128), "wrb", eng="pool")
        P.op("dve", lambda e: e.tensor_copy(out=identb[:], in_=identf[:]), reads=["identf"], writes=["identb"])
        NTRI = cstb[:, 0, :]
        NONES = cstb[:, 1, :]
        NMSTRICT = cstb[:, 2, :]
        ZEROS = cstb[:, 3, :]
        ONESB = cstb[:, 4, :]

        A_H = sb("A_H", [128, 16384], BF16)
        hT = A_H[:, :].rearrange("p (c t) -> p c t", t=S)
        A_O = sb("A_O", [128, 16384], F32)
        A_Ob = A_O[:, :].bitcast(BF16)
        oT_sb = A_Ob[:, 0:8192].rearrange("p (a t) -> p a t", t=S)
        oT_mem = A_Ob[:, 8192:16384].rearrange("p (a t) -> p a t", t=S)
        oT_dil = A_Ob[:, 16384:24576].rearrange("p (a t) -> p a t", t=S)
        AOs_f = A_O[:, 12288:16384]
        AOs_b = A_Ob[:, 24576:32768]
        acc = A_O[:, :].rearrange("p (j n) -> p j n", n=D)
        AO_KEYS = ["oT_sb", "oT_mem", "oT_dil", "AOs"]
        A_1 = sb("A_1", [128, 12288], F32)
        A_1b = A_1[:, :].bitcast(BF16)
        QT = A_1b[:, 0:8192].rearrange("p (a t) -> p a t", t=S)
        KT = A_1b[:, 8192:16384].rearrange("p (a t) -> p a t", t=S)
        Vt = A_1b[:, 16384:24576].rearrange("p (j n) -> p j n", n=512)
        A1_KEYS = ["QT", "KT", "Vt"]
        wbuf = [sb(f"wbuf{i}", [128, 8, 512], BF16) for i in range(2)]
        xt = [sb(f"xt{i}", [128, D], F32) for i in range(2)]
        xn = [sb(f"xn{i}", [128, D], BF16) for i in range(2)]
        stat = sb("stat", [128, 64], F32)
        st2 = sb("st2", [128, 128], F32)
        hm = [sb(f"hm{i}", [128, 4, 512], BF16) for i in range(2)]
        Lall = sb("Lall", [128, NSEQ * NT, 20], F32)
        rsm = sb("rsm", [128, 128], F32)
        sli = sb("sli", [128, 64], I32)
        idxw = sb("idxw", [128, 80], I32)
        pb = [ps(f"pb{i}", [128, 512], F32) for i in range(8)]

        wctr = [0]

        def load_w(src_aps):
            if not isinstance(src_aps, (list, tuple)):
                src_aps = [src_aps]
            i = wctr[0] % 2
            wctr[0] += 1
            o = 0
            for gi, sap in enumerate(src_aps):
                shp = list(sap.shape)
                dst = wbuf[i][:, 0:shp[1], o:o + shp[2]]
                o += shp[2]
                P.op("pool", lambda e, dst=dst, sap=sap: e.dma_start(out=dst, in_=sap), writes=[f"wbuf{i}" if gi == 0 else f"wbuf{i}g{gi}"],
                     dma_key=f"w{i}_{gi}")
            WK[f"wbuf{i}"] = [f"wbuf{i}"] + [f"wbuf{i}g{gi}" for gi in range(1, 3)]
            return wbuf[i], f"wbuf{i}"

        WK = P.alias
        w_in_r = w_in_d.rearrange("(c p) n -> p c n", p=128)

        def rmsnorm_T(src_dram_tile, gain, gkey, dstT, dst_key, j, nsl, junk=None):
            b = nsl % 2
            P.op("sp", lambda e: e.dma_start(out=xt[b][:], in_=src_dram_tile), writes=[f"xt{b}"], dma_key=f"x{b}")
            norm_tile_T(xt[b][:], f"xt{b}", gain, gkey, dstT, dst_key, j, b, junk)

        def norm_tile_T(src, src_key, gain, gkey, dstT, dst_key, j, b, junk=None):
            ss = stat[:, b:b + 1]
            rs = stat[:, 2 + b:3 + b]
            jout, jkey = (xn[b][:], f"xn{b}") if junk is None else junk
            P.op("act", lambda e: e.activation(out=jout, in_=src, func=AF.Square, accum_out=ss),
                 reads=[src_key], writes=[jkey, f"ss{b}"])
            P.op("act", lambda e: e.activation(out=rs, in_=ss, func=AF.Ln, scale=1.0 / D, bias=EPS),
                 reads=[f"ss{b}"], writes=[f"rs{b}"])
            P.op("act", lambda e: e.activation(out=rs, in_=rs, func=AF.Exp, scale=-0.5),
                 reads=[f"rs{b}"], writes=[f"rs{b}"])
            P.op("dve", lambda e: e.tensor_scalar(out=xn[b][:], in0=src, scalar1=rs, scalar2=None, op0=ALU.mult),
                 reads=[src_key, f"rs{b}"], writes=[f"xn{b}"])
            pT = pb[6 + b][:, 0:512].bitcast(BF16).rearrange("p (c t) -> p c t", t=128)
            for c in range(8):
                P.op("pe", lambda e, c=c: e.transpose(out=pT[:, c, :], in_=xn[b][:, c * 128:(c + 1) * 128], identity=identb[:]),
                     reads=[f"xn{b}", "identb"], writes=[f"pb{6 + b}"])
            P.op("dve", lambda e: e.tensor_tensor(out=dstT[:, :, j * 128:(j + 1) * 128], in0=pT,
                                                  in1=gain[:].unsqueeze(2).to_broadcast([128, 8, 128]), op=ALU.mult),
                 reads=[f"pb{6 + b}", gkey], writes=[dst_key])

        SBTMP = dict(
            eT=[sb(f"sb_e{i}", [128, 512], F32) for i in range(2)],
            spT=[sb(f"sb_sp{i}", [128, 512], BF16) for i in range(2)],
            aT=[sb(f"sb_a{i}", [128, 512], BF16) for i in range(2)],
            spsum=[sb(f"sb_sum{i}", [128, 512], BF16) for i in range(2)],
        )
        eT, spT, aT, spsum_ = SBTMP["eT"], SBTMP["spT"], SBTMP["aT"], SBTMP["spsum"]
        spx = sb("sb_spx", [128, 512], BF16)
        aTx = sb("sb_ax", [128, 512], BF16)

        def headnorm_rstd(ps_ap, ps_key, nh, hd, tmpf, tmpkey, ss_ap, sskey):
            n = nh * hd
            P.op("act", lambda e: e.activation(out=tmpf[:, 0:n], in_=ps_ap, func=AF.Square), reads=[ps_key], writes=[tmpkey])
            P.op("dve", lambda e: e.tensor_reduce(out=ss_ap, in_=tmpf[:, 0:n].rearrange("p (h d) -> p h d", d=hd), axis=AX.X, op=ALU.add),
                 reads=[tmpkey], writes=[sskey])
            P.op("act", lambda e: e.activation(out=ss_ap, in_=ss_ap, func=AF.Ln, scale=1.0 / hd, bias=EPS), reads=[sskey], writes=[sskey])
            P.op("act", lambda e: e.activation(out=ss_ap, in_=ss_ap, func=AF.Exp, scale=-0.5), reads=[sskey], writes=[sskey])

        KTz1 = AOs_b.rearrange("p (a t) -> p a t", t=S)

        def sb_proj():
            P.op("pool", lambda e: e.memset(KT[64:128, :, :], 0.0), writes=["KT"])
            P.op("pool", lambda e: e.memset(KTz1[0:64, :, :], 0.0), writes=["AOs"])
            for qk in range(2):
                dstT = QT if qk == 0 else KT
                dkey = "QT" if qk == 0 else "KT"
                wv, wkey = load_w(w_in_r[:, :, OFF_B + qk * 512: OFF_B + (qk + 1) * 512])
                for p in range(4):
                    for c in range(4):
                        bk = (p * 4 + c) % 4
                        for k in range(8):
                            P.op("pe", lambda e, k=k, p=p, c=c, bk=bk, wv=wv: e.matmul(
                                pb[bk][:, :], lhsT=wv[:, k, p * 128:(p + 1) * 128], rhs=hT[:, k, c * 512:(c + 1) * 512],
                                start=(k == 0), stop=(k == 7)), reads=["hT", wkey], writes=[f"pb{bk}"])
                        if qk == 0:
                            P.op("act", lambda e, p=p, c=c, bk=bk: e.activation(
                                out=QT[:, p, c * 512:(c + 1) * 512], in_=pb[bk][:, :], func=AF.Copy, scale=0.125),
                                reads=[f"pb{bk}"], writes=["QT"])
                        else:
                            P.op("act", lambda e, p=p, c=c, bk=bk: e.activation(
                                out=KT[0:64, p, c * 512:(c + 1) * 512], in_=pb[bk][0:64, :], func=AF.Copy),
                                reads=[f"pb{bk}"], writes=["KT"])
                            P.op("dve", lambda e, p=p, c=c, bk=bk: e.tensor_copy(
                                out=KTz1[64:128, p, c * 512:(c + 1) * 512], in_=pb[bk][64:128, :]),
                                reads=[f"pb{bk}"], writes=["AOs"])
            wv, wkey = load_w(w_in_r[:, :, OFF_B + 1024: OFF_B + 1536])
            for j in range(NT):
                bk = j % 4
                for k in range(8):
                    P.op("pe", lambda e, k=k, j=j, bk=bk, wv=wv: e.matmul(
                        pb[bk][:, :], lhsT=hT[:, k, j * 128:(j + 1) * 128], rhs=wv[:, k, :],
                        start=(k == 0), stop=(k == 7)), reads=["hT", wkey], writes=[f"pb{bk}"])
                P.op("dve", lambda e, j=j, bk=bk: e.tensor_copy(out=Vt[:, j, :], in_=pb[bk][:, :]),
                     reads=[f"pb{bk}"], writes=["Vt"])

        MEMQ = []

        def mem_items(s):
            memT2 = hm[0][:, :, :].rearrange("p a (b c) -> p (a b) c", c=256)
            KTm2 = hm[1][:, 0:2, :].rearrange("p a (b c) -> p (a b) c", c=256)
            Vm2 = hm[1][:, 2:4, :]
            QTm = oT_dil
            items = []

            def norm_mem(mt):
                def f():
                    b = mt % 2
                    P.op("sp", lambda e: e.dma_start(out=xt[b][:], in_=mem_d[s, mt * 128:(mt + 1) * 128, :]), writes=[f"xt{b}"], dma_key=f"x{b}")
                    norm_tile_T(xt[b][:], f"xt{b}", nmem, "nmem", memT2, "hm0", mt, b)
                return f
            items += [norm_mem(0), norm_mem(1)]
            wref = {}

            def loadw(name, src_ap):
                def f():
                    wref[name] = load_w(src_ap)
                return f

            def hn_stats(ti, bank):
                tmpf, tk = xt[ti % 2][:, 0:512], f"xt{ti % 2}"
                ssap = st2[:, (ti % 2) * 4:(ti % 2) * 4 + 4]
                headnorm_rstd(pb[bank][:, :], f"pb{bank}", 4, 128, xt[ti % 2], tk, ssap, f"st2m{ti % 2}")

            def hn_apply(ti, bank, gain_ap, gkey):
                tmpf, tk = xt[ti % 2][:, 0:512], f"xt{ti % 2}"
                ssap, sk = st2[:, (ti % 2) * 4:(ti % 2) * 4 + 4], f"st2m{ti % 2}"
                ob, ok = xn[ti % 2][:, 0:512], f"xn{ti % 2}"
                P.op("dve", lambda e: e.tensor_tensor(out=tmpf.rearrange("p (h d) -> p h d", d=128), in0=pb[bank][:, :].rearrange("p (h d) -> p h d", d=128),
                                                      in1=ssap.unsqueeze(2).to_broadcast([128, 4, 128]), op=ALU.mult), reads=[f"pb{bank}", sk], writes=[tk])
                P.op("dve", lambda e: e.tensor_tensor(out=ob.rearrange("p (h d) -> p h d", d=128), in0=tmpf.rearrange("p (h d) -> p h d", d=128),
                                                      in1=gain_ap.unsqueeze(1).to_broadcast([128, 4, 128]), op=ALU.mult), reads=[tk, gkey], writes=[ok])

            def hn_T(ti, dstT, dkey, col0):
                ob, ok = xn[ti % 2][:, 0:512], f"xn{ti % 2}"
                pT = pb[7][:, 0:256].bitcast(BF16).rearrange("p (h t) -> p h t", t=128)
                for h in range(4):
                    P.op("pe", lambda e, h=h: e.transpose(out=pT[:, h, :], in_=ob[:, h * 128:(h + 1) * 128], identity=identb[:]), reads=[ok, "identb"], writes=["pb7"])
                P.op("act", lambda e: e.activation(out=dstT[:, :, col0:col0 + 128], in_=pT, func=AF.Copy), reads=["pb7"], writes=[dkey])

            items.append(loadw("k", wkv_r[:, :, 0:512]))
            items.append(loadw("v", wkv_r[:, :, 512:1024]))
            for mt in range(2):
                def kproj(mt=mt):
                    wk, wkk = wref["k"]
                    for k in range(8):
                        P.op("pe", lambda e, k=k: e.matmul(pb[6][:, :], lhsT=memT2[:, k, mt * 128:(mt + 1) * 128], rhs=wk[:, k, :], start=(k == 0), stop=(k == 7)),
                             reads=["hm0", wkk], writes=["pb6"])
                    hn_stats(mt, 6)
                items.append(kproj)
                items.append(lambda mt=mt: hn_apply(mt, 6, knm[:, :], "knm"))
                items.append(lambda mt=mt: hn_T(mt, KTm2, "hm1", mt * 128))

                def vproj(mt=mt):
                    wv, wvk = wref["v"]
                    for k in range(8):
                        P.op("pe", lambda e, k=k: e.matmul(pb[6][:, :], lhsT=memT2[:, k, mt * 128:(mt + 1) * 128], rhs=wv[:, k, :], start=(k == 0), stop=(k == 7)),
                             reads=["hm0", wvk], writes=["pb6"])
                    P.op("dve", lambda e: e.tensor_copy(out=Vm2[:, mt, :], in_=pb[6][:, :]), reads=["pb6"], writes=["hm1"])
                items.append(vproj)
            items.append(loadw("q", w_in_r[:, :, OFF_M:OFF_M + 512]))
            for j in range(NT):
                def qproj(j=j):
                    wq, wqk = wref["q"]
                    for k in range(8):
                        P.op("pe", lambda e, k=k: e.matmul(pb[6][:, :], lhsT=hT[:, k, j * 128:(j + 1) * 128], rhs=wq[:, k, :], start=(k == 0), stop=(k == 7)),
                             reads=["hT", wqk], writes=["pb6"])
                    hn_stats(j, 6)
                items.append(qproj)
                items.append(lambda j=j: hn_apply(j, 6, qnm[:, :], "qnm"))
                items.append(lambda j=j: hn_T(j, QTm, "oT_dil", j * 128))
            n = 0
            for c8 in range(8):
                for h in range(4):
                    pm, pk = xn[n % 2][:, 512:1024], f"xn{n % 2}"
                    rc, rk = xt[n % 2][:, 512:768], f"xt{n % 2}"
                    n += 1
                    q0 = c8 * 256

                    def att_a(h=h, q0=q0, pm=pm, pk=pk):
                        for mb in range(2):
                            P.op("pe", lambda e, mb=mb: e.matmul(pb[6][:, mb * 256:(mb + 1) * 256], lhsT=KTm2[:, h, mb * 128:(mb + 1) * 128], rhs=QTm[:, h, q0:q0 + 256],
                                                                 start=True, stop=True), reads=["hm1", "oT_dil"], writes=["pb6"])
                        P.op("act", lambda e: e.activation(out=pm, in_=pb[6][:, :], func=AF.Exp, scale=128.0 ** -0.5), reads=["pb6"], writes=[pk])

                    def att_b(h=h, q0=q0, pm=pm, pk=pk, rc=rc, rk=rk):
                        for mb in range(2):
                            P.op("pe", lambda e, mb=mb: e.matmul(pb[7][:, 0:256], lhsT=Vm2[:, mb, h * 128:(h + 1) * 128], rhs=pm[:, mb * 256:(mb + 1) * 256],
                                                                 start=(mb == 0), stop=(mb == 1)), reads=["hm1", pk], writes=["pb7"])
                        for mb in range(2):
                            P.op("pe", lambda e, mb=mb: e.matmul(pb[7][:, 256:512], lhsT=ONESB, rhs=pm[:, mb * 256:(mb + 1) * 256],
                                                                 start=(mb == 0), stop=(mb == 1)), reads=["cstb", pk], writes=["pb7"])
                        P.op("dve", lambda e: e.reciprocal(out=rc, in_=pb[7][:, 256:512]), reads=["pb7"], writes=[rk])
                        P.op("dve", lambda e: e.tensor_tensor(out=oT_mem[:, h, q0:q0 + 256], in0=pb[7][:, 0:256], in1=rc, op=ALU.mult),
                             reads=["pb7", rk], writes=["oT_mem"])
                    items.append(att_a)
                    items.append(att_b)
            return items

        def mem_drain(k=None):
            cnt = 0
            while MEMQ and (k is None or cnt < k):
                MEMQ.pop(0)()
                cnt += 1

        CONV = [(m_, e_x) for e_x in range(16) for m_ in range(3)]

        def sb_attention():
            its = []
            hcount = 0
            for c in range(4):
                for p in range(4):
                    for hh in range(2):
                        nkb = 4 * c + 4
                        for kb in range(nkb - 1, -1, -1):
                            its.append((c, p, hh, kb, nkb, hcount))
                        hcount += 1
            spT3 = spT + [spx]
            aT3 = aT + [aTx]

            def geom(n):
                c, p, hh, kb, nkb, hc = its[n]
                P0 = 64 * hh
                off = max(0, kb - 4 * c) * 128
                diag = kb >= 4 * c
                ob = 4 + hc % 2
                sm = hc % 2
                return c, p, hh, kb, nkb, hc, P0, off, diag, ob, sm

            def st_a(n):
                if CONV and n % 5 == 0:
                    m_, e_x = CONV.pop(0)
                    wsrc_ = (wg_d, wu_d, wd_d)[m_]
                    P.op("pool", lambda e: e.dma_start(out=WBF_d[m_][e_x * 128:(e_x + 1) * 128, :].rearrange("p (a n) -> p a n", n=2048),
                                                       in_=wsrc_[e_x * 128:(e_x + 1) * 128, :].rearrange("p (a n) -> p a n", n=2048)),
                         writes=["WBFd"], dma_key=f"cv{len(CONV) % 4}")
                if n % 5 in (1, 3):
                    mem_drain(1)
                c, p, hh, kb, nkb, hc, P0, off, diag, ob, sm = geom(n)
                h = 2 * p + hh
                i = n % 2
                zb = i
                e_, s_ = eT[i], spT3[n % 3]
                psO = pb[ob][:, :]
                qs = QT[:, p, c * 512:(c + 1) * 512]
                ks = (KT if hh == 0 else KTz1)[:, p, kb * 128:(kb + 1) * 128]
                if kb == nkb - 1:
                    P.op("pe", lambda e: e.matmul(psO, lhsT=ZEROS, rhs=qs, start=True, stop=False),
                         reads=["cstb", "QT"], writes=[f"pb{ob}"])
                    P.op("pool", lambda e: e.memset(spsum_[sm][:], 0.0), writes=[f"spsum{sm}"])
                P.op("pe", lambda e: e.matmul(pb[zb][:, off:512], lhsT=ks, rhs=qs[:, off:512], start=True, stop=not diag),
                     reads=["KT", "QT", "AOs"], writes=[f"pb{zb}"])
                if diag:
                    P.op("pe", lambda e: e.matmul(pb[zb][:, off:off + 128], lhsT=identb[:], rhs=NMSTRICT, start=False, stop=True),
                         reads=["identb", "cstb"], writes=[f"pb{zb}"])
                P.op("act", lambda e: e.activation(out=e_[:, off:512], in_=pb[zb][:, off:512], func=AF.Exp), reads=[f"pb{zb}"], writes=[f"eT{i}"])
                P.op("act", lambda e: e.activation(out=s_[:, off:512], in_=e_[:, off:512], func=AF.Ln, bias=1.0), reads=[f"eT{i}"], writes=[f"spT{n % 3}"])

            def st_b(n):
                c, p, hh, kb, nkb, hc, P0, off, diag, ob, sm = geom(n)
                cb = 2 + n % 2
                s_, a_ = spT3[n % 3], aT3[n % 3]
                qs = QT[:, p, c * 512:(c + 1) * 512]
                ks = (KT if hh == 0 else KTz1)[:, p, kb * 128:(kb + 1) * 128]
                spsum = spsum_[sm]
                P.op("pe", lambda e: e.matmul(pb[cb][:, off:512], lhsT=ks, rhs=qs[:, off:512], start=True, stop=False),
                     reads=["KT", "QT", "AOs"], writes=[f"pb{cb}"])
                if diag:
                    P.op("pe", lambda e: e.matmul(pb[cb][:, off:off + 128], lhsT=identb[:], rhs=NMSTRICT, start=False, stop=False),
                         reads=["identb", "cstb"], writes=[f"pb{cb}"])
                off2 = off + 128 if diag else 0
                last_is_tri = not (kb < nkb - 1 and off2 < 512)
                P.op("pe", lambda e: e.matmul(pb[cb][:, off:512], lhsT=NTRI, rhs=s_[:, off:512], start=False, stop=last_is_tri),
                     reads=["cstb", f"spT{n % 3}"], writes=[f"pb{cb}"])
                if not last_is_tri:
                    P.op("pe", lambda e: e.matmul(pb[cb][:, off2:512], lhsT=NONES, rhs=spsum[:, off2:512], start=False, stop=True),
                         reads=["cstb", f"spsum{sm}"], writes=[f"pb{cb}"])
                P.op("act", lambda e: e.activation(out=a_[:, off:512], in_=pb[cb][:, off:512], func=AF.Exp), reads=[f"pb{cb}"], writes=[f"aT{n % 3}"])
                if kb > 0:
                    P.op("pool", lambda e: e.tensor_tensor(out=spsum[:, off:512], in0=spsum[:, off:512], in1=s_[:, off:512], op=ALU.add),
                         reads=[f"spT{n % 3}", f"spsum{sm}"], writes=[f"spsum{sm}"])

            def st_c(n):
                c, p, hh, kb, nkb, hc, P0, off, diag, ob, sm = geom(n)
                h = 2 * p + hh
                a_ = aT3[n % 3]
                psO = pb[ob][:, :]
                P.op("pe", lambda e: e.matmul(psO[:, off:512], lhsT=Vt[:, kb, p * 128:(p + 1) * 128], rhs=a_[:, off:512], start=False, stop=(kb == 0)),
                     reads=["Vt", f"aT{n % 3}"], writes=[f"pb{ob}"])
                if kb == 0:
                    P.op("dve", lambda e: e.tensor_copy(out=oT_sb[P0:P0 + 64, p, c * 512:(c + 1) * 512], in_=psO[P0:P0 + 64, :]), reads=[f"pb{ob}"], writes=["oT_sb"])

            NI = len(its)
            for n in range(NI + 2):
                if n < NI:
                    st_a(n)
                if 0 <= n - 1 < NI:
                    st_b(n - 1)
                if 0 <= n - 2 < NI:
                    st_c(n - 2)

        memT = A_1b[:, 8192:8192 + 2048].rearrange("p (c t) -> p c t", t=256)
        KTm = A_1b[:, 8192 + 2048:8192 + 3072].rearrange("p (h t) -> p h t", t=256)
        Vm = A_1b[:, 8192 + 3072:8192 + 4096].rearrange("p (m n) -> p m n", n=512)
        wkv_r = wkv_d.rearrange("(c p) n -> p c n", p=128)

        def headnorm_to_T(ps_ap, ps_key, gain_ap, gkey, dstT, dkey, col0, ti):
            tmpf, tk = eT[ti % 2], f"eT{ti % 2}"
            ssap = st2[:, (ti % 2) * 4:(ti % 2) * 4 + 4]
            sk = f"st2m{ti % 2}"
            headnorm_rstd(ps_ap, ps_key, 4, 128, tmpf, tk, ssap, sk)
            P.op("dve", lambda e: e.tensor_tensor(out=tmpf[:, :].rearrange("p (h d) -> p h d", d=128),
                                                  in0=ps_ap.rearrange("p (h d) -> p h d", d=128),
                                                  in1=ssap.unsqueeze(2).to_broadcast([128, 4, 128]), op=ALU.mult),
                 reads=[ps_key, sk], writes=[tk])
            ob, ok = aT[ti % 2], f"aT{ti % 2}"
            P.op("dve", lambda e: e.tensor_tensor(out=ob[:, :].rearrange("p (h d) -> p h d", d=128),
                                                  in0=tmpf[:, :].rearrange("p (h d) -> p h d", d=128),
                                                  in1=gain_ap.unsqueeze(1).to_broadcast([128, 4, 128]), op=ALU.mult),
                 reads=[tk, gkey], writes=[ok])
            tb = 6 + ti % 2
            pT = pb[tb][:, 0:256].bitcast(BF16).rearrange("p (h t) -> p h t", t=128)
            for h in range(4):
                P.op("pe", lambda e, h=h: e.transpose(out=pT[:, h, :], in_=ob[:, h * 128:(h + 1) * 128], identity=identb[:]),
                     reads=[ok, "identb"], writes=[f"pb{tb}"])
            P.op("act", lambda e: e.activation(out=dstT[:, :, col0:col0 + 128], in_=pT, func=AF.Copy), reads=[f"pb{tb}"], writes=[dkey])

        def mem_mixer(s):
            for mt in range(2):
                rmsnorm_T(mem_d[s, mt * 128:(mt + 1) * 128, :], nmem, "nmem", memT, "KT", mt, mt)
            wk, wkk = load_w(wkv_r[:, :, 0:512])
            wv, wvk = load_w(wkv_r[:, :, 512:1024])
            for mt in range(2):
                bk = mt
                for k in range(8):
                    P.op("pe", lambda e, k=k, mt=mt, bk=bk: e.matmul(pb[bk][:, :], lhsT=memT[:, k, mt * 128:(mt + 1) * 128], rhs=wk[:, k, :],
                                                                      start=(k == 0), stop=(k == 7)), reads=["KT", wkk], writes=[f"pb{bk}"])
                headnorm_to_T(pb[bk][:, :], f"pb{bk}", knm[:, :], "knm", KTm, "KT", mt * 128, mt)
                bv = 2 + mt
                for k in range(8):
                    P.op("pe", lambda e, k=k, mt=mt, bv=bv: e.matmul(pb[bv][:, :], lhsT=memT[:, k, mt * 128:(mt + 1) * 128], rhs=wv[:, k, :],
                                                                      start=(k == 0), stop=(k == 7)), reads=["KT", wvk], writes=[f"pb{bv}"])
                P.op("dve", lambda e, mt=mt, bv=bv: e.tensor_copy(out=Vm[:, mt, :], in_=pb[bv][:, :]), reads=[f"pb{bv}"], writes=["KT"])
            wq, wqk = load_w(w_in_r[:, :, OFF_M:OFF_M + 512])
            def mq_mm(j):
                bk = j % 4
                for k in range(8):
                    P.op("pe", lambda e, k=k, j=j, bk=bk: e.matmul(pb[bk][:, :], lhsT=hT[:, k, j * 128:(j + 1) * 128], rhs=wq[:, k, :],
                                                                    start=(k == 0), stop=(k == 7)), reads=["hT", wqk], writes=[f"pb{bk}"])
            mq_mm(0)
            mq_mm(1)
            for j in range(NT):
                if j + 2 < NT:
                    mq_mm(j + 2)
                bk = j % 4
                headnorm_to_T(pb[bk][:, :], f"pb{bk}", qnm[:, :], "qnm", QT, "QT", j * 128, j)
            n = 0
            for c in range(4):
                for h in range(4):
                    nb_, db_ = 4, 5
                    for mb in range(2):
                        sbk = n % 2
                        pm, pk = spT[n % 2], f"spT{n % 2}"
                        n += 1
                        P.op("pe", lambda e, h=h, c=c, mb=mb, sbk=sbk: e.matmul(pb[sbk][:, :], lhsT=KTm[:, h, mb * 128:(mb + 1) * 128],
                                                                                 rhs=QT[:, h, c * 512:(c + 1) * 512], start=True, stop=True),
                             reads=["KT", "QT"], writes=[f"pb{sbk}"])
                        P.op("act", lambda e, sbk=sbk, pm=pm: e.activation(out=pm[:, :], in_=pb[sbk][:, :], func=AF.Exp, scale=128.0 ** -0.5),
                             reads=[f"pb{sbk}"], writes=[pk])
                        P.op("pe", lambda e, h=h, mb=mb, pm=pm: e.matmul(pb[4][:, :], lhsT=Vm[:, mb, h * 128:(h + 1) * 128], rhs=pm[:, :],
                                                                          start=(mb == 0), stop=(mb == 1)), reads=["KT", pk], writes=["pb4"])
                        P.op("pe", lambda e, mb=mb, pm=pm: e.matmul(pb[5][:, :], lhsT=ONESB, rhs=pm[:, :],
                                                                     start=(mb == 0), stop=(mb == 1)), reads=["cstb", pk], writes=["pb5"])
                    rc, rk = eT[(c * 4 + h) % 2], f"eT{(c * 4 + h) % 2}"
                    P.op("dve", lambda e, rc=rc: e.reciprocal(out=rc[:, :], in_=pb[5][:, :]), reads=["pb5"], writes=[rk])
                    P.op("dve", lambda e, rc=rc, h=h, c=c: e.tensor_tensor(out=oT_mem[:, h, c * 512:(c + 1) * 512], in0=pb[4][:, :], in1=rc[:, :], op=ALU.mult),
                         reads=["pb4", rk], writes=["oT_mem"])

        QKTA = A_1b[:, 0:18432].rearrange("p (a t) -> p a t", t=S)
        VA = A_1b[:, 18432:18432 + 6144].rearrange("p (j g n) -> p j g n", g=3, n=128)
        ropeT = AOs_f.rearrange("p (a j d) -> p a j d", a=4, d=64)
        TWO_PI = float(2 * np.pi)

        def rope_tables(s):
            posi = st2[:, 16:32].bitcast(I32)
            posf = st2[:, 32:48]
            P.op("sp", lambda e: e.dma_start(out=posi, in_=pos_d[s]), writes=["posi"], dma_key="pos")
            P.op("dve", lambda e: e.tensor_copy(out=posf, in_=posi), reads=["posi"], writes=["posf"])
            ang = eT[0][:, :].rearrange("p (j d) -> p j d", d=32)
            P.op("dve", lambda e: e.tensor_tensor(out=ang, in0=posf.unsqueeze(2).to_broadcast([128, NT, 32]),
                                                  in1=invf[:, :].unsqueeze(1).to_broadcast([128, NT, 32]), op=ALU.mult),
                 reads=["posf", "invf"], writes=["eT0"])
            red = eT[1][:, :]
            ki = xt[0][:, 0:512].bitcast(I32)
            kf = xt[0][:, 512:1024]
            for which in range(2):
                shift = 0.0 if which == 0 else float(np.pi / 2)
                P.op("dve", lambda e, shift=shift: e.tensor_scalar(out=red, in0=eT[0][:, :], scalar1=shift, scalar2=None, op0=ALU.add),
                     reads=["eT0"], writes=["eT1"])
                P.op("dve", lambda e: e.tensor_scalar(out=ki, in0=red, scalar1=1.0 / TWO_PI, scalar2=None, op0=ALU.mult), reads=["eT1"], writes=["xt0"])
                P.op("dve", lambda e: e.tensor_copy(out=kf, in_=ki), reads=["xt0"], writes=["xt0"])
                P.op("dve", lambda e: e.scalar_tensor_tensor(out=red, in0=kf, scalar=-TWO_PI, in1=red, op0=ALU.mult, op1=ALU.add),
                     reads=["xt0", "eT1"], writes=["eT1"])
                P.op("dve", lambda e: e.tensor_scalar(out=kf, in0=red, scalar1=float(np.pi), scalar2=None, op0=ALU.is_gt), reads=["eT1"], writes=["xt0"])
                P.op("dve", lambda e: e.scalar_tensor_tensor(out=red, in0=kf, scalar=-TWO_PI, in1=red, op0=ALU.mult, op1=ALU.add),
                     reads=["xt0", "eT1"], writes=["eT1"])
                P.op("dve", lambda e: e.tensor_scalar(out=kf, in0=red, scalar1=float(-np.pi), scalar2=None, op0=ALU.is_lt), reads=["eT1"], writes=["xt0"])
                P.op("dve", lambda e: e.scalar_tensor_tensor(out=red, in0=kf, scalar=TWO_PI, in1=red, op0=ALU.mult, op1=ALU.add),
                     reads=["xt0", "eT1"], writes=["eT1"])
                P.op("dve", lambda e: e.tensor_scalar(out=red, in0=red, scalar1=3.1415925, scalar2=-3.1415925, op0=ALU.min, op1=ALU.max),
                     reads=["eT1"], writes=["eT1"])
                trig = xt[1][:, which * 512:(which + 1) * 512]
                P.op("act", lambda e, trig=trig: e.activation(out=trig, in_=red, func=AF.Sin), reads=["eT1"], writes=["xt1"])
            sin3 = xt[1][:, 0:512].rearrange("p (j d) -> p j d", d=32)
            cos3 = xt[1][:, 512:1024].rearrange("p (j d) -> p j d", d=32)
            for qk in range(2):
                Ct = ropeT[:, 2 * qk, :, :].rearrange("p j (u d) -> p j u d", d=32)
                St = ropeT[:, 2 * qk + 1, :, :].rearrange("p j (u d) -> p j u d", d=32)
                gn = qk4[:, 2 * qk, :].rearrange("p (u d) -> p u d", d=32)
                gs = qk4[:, 2 * qk + 1, :].rearrange("p (u d) -> p u d", d=32)
                P.op("dve", lambda e, Ct=Ct, gn=gn: e.tensor_tensor(out=Ct, in0=cos3.unsqueeze(2).to_broadcast([128, NT, 2, 32]),
                                                                  in1=gn.unsqueeze(1).to_broadcast([128, NT, 2, 32]), op=ALU.mult),
                     reads=["xt1", "qk4"], writes=["AOs"])
                P.op("dve", lambda e, St=St, gs=gs: e.tensor_tensor(out=St, in0=sin3.unsqueeze(2).to_broadcast([128, NT, 2, 32]),
                                                                  in1=gs.unsqueeze(1).to_broadcast([128, NT, 2, 32]), op=ALU.mult),
                     reads=["xt1", "qk4"], writes=["AOs"])
                P.op("dve", lambda e, St=St: e.tensor_scalar(out=St[:, :, 0, :], in0=St[:, :, 0, :], scalar1=-1.0, scalar2=None, op0=ALU.mult),
                     reads=["AOs"], writes=["AOs"])

        def dil_mixer(s):
            rope_tables(s)
            P.op("pool", lambda e: e.memset(QKTA[64:128, 3:6, :], 0.0), writes=["QT", "KT", "Vt"])
            P.op("pool", lambda e: e.memset(QKTA[0:64, 6:9, :], 0.0), writes=["QT", "KT", "Vt"])
            ucount = [0]
            for p in range(4):
                wts = []
                for t in range(2):
                    w_, wk_ = load_w([w_in_r[:, :, t * 1536 + g * 512 + p * 128: t * 1536 + g * 512 + (p + 1) * 128] for g in range(3)])
                    wts.append((w_, wk_))
                def pj_mm(j, wts=wts):
                    for t in range(2):
                        w_, wk_ = wts[t]
                        bk = 2 * (j % 3) + t
                        for k in range(8):
                            P.op("pe", lambda e, k=k, j=j, bk=bk, w_=w_: e.matmul(pb[bk][:, 0:384], lhsT=hT[:, k, j * 128:(j + 1) * 128], rhs=w_[:, k, 0:384],
                                                                                   start=(k == 0), stop=(k == 7)), reads=["hT", wk_], writes=[f"pb{bk}"])

                PEND = []
                pj_mm(0)
                pj_mm(1)
                for j in range(NT):
                    if j + 2 < NT:
                        pj_mm(j + 2)
                    ssq = st2[:, 48 + 12 * (j % 2):48 + 12 * (j % 2) + 12]
                    ssk = f"st2a{j % 2}"
                    obuf, obk = (spT[j % 2], f"spT{j % 2}")
                    for t in range(2):
                        bk = 2 * (j % 3) + t
                        psv = pb[bk][:, 0:384]
                        tf, tfk = eT[t], f"eT{t}"
                        headnorm_rstd(psv, f"pb{bk}", 6, 64, tf, tfk, ssq[:, 6 * t:6 * t + 6], ssk + str(t))
                        Cj = ropeT[:, 2 * t, j, :]
                        Sj = ropeT[:, 2 * t + 1, j, :]
                        ps3 = psv.rearrange("p (a d) -> p a d", d=64)
                        tA = xt[t][:, 0:384].rearrange("p (a d) -> p a d", d=64)
                        tB = xt[t][:, 384:768].rearrange("p (a d) -> p a d", d=64)
                        xk = f"xt{t}"
                        P.op("dve", lambda e, tA=tA, ps3=ps3, Cj=Cj: e.tensor_tensor(out=tA, in0=ps3, in1=Cj.unsqueeze(1).to_broadcast([128, 6, 64]), op=ALU.mult),
                             reads=[f"pb{bk}", "AOs"], writes=[xk])
                        P.op("dve", lambda e, tB=tB, ps3=ps3, Sj=Sj: e.tensor_tensor(out=tB[:, :, 0:32], in0=ps3[:, :, 32:64],
                                                                                      in1=Sj[:, 0:32].unsqueeze(1).to_broadcast([128, 6, 32]), op=ALU.mult),
                             reads=[f"pb{bk}", "AOs"], writes=[xk])
                        P.op("dve", lambda e, tB=tB, ps3=ps3, Sj=Sj: e.tensor_tensor(out=tB[:, :, 32:64], in0=ps3[:, :, 0:32],
                                                                                      in1=Sj[:, 32:64].unsqueeze(1).to_broadcast([128, 6, 32]), op=ALU.mult),
                             reads=[f"pb{bk}", "AOs"], writes=[xk])
                        P.op("pool", lambda e, tA=tA, tB=tB: e.tensor_tensor(out=tA, in0=tA, in1=tB, op=ALU.add), reads=[xk], writes=[xk])
                        rs_ = ssq[:, 6 * t:6 * t + 6]
                        dstb = obuf if t == 0 else aT[j % 2]
                        dstk = obk if t == 0 else f"aT{j % 2}"
                        P.op("pool", lambda e, tA=tA, rs_=rs_, dstb=dstb: e.tensor_tensor(
                            out=dstb[:, 0:384].rearrange("p (a d) -> p a d", d=64),
                            in0=tA, in1=rs_.unsqueeze(2).to_broadcast([128, 6, 64]), op=ALU.mult),
                            reads=[xk, ssk + str(t)], writes=[dstk])
                    tb = 6 + j % 2
                    pT = pb[tb][:, 0:384].bitcast(BF16).rearrange("p (a t) -> p a t", t=128)
                    for t in range(2):
                        srcb = obuf if t == 0 else aT[j % 2]
                        srck = obk if t == 0 else f"aT{j % 2}"
                        for g in range(3):
                            P.op("pe", lambda e, t=t, g=g, srcb=srcb, pT=pT: e.transpose(out=pT[:, 3 * t + g, :], in_=srcb[:, g * 128:(g + 1) * 128], identity=identb[:]),
                                 reads=[srck, "identb"], writes=[f"pb{tb}"])
                    def evac(j=j, pT=pT, tb=tb):
                        P.op("act", lambda e: e.activation(out=QKTA[:, 0:3, j * 128:(j + 1) * 128], in_=pT[:, 0:3, :], func=AF.Copy),
                             reads=[f"pb{tb}"], writes=["QT", "KT", "Vt"])
                        P.op("act", lambda e: e.activation(out=QKTA[0:64, 3:6, j * 128:(j + 1) * 128], in_=pT[0:64, 3:6, :], func=AF.Copy),
                             reads=[f"pb{tb}"], writes=["QT", "KT", "Vt"])
                        P.op("act", lambda e: e.activation(out=QKTA[64:128, 6:9, j * 128:(j + 1) * 128], in_=pT[64:128, 3:6, :], func=AF.Copy),
                             reads=[f"pb{tb}"], writes=["QT", "KT", "Vt"])
                    if PEND:
                        PEND.pop(0)()
                    PEND.append(evac)
                while PEND:
                    PEND.pop(0)()
                w_, wk_ = load_w([w_in_r[:, :, 2 * 1536 + g * 512 + p * 128: 2 * 1536 + g * 512 + (p + 1) * 128] for g in range(3)])
                for j in range(NT):
                    bk = 4 + j % 2
                    for k in range(8):
                        P.op("pe", lambda e, k=k, j=j, bk=bk, w_=w_: e.matmul(pb[bk][:, 0:384], lhsT=hT[:, k, j * 128:(j + 1) * 128], rhs=w_[:, k, 0:384],
                                                                               start=(k == 0), stop=(k == 7)), reads=["hT", wk_], writes=[f"pb{bk}"])
                    P.op("act", lambda e, j=j, bk=bk: e.activation(out=VA[:, j, :, :], in_=pb[bk][:, 0:384].rearrange("p (g n) -> p g n", g=3),
                                                                   func=AF.Copy), reads=[f"pb{bk}"], writes=["QT", "KT", "Vt"])
                units_all = []
                for c in range(4):
                    for hh in range(2):
                        ul = []
                        for jj in range(4 * c - 1, 4 * c + 4):
                            if jj < 0:
                                continue
                            t0_, t1_ = max(jj, 4 * c), min(jj + 1, 4 * c + 3)
                            ul.append((0, jj, t0_ - 4 * c, t1_ - 4 * c + 1, 0 + (t0_ - jj)))
                        for jj in range(4 * c - 4, 4 * c + 4):
                            if jj < 0:
                                continue
                            t0_, t1_ = max(jj, 4 * c), min(jj + 4, 4 * c + 3)
                            ul.append((1, jj, t0_ - 4 * c, t1_ - 4 * c + 1, 2 + (t0_ - jj)))
                        for jj in range(0, 4 * c + 4):
                            t0_ = max(jj, 4 * c)
                            ul.append((2, jj, t0_ - 4 * c, 4, 7 if t0_ == jj else 8))
                        for ui, u in enumerate(ul):
                            units_all.append((c, hh, ui, len(ul), u))
                NB = 4 + (p % 2) * 0
                PMB = [hm[i_][:, k_, :] for i_ in range(2) for k_ in range(4)]
                SBK = [0, 1, 2, 7]

                def stage_a(n):
                    c, hh, ui, nu, (g, jj, ta, tb_, mi) = units_all[n]
                    P0 = 64 * hh
                    lo, hi = ta * 128, tb_ * 128
                    sbk = SBK[n % 4]
                    pm, pk = PMB[n % 8], f"hmA{n % 8}"
                    P.op("pe", lambda e: e.matmul(pb[sbk][:, lo:hi], lhsT=QKTA[:, 3 + 3 * hh + g, jj * 128:(jj + 1) * 128],
                                                  rhs=QKTA[:, g, c * 512 + lo:c * 512 + hi], start=True, stop=True),
                         reads=["QT", "KT", "Vt"], writes=[f"pb{sbk}"])
                    P.op("act", lambda e: e.activation(out=pm[:, lo:hi], in_=pb[sbk][:, lo:hi], func=AF.Exp, scale=0.125),
                         reads=[f"pb{sbk}"], writes=[pk])
                    nt_ = tb_ - ta
                    P.op("dve", lambda e: e.tensor_tensor(out=pm[:, lo:hi].rearrange("p (t q) -> p t q", q=128),
                                                          in0=pm[:, lo:hi].rearrange("p (t q) -> p t q", q=128),
                                                          in1=maskb[:, mi:mi + nt_, :], op=ALU.mult),
                         reads=[pk, "maskb"], writes=[pk])

                def stage_b(n, pp=p):
                    c, hh, ui, nu, (g, jj, ta, tb_, mi) = units_all[n]
                    P0 = 64 * hh
                    lo, hi = ta * 128, tb_ * 128
                    pm, pk = PMB[n % 8], f"hmA{n % 8}"
                    hcn = 2 * c + hh
                    nbk, dbk = 3 + 2 * (hcn % 2), 4 + 2 * (hcn % 2)
                    if ui == 0:
                        for bk_ in (nbk, dbk):
                            P.op("pe", lambda e, bk_=bk_: e.matmul(pb[bk_][:, :], lhsT=ZEROS, rhs=QKTA[:, 0, 0:512],
                                                                   start=True, stop=False), reads=["cstb", "QT"], writes=[f"pb{bk_}"])
                    last = (ui == nu - 1)
                    P.op("pe", lambda e: e.matmul(pb[nbk][:, lo:hi], lhsT=VA[:, jj, g, :], rhs=pm[:, lo:hi], start=False, stop=last),
                         reads=[pk, "QT", "KT", "Vt"], writes=[f"pb{nbk}"])
                    P.op("pe", lambda e: e.matmul(pb[dbk][:, lo:hi], lhsT=ONESB, rhs=pm[:, lo:hi], start=False, stop=last),
                         reads=[pk, "cstb"], writes=[f"pb{dbk}"])
                    if last:
                        rc, rk = eT[hcn % 2], f"eT{hcn % 2}"
                        P.op("act", lambda e: e.activation(out=rc[P0:P0 + 64, :], in_=pb[dbk][P0:P0 + 64, :], func=AF.Ln), reads=[f"pb{dbk}"], writes=[rk])
                        P.op("act", lambda e: e.activation(out=rc[P0:P0 + 64, :], in_=rc[P0:P0 + 64, :], func=AF.Exp, scale=-1.0), reads=[rk], writes=[rk])
                        P.op("dve", lambda e: e.tensor_tensor(out=oT_dil[P0:P0 + 64, pp, c * 512:(c + 1) * 512], in0=pb[nbk][P0:P0 + 64, :],
                                                              in1=rc[P0:P0 + 64, :], op=ALU.mult),
                             reads=[f"pb{nbk}", rk], writes=["oT_dil"])

                NU = len(units_all)
                LAG = 3
                for n in range(NU + LAG):
                    if n < NU:
                        stage_a(n)
                    if n - LAG >= 0:
                        stage_b(n - LAG)

        mergedT = A_1b[:, 0:16384].rearrange("p (c t) -> p c t", t=S)
        wo_dil = A_1b[:, 16384:16384 + 4096].rearrange("p (k n) -> p k n", n=D)
        wo_sbm = AOs_b.rearrange("p (b k n) -> p b k n", b=2, n=D)
        wout_b = AOs_b.rearrange("p (k n) -> p k n", n=D)
        wo_r = wo_d.rearrange("b (k p) n -> p b k n", p=128)
        oTs = [oT_sb, oT_mem, oT_dil]
        oTk = ["oT_sb", "oT_mem", "oT_dil"]
        GOFF = [OFF_G + 1 * D, OFF_G + 2 * D, OFF_G + 0 * D]
        GB = [1, 2, 0]

        def merge_phase(s):
            P.op("pool", lambda e: e.dma_start(out=wo_sbm[:, 0], in_=wo_r[:, 0]), writes=["AOs"], dma_key="wo0")
            P.op("pool", lambda e: e.dma_start(out=wo_sbm[:, 1], in_=wo_r[:, 1]), writes=["AOs"], dma_key="wo0")
            P.op("pool", lambda e: e.dma_start(out=wo_dil, in_=wo_r[:, 2]), writes=["Vt"], dma_key="wo1")
            n = 0
            for f in range(8):
                wgt, wgk = load_w([w_in_r[:, :, GOFF[b] + f * 128: GOFF[b] + (f + 1) * 128] for b in range(3)])
                for c in range(4):
                    mt_ = xt[0][:, 0:512]
                    tt_ = xt[0][:, 512:1024]
                    for b in range(3):
                        gbk, bbk = n % 2, 2 + n % 2
                        gs_, gsk = eT[n % 2], f"eT{n % 2}"
                        n += 1
                        for k in range(8):
                            P.op("pe", lambda e, k=k, b=b, c=c, gbk=gbk, wgt=wgt: e.matmul(pb[gbk][:, :], lhsT=wgt[:, k, b * 128:(b + 1) * 128],
                                                                                          rhs=hT[:, k, c * 512:(c + 1) * 512], start=(k == 0), stop=(k == 7)),
                                 reads=[wgk, "hT"], writes=[f"pb{gbk}"])
                        bias_ap = bgate[:, GB[b] * 8 + f: GB[b] * 8 + f + 1]
                        P.op("act", lambda e, gbk=gbk, gs_=gs_, bias_ap=bias_ap: e.activation(out=gs_[:, :], in_=pb[gbk][:, :], func=AF.Sigmoid, bias=bias_ap),
                             reads=[f"pb{gbk}", "bgate"], writes=[gsk])
                        wsrc = wo_sbm[:, b] if b < 2 else wo_dil
                        wkey = "AOs" if b < 2 else "Vt"
                        for k in range(4):
                            P.op("pe", lambda e, k=k, b=b, c=c, f=f, bbk=bbk, wsrc=wsrc: e.matmul(pb[bbk][:, :], lhsT=wsrc[:, k, f * 128:(f + 1) * 128],
                                                                                                 rhs=oTs[b][:, k, c * 512:(c + 1) * 512], start=(k == 0), stop=(k == 3)),
                                 reads=[wkey, oTk[b]], writes=[f"pb{bbk}"])
                        if b == 0:
                            P.op("dve", lambda e, gs_=gs_, bbk=bbk: e.tensor_tensor(out=mt_, in0=pb[bbk][:, :], in1=gs_[:, :], op=ALU.mult),
                                 reads=[f"pb{bbk}", gsk], writes=["xt0"])
                        else:
                            P.op("dve", lambda e, gs_=gs_, bbk=bbk: e.tensor_tensor(out=tt_, in0=pb[bbk][:, :], in1=gs_[:, :], op=ALU.mult),
                                 reads=[f"pb{bbk}", gsk], writes=["xt0"])
                            dst = mt_ if b == 1 else mergedT[:, f, c * 512:(c + 1) * 512]
                            P.op("dve", lambda e, dst=dst: e.tensor_tensor(out=dst, in0=mt_, in1=tt_, op=ALU.add),
                                 reads=["xt0"], writes=["xt0"] if b == 1 else ["QT", "KT"])
            wout_r = wout_d.rearrange("(k p) n -> p k n", p=128)
            P.op("pool", lambda e: e.dma_start(out=wout_b[:, 0:4], in_=wout_r[:, 0:4]), writes=["AOs"], dma_key="wo0")
            P.op("pool", lambda e: e.dma_start(out=wout_b[:, 4:8], in_=wout_r[:, 4:8]), writes=["AOs"], dma_key="wo0")
            def p5_a(j):
                b = j % 2
                P.op("sp", lambda e: e.dma_start(out=xt[b][:], in_=x_d[s, j * 128:(j + 1) * 128, :]), writes=[f"xt{b}"], dma_key=f"x{b}")
                for hf in range(2):
                    bk = 2 * b + hf
                    for k in range(8):
                        P.op("pe", lambda e, k=k, hf=hf, bk=bk: e.matmul(pb[bk][:, :], lhsT=mergedT[:, k, j * 128:(j + 1) * 128],
                                                                          rhs=wout_b[:, k, hf * 512:(hf + 1) * 512], start=(k == 0), stop=(k == 7)),
                             reads=["QT", "KT", "AOs"], writes=[f"pb{bk}"])
                    P.op("dve", lambda e, hf=hf, bk=bk: e.tensor_tensor(out=xt[b][:, hf * 512:(hf + 1) * 512], in0=pb[bk][:, :],
                                                                         in1=xt[b][:, hf * 512:(hf + 1) * 512], op=ALU.add),
                         reads=[f"pb{bk}", f"xt{b}"], writes=[f"xt{b}"])
                P.op("pool", lambda e: e.dma_start(out=out_d[s, j * 128:(j + 1) * 128, :], in_=xt[b][:]), reads=[f"xt{b}"], writes=[f"outd_{s}_{j}"],
                     dma_key=f"x2st{b}")
                ss = stat[:, b:b + 1]
                rs = stat[:, 2 + b:3 + b]
                P.op("act", lambda e: e.activation(out=xn[b][:], in_=xt[b][:], func=AF.Square, accum_out=ss), reads=[f"xt{b}"], writes=[f"xn{b}", f"ss{b}"])
                P.op("act", lambda e: e.activation(out=rs, in_=ss, func=AF.Ln, scale=1.0 / D, bias=EPS), reads=[f"ss{b}"], writes=[f"rs{b}"])
                P.op("act", lambda e: e.activation(out=rs, in_=rs, func=AF.Exp, scale=-0.5), reads=[f"rs{b}"], writes=[f"rs{b}"])

                def tail():
                    P.op("dve", lambda e: e.tensor_scalar(out=xn[b][:], in0=xt[b][:], scalar1=rs, scalar2=None, op0=ALU.mult),
                         reads=[f"xt{b}", f"rs{b}"], writes=[f"xn{b}"])
                    P.op("pool", lambda e: e.dma_start(out=XN_d[s * S + j * 128: s * S + (j + 1) * 128, :], in_=xn[b][:]), reads=[f"xn{b}"], writes=[f"XNd_{s}_{j}"],
                         dma_key=f"xnst{b}")
                return tail

            def p5_b(j):
                b = j % 2
                pT = pb[6 + b][:, 0:512].bitcast(BF16).rearrange("p (c t) -> p c t", t=128)
                for c in range(8):
                    P.op("pe", lambda e, c=c: e.transpose(out=pT[:, c, :], in_=xn[b][:, c * 128:(c + 1) * 128], identity=identb[:]),
                         reads=[f"xn{b}", "identb"], writes=[f"pb{6 + b}"])
                P.op("dve", lambda e: e.tensor_tensor(out=hT[:, :, j * 128:(j + 1) * 128], in0=pT,
                                                      in1=nffn[:].unsqueeze(2).to_broadcast([128, 8, 128]), op=ALU.mult),
                     reads=[f"pb{6 + b}", "nffn"], writes=["hT"])

            def p5_c(j):
                b = j % 2
                for k in range(8):
                    P.op("pe", lambda e, k=k: e.matmul(pb[4 + b][:, 0:20], lhsT=hT[:, k, j * 128:(j + 1) * 128], rhs=wrb[:, k, :],
                                                       start=(k == 0), stop=(k == 7)), reads=["hT", "wrb"], writes=[f"pb{4 + b}"])
                P.op("dve", lambda e: e.tensor_tensor(out=Lall[:, s * NT + j, :], in0=pb[4 + b][:, 0:20], in1=brt[:, :], op=ALU.add),
                     reads=[f"pb{4 + b}", "brt"], writes=["Lall"])

            for n in range(NT + 2):
                tl = p5_a(n) if n < NT else None
                if 0 <= n - 1 < NT:
                    p5_b(n - 1)
                if 0 <= n - 2 < NT:
                    p5_c(n - 2)
                if tl is not None:
                    tl()

        BREG = {}

        def breg(e, val):
            if val not in BREG:
                BREG[val] = e.to_reg(val)
            return BREG[val]

        def moe_sparse():
            NTT = NSEQ * NT
            P.barrier()
            R_ = A_1[:, :]
            rk = "A1"
            o = [0]

            def T(n, m=NTT):
                v = R_[:, o[0]:o[0] + m * n].rearrange("p (t n) -> p t n", n=n)
                o[0] += m * n
                return v
            G = Lall[:, :, 0:4]
            E = Lall[:, :, 4:20].rearrange("p t (g e) -> p t g e", e=4)
            gmax, ohg, gex, gsum, gtop = T(1), T(4), T(4), T(1), T(1)
            prod, esel, m1, oh1, esel2, m2, oh2 = T(16), T(4), T(1), T(4), T(4), T(1), T(4)
            dd, ed, den = T(1), T(1), T(1)
            selA, selB, sel, cntS, offs, slot, tmp16 = T(16), T(16), T(16), T(16), T(16), T(16), T(16)
            ne, q_, kf, gt_, pc, base = T(1, 16), T(1, 16), T(1, 16), T(1, 16), T(1, 16), T(1, 16)
            ki = T(1, 16).bitcast(I32)
            cmpE = T(16, NTILE)
            Et, neq, idxf = T(1, NTILE), T(1, NTILE), T(1, NTILE)
            selbf = T(8).bitcast(BF16)
            w1 = rsm[:, 0:32].unsqueeze(2)
            w2 = rsm[:, 32:64].unsqueeze(2)
            slotA_f = rsm[:, 64:96]
            slotB_f = rsm[:, 96:128]
            slotA_i = sli[:, 0:32]
            slotB_i = sli[:, 32:64]
            idxW_i = idxw[:, :]

            def V(fn, reads=("Lall",), eng="dve"):
                P.op(eng, fn, reads=list(reads) + [rk, "rsm"], writes=[rk, "rsm"])

            def bc(v, n):
                return v.to_broadcast([128, NTT, n])
            V(lambda e: e.tensor_reduce(out=gmax[:, :, 0], in_=G, axis=AX.X, op=ALU.max))
            V(lambda e: e.tensor_tensor(out=ohg, in0=G, in1=bc(gmax, 4), op=ALU.is_equal))
            V(lambda e: e.tensor_tensor(out=gex, in0=G, in1=bc(gmax, 4), op=ALU.subtract))
            V(lambda e: e.activation(out=gex, in_=gex, func=AF.Exp), eng="act")
            V(lambda e: e.tensor_reduce(out=gsum[:, :, 0], in_=gex, axis=AX.X, op=ALU.add))
            V(lambda e: e.reciprocal(out=gtop, in_=gsum))
            prod4 = prod.rearrange("p t (g e) -> p t g e", e=4)
            V(lambda e: e.tensor_tensor(out=prod4, in0=E, in1=ohg.unsqueeze(3).to_broadcast([128, NTT, 4, 4]), op=ALU.mult))
            V(lambda e: e.tensor_reduce(out=esel, in_=prod.rearrange("p t (g e) -> p t e g", e=4), axis=AX.X, op=ALU.add))
            V(lambda e: e.tensor_reduce(out=m1[:, :, 0], in_=esel, axis=AX.X, op=ALU.max))
            V(lambda e: e.tensor_tensor(out=oh1, in0=esel, in1=bc(m1, 4), op=ALU.is_equal))
            V(lambda e: e.scalar_tensor_tensor(out=esel2, in0=oh1, scalar=NEG, in1=esel, op0=ALU.mult, op1=ALU.add))
            V(lambda e: e.tensor_reduce(out=m2[:, :, 0], in_=esel2, axis=AX.X, op=ALU.max))
            V(lambda e: e.tensor_tensor(out=oh2, in0=esel2, in1=bc(m2, 4), op=ALU.is_equal))
            V(lambda e: e.tensor_tensor(out=dd, in0=m2, in1=m1, op=ALU.subtract))
            V(lambda e: e.activation(out=ed, in_=dd, func=AF.Exp), eng="act")
            V(lambda e: e.tensor_scalar(out=den, in0=ed, scalar1=1.0, scalar2=None, op0=ALU.add))
            V(lambda e: e.reciprocal(out=den, in_=den))
            V(lambda e: e.tensor_tensor(out=w1, in0=gtop, in1=den, op=ALU.mult))
            V(lambda e: e.tensor_tensor(out=w2, in0=w1, in1=ed, op=ALU.mult))
            for sl, oh in ((selA, oh1), (selB, oh2)):
                V(lambda e, sl=sl, oh=oh: e.tensor_tensor(out=sl.rearrange("p t (g e) -> p t g e", e=4),
                                                         in0=ohg.unsqueeze(3).to_broadcast([128, NTT, 4, 4]),
                                                         in1=oh.unsqueeze(2).to_broadcast([128, NTT, 4, 4]), op=ALU.mult))
            V(lambda e: e.tensor_tensor(out=sel, in0=selA, in1=selB, op=ALU.add))
            V(lambda e: e.tensor_copy(out=selbf, in_=sel))
            selbf2 = selbf.rearrange("p t e -> p (t e)")
            P.op("pe", lambda e: e.matmul(pb[0][:, :], lhsT=ONESB, rhs=selbf2, start=True, stop=True), reads=[rk, "cstb"], writes=["pb0"])
            P.op("pe", lambda e: e.matmul(pb[1][:, :], lhsT=cstb[:, 5, :], rhs=selbf2, start=True, stop=True), reads=[rk, "cstb"], writes=["pb1"])
            V(lambda e: e.tensor_copy(out=cntS, in_=pb[0][:, :].rearrange("p (t e) -> p t e", e=16)), reads=("pb0",))
            V(lambda e: e.memset(offs[:, 0, :], 0.0))
            for j in range(1, NTT):
                V(lambda e, j=j: e.tensor_tensor(out=offs[:, j, :], in0=offs[:, j - 1, :], in1=cntS[:, j - 1, :], op=ALU.add))
            ne2, q2, kf2, gt2, pc2, base2 = [v[:, :, 0] for v in (ne, q_, kf, gt_, pc, base)]
            ki2 = ki[:, :, 0]
            V(lambda e: e.tensor_tensor(out=ne2, in0=offs[:, NTT - 1, :], in1=cntS[:, NTT - 1, :], op=ALU.add))
            V(lambda e: e.tensor_scalar(out=q2, in0=ne2, scalar1=127.0, scalar2=1.0 / 128, op0=ALU.add, op1=ALU.mult))
            V(lambda e: e.tensor_copy(out=ki2, in_=q2))
            V(lambda e: e.tensor_copy(out=kf2, in_=ki2))
            V(lambda e: e.tensor_tensor(out=gt2, in0=kf2, in1=q2, op=ALU.is_gt))
            V(lambda e: e.tensor_tensor(out=kf2, in0=kf2, in1=gt2, op=ALU.subtract))
            V(lambda e: e.tensor_scalar(out=pc2, in0=kf2, scalar1=128.0, scalar2=None, op0=ALU.mult))
            V(lambda e: e.memset(base2[:, 0:1], 0.0))
            for ex in range(1, 16):
                V(lambda e, ex=ex: e.tensor_tensor(out=base2[:, ex:ex + 1], in0=base2[:, ex - 1:ex], in1=pc2[:, ex - 1:ex], op=ALU.add))
            V(lambda e: e.tensor_tensor(out=slot, in0=pb[1][:, :].rearrange("p (t e) -> p t e", e=16), in1=offs, op=ALU.add), reads=("pb1",))
            V(lambda e: e.tensor_tensor(out=slot, in0=slot, in1=base2.unsqueeze(1).to_broadcast([128, NTT, 16]), op=ALU.add))
            for sl, dstf, dsti in ((selA, slotA_f, slotA_i), (selB, slotB_f, slotB_i)):
                V(lambda e, sl=sl: e.tensor_tensor(out=tmp16, in0=sl, in1=slot, op=ALU.mult))
                V(lambda e, dstf=dstf: e.tensor_reduce(out=dstf, in_=tmp16, axis=AX.X, op=ALU.add))
                V(lambda e, dstf=dstf, dsti=dsti: e.tensor_copy(out=dsti, in_=dstf))
            V(lambda e: e.tensor_tensor(out=cmpE, in0=base2.unsqueeze(1).to_broadcast([128, NTILE, 16]),
                                        in1=t128[:, :].unsqueeze(2).to_broadcast([128, NTILE, 16]), op=ALU.is_le), reads=("t128",))
            Et2, neq2, idxf2 = Et[:, :, 0], neq[:, :, 0], idxf[:, :, 0]
            V(lambda e: e.tensor_reduce(out=Et2, in_=cmpE, axis=AX.X, op=ALU.add))
            V(lambda e: e.memset(neq2[:, 0:2], 1.0))
            V(lambda e: e.tensor_tensor(out=neq2[:, 2:NTILE], in0=Et2[:, 2:NTILE], in1=Et2[:, 0:NTILE - 2], op=ALU.not_equal))
            V(lambda e: e.tensor_scalar(out=idxf2, in0=Et2, scalar1=128.0, scalar2=-(128.0 + BIGIDX), op0=ALU.mult, op1=ALU.add))
            V(lambda e: e.tensor_scalar(out=idxf2, in0=idxf2, scalar1=piota[:, 0:1], scalar2=None, op0=ALU.add), reads=("piota",))
            V(lambda e: e.tensor_tensor(out=idxf2, in0=idxf2, in1=neq2, op=ALU.mult))
            V(lambda e: e.tensor_scalar(out=idxf2, in0=idxf2, scalar1=BIGIDX, scalar2=None, op0=ALU.add))
            P.op("dve", lambda e: e.tensor_copy(out=idxW_i, in_=idxf2), reads=[rk], writes=["st2i"])
            P.barrier()
            WB = [A_1b[:, i * 12288:(i + 1) * 12288] for i in range(2)]
            for t in range(2):
                for m in range(3):
                    P.op("pool", lambda e, m=m, t=t: e.indirect_dma_start(
                        out=WB[t][:, m * 4096:(m + 1) * 4096], out_offset=None, in_=WBF_d[m][:, :], in_offset=bass.IndirectOffsetOnAxis(ap=idxW_i[:, t:t + 1], axis=0),
                        bounds_check=breg(e, 2047), oob_is_err=False), reads=["st2i"], writes=[f"wb{t}{m}"], dma_key=f"wg{t}{m}")
            xsb = [xn[0][:, :], xn[1][:, :], xt[0][:, :].bitcast(BF16)[:, 0:1024], xt[1][:, :].bitcast(BF16)[:, 0:1024]]
            xsk = ["xn0", "xn1", "xt0", "xt1"]
            for jt in range(NTT):
                b = jt % 4
                P.op("sp", lambda e, jt=jt, b=b: e.dma_start(out=xsb[b], in_=XN_d[jt * 128:(jt + 1) * 128, :]), reads=["XNd"], writes=[xsk[b]], dma_key=f"xnl{b}")
                for si, sl_i in enumerate((slotA_i, slotB_i)):
                    P.op("pool", lambda e, jt=jt, b=b, sl_i=sl_i: e.indirect_dma_start(
                        out=XS_d[:, :], out_offset=bass.IndirectOffsetOnAxis(ap=sl_i[:, jt:jt + 1], axis=0), in_=xsb[b], in_offset=None,
                        bounds_check=breg(e, NSLOT - 1), oob_is_err=False), reads=[xsk[b], "rsm"], writes=[f"XSd_{jt}_{si}"], dma_key=f"xsc{b}{si}")
            P.barrier()
            WB = [A_1b[:, i * 12288:(i + 1) * 12288] for i in range(2)]
            xsT = [hm[i][:, 0:2, :].rearrange("p a (b c) -> p (a b) c", c=128) for i in range(2)]
            hTb = [aT[i][:, :].rearrange("p (a c) -> p a c", c=128) for i in range(2)]

            def et_wload(t, ms):
                b = t % 2
                for m in ms:
                    P.op("pool", lambda e, m=m: e.indirect_dma_start(
                        out=WB[b][:, m * 4096:(m + 1) * 4096], out_offset=None, in_=WBF_d[m][:, :], in_offset=bass.IndirectOffsetOnAxis(ap=idxW_i[:, t:t + 1], axis=0),
                        bounds_check=breg(e, 2047), oob_is_err=False), reads=["st2i"], writes=[f"wb{b}{m}"], dma_key=f"wg{b}{m}")

            def et_xload(t):
                b = t % 2
                P.op("sp", lambda e: e.dma_start(out=xn[b][:], in_=XS_d[t * 128:(t + 1) * 128, :]), writes=[f"xn{b}"], dma_key=f"xsl{b}")

            SG = [spT[0], spT[1]]
            HB = [spx, aTx]

            def et_T(t):
                b = t % 2
                pT = pb[6 + b][:, 0:512].bitcast(BF16).rearrange("p (c t) -> p c t", t=128)
                for c in range(8):
                    P.op("pe", lambda e, c=c: e.transpose(out=pT[:, c, :], in_=xn[b][:, c * 128:(c + 1) * 128], identity=identb[:]),
                         reads=[f"xn{b}", "identb"], writes=[f"pb{6 + b}"])
                P.op("dve", lambda e: e.tensor_tensor(out=xsT[b], in0=pT, in1=nffn[:].unsqueeze(2).to_broadcast([128, 8, 128]), op=ALU.mult),
                     reads=[f"pb{6 + b}", "nffn"], writes=[f"xsT{b}"])

            def et_GU(t):
                b = t % 2
                wgb = WB[b][:, 0:4096].rearrange("p (k n) -> p k n", n=512)
                wub = WB[b][:, 4096:8192].rearrange("p (k n) -> p k n", n=512)
                for k in range(8):
                    P.op("pe", lambda e, k=k: e.matmul(pb[0][:, :], lhsT=xsT[b][:, k, :], rhs=wgb[:, k, :], start=(k == 0), stop=(k == 7)),
                         reads=[f"xsT{b}", f"wb{b}0"], writes=["pb0"])
                for k in range(8):
                    P.op("pe", lambda e, k=k: e.matmul(pb[1][:, :], lhsT=xsT[b][:, k, :], rhs=wub[:, k, :], start=(k == 0), stop=(k == 7)),
                         reads=[f"xsT{b}", f"wb{b}1"], writes=["pb1"])
                P.op("act", lambda e: e.activation(out=SG[b][:, :], in_=pb[0][:, :], func=AF.Silu), reads=["pb0"], writes=[f"etsg{b}"])
                P.op("dve", lambda e: e.tensor_tensor(out=HB[b][:, :], in0=pb[1][:, :], in1=SG[b][:, :], op=ALU.mult), reads=["pb1", f"etsg{b}"], writes=[f"ethb{b}"])

            def et_s2(t):
                b = t % 2
                wdb = WB[b][:, 8192:12288].rearrange("p (k n) -> p k n", n=D)
                pH = pb[2][:, 0:256].bitcast(BF16).rearrange("p (c t) -> p c t", t=128)
                for c in range(4):
                    P.op("pe", lambda e, c=c: e.transpose(out=pH[:, c, :], in_=HB[b][:, c * 128:(c + 1) * 128], identity=identb[:]),
                         reads=[f"ethb{b}", "identb"], writes=["pb2"])
                P.op("act", lambda e: e.activation(out=hTb[b], in_=pH, func=AF.Copy), reads=["pb2"], writes=[f"hTb{b}"])
                for hf in range(2):
                    yb = 4 + hf
                    for hc in range(4):
                        P.op("pe", lambda e, hc=hc, hf=hf, yb=yb: e.matmul(pb[yb][:, :], lhsT=hTb[b][:, hc, :], rhs=wdb[:, hc, hf * 512:(hf + 1) * 512],
                                                                           start=(hc == 0), stop=(hc == 3)), reads=[f"hTb{b}", f"wb{b}2"], writes=[f"pb{yb}"])
                    if hf == 0:
                        P.op("act", lambda e: e.activation(out=xt[b][:, 0:512], in_=pb[4][:, :], func=AF.Copy), reads=["pb4"], writes=[f"xt{b}"])
                    else:
                        P.op("dve", lambda e: e.tensor_copy(out=xt[b][:, 512:1024], in_=pb[5][:, :]), reads=["pb5"], writes=[f"xt{b}"])
                P.op("sp", lambda e: e.dma_start(out=YS_d[t * 128:(t + 1) * 128, :], in_=xt[b][:]), reads=[f"xt{b}"], writes=[f"YSd_{t}"], dma_key=f"yst{b}")

            et_xload(0)
            et_xload(1)
            for n in range(NTILE + 3):
                if n < NTILE:
                    et_T(n)
                if 0 <= n - 3 < NTILE:
                    et_s2(n - 3)
                if 2 <= n - 1 < NTILE:
                    et_wload(n - 1, (2,))
                if 0 <= n - 1 < NTILE:
                    et_GU(n - 1)
                if 2 <= n + 1 < NTILE:
                    et_wload(n + 1, (0, 1))
                if 2 <= n + 2 < NTILE:
                    et_xload(n + 2)
            P.barrier()
            yAB = [[A_O[:, (2 * i + k) * 1024:(2 * i + k + 1) * 1024] for k in range(2)] for i in range(2)]
            x2b = [A_O[:, (4 + i) * 1024:(5 + i) * 1024] for i in range(2)]
            for jt in range(NTT):
                b = jt % 2
                s_, j_ = jt // NT, jt % NT
                rows = out_d[s_, j_ * 128:(j_ + 1) * 128, :]
                P.op("sp", lambda e, rows=rows, b=b: e.dma_start(out=x2b[b], in_=rows), reads=["outd"], writes=[f"x2b{b}"], dma_key=f"x2l{b}")
                for k, sl_i in enumerate((slotA_i, slotB_i)):
                    P.op("pool", lambda e, jt=jt, b=b, k=k, sl_i=sl_i: e.indirect_dma_start(
                        out=yAB[b][k], out_offset=None, in_=YS_d[:, :], in_offset=bass.IndirectOffsetOnAxis(ap=sl_i[:, jt:jt + 1], axis=0),
                        bounds_check=breg(e, NSLOT - 1), oob_is_err=False), reads=["rsm", "YSd"], writes=[f"yab{b}{k}"], dma_key=f"yg{b}{k}")
                for k, wv_ in enumerate((rsm[:, 0:32], rsm[:, 32:64])):
                    P.op("dve", lambda e, jt=jt, b=b, k=k, wv_=wv_: e.scalar_tensor_tensor(out=x2b[b], in0=yAB[b][k], scalar=wv_[:, jt:jt + 1], in1=x2b[b],
                                                                                       op0=ALU.mult, op1=ALU.add),
                         reads=[f"yab{b}{k}", f"x2b{b}", "rsm"], writes=[f"x2b{b}"])
                P.op("act", lambda e, rows=rows, b=b: e.dma_start(out=rows, in_=x2b[b]), reads=[f"x2b{b}"], writes=[f"outd2_{jt}"], dma_key=f"ost{b}", is_out=True)

        for s in range(nseq):
            for j in range(NT):
                rmsnorm_T(x_d[s, j * 128:(j + 1) * 128, :], nmix, "nmix", hT, "hT", j, j,
                          junk=(hm[0][:, 2 * (j % 2):2 * (j % 2) + 2, :].rearrange("p a b -> p (a b)"), f"hmj{j % 2}"))
            if stage in ("full", "sb", "sbproj", "merge"):
                sb_proj()
                if stage == "sbproj":
                    break
                if stage in ("full", "merge"):
                    MEMQ.extend(mem_items(s))
                sb_attention()
                mem_drain()
                if stage == "sb":
                    break
            if stage == "mem":
                mem_mixer(s)
                break
            if stage in ("full", "dil", "merge"):
                dil_mixer(s)
                if stage == "dil":
                    break
            merge_phase(s)
            if stage == "merge":
                break

        if stage == "full":
            moe_sparse()

        dsrc = {"sbproj": (QT, ["QT"]), "sb": (oT_sb, ["oT_sb"]), "mem": (oT_mem, ["oT_mem"]), "dil": (oT_dil, ["oT_dil"]),
                "merge": (mergedT, ["QT", "KT"])}.get(stage)
        if dsrc is not None:
            n = 0
            for a in range(4):
                for hf in range(2):
                    b = n % 2
                    n += 1
                    P.op("dve", lambda e, a=a, hf=hf, b=b: e.tensor_copy(out=xt[b][:], in_=dsrc[0][:, a, hf * 1024:(hf + 1) * 1024]),
                         reads=dsrc[1], writes=[f"xt{b}"])
                    P.op("sp", lambda e, a=a, hf=hf, b=b: e.dma_start(out=dbg_d[:, a, hf * 1024:(hf + 1) * 1024], in_=xt[b][:]),
                         reads=[f"xt{b}"], dma_key=f"dbg{b}", is_out=True)
        P.emit()
    return nc


def _consts():
    j = np.arange(128)[:, None]
    s_ = np.arange(128)[None, :]
    cst = np.zeros((128, 6, 128), np.float32)
    cst[:, 0, :] = -(j >= s_).astype(np.float32)
    cst[:, 1, :] = -1.0
    cst[:, 2, :] = np.where(j < s_, 0.0, NEG)
    cst[:, 3, :] = 0.0
    cst[:, 4, :] = 1.0
    cst[:, 5, :] = (j < s_).astype(np.float32)
    k = np.arange(128)[:, None]
    q = np.arange(128)[None, :]
    masks = np.zeros((128, 23, 128), np.float32)
    masks[:, 0, :] = (k <= q)
    masks[:, 1, :] = (q <= k)
    same4 = (k % 4) == (q % 4)
    for off in range(5):
        ok = same4.copy()
        if off == 0:
            ok &= (k <= q)
        if off == 4:
            ok &= (q <= k)
        masks[:, 2 + off, :] = ok
    same16 = (k % 16) == (q % 16)
    masks[:, 7, :] = same16 & (k <= q)
    for r in range(8, 12):
        masks[:, r, :] = same16
    invf = (10000.0 ** (-np.arange(0, 64, 2, dtype=np.float32) / 64)).astype(np.float32)
    invf = np.ascontiguousarray(np.broadcast_to(invf[None, :], (128, 32))).astype(np.float32)
    return cst, masks, invf


def _pc(v):
    return np.ascontiguousarray(np.asarray(v, np.float32).reshape(-1, 128).T)


def _rows(w, k):
    w = np.asarray(w, np.float32)
    e, kp, n = w.shape
    return np.ascontiguousarray(w.reshape(e, k, 128, n).transpose(0, 2, 1, 3).reshape(e * 128, k * n))


def make_in_maps(inputs):
    f = lambda a: np.ascontiguousarray(np.asarray(a))
    x = f(inputs["x"]); mem = f(inputs["mem"]); pos = f(inputs["positions"])
    cst, masks, invf = _consts()
    qn = np.asarray(inputs["qn_dil"], np.float32)[0]
    kn = np.asarray(inputs["kn_dil"], np.float32)[0]
    qk4 = np.zeros((128, 4, 64), np.float32)
    qk4[:, 0, :] = qn[None, :]
    qk4[:, 1, :] = np.concatenate([qn[32:], qn[:32]])[None, :]
    qk4[:, 2, :] = kn[None, :]
    qk4[:, 3, :] = np.concatenate([kn[32:], kn[:32]])[None, :]
    shared = dict(
        w_in=f(inputs["w_in"][0]),
        nmix=_pc(inputs["norm_mix"][0]), nmem=_pc(inputs["norm_mem"][0]), nffn=_pc(inputs["norm_ffn"][0]),
        bgate=_pc(inputs["b_gate"][0]),
        qk4=qk4,
        qnm=np.ascontiguousarray(np.broadcast_to(np.asarray(inputs["qn_mem"], np.float32)[0][None, :], (128, 128))),
        knm=np.ascontiguousarray(np.broadcast_to(np.asarray(inputs["kn_mem"], np.float32)[0][None, :], (128, 128))),
        wkv=f(inputs["w_mem_kv"][0]),
        wo=np.ascontiguousarray(np.stack([inputs["w_o_sb"][0], inputs["w_o_mem"][0], inputs["w_o_dil"][0]])),
        wout=f(inputs["w_out"][0]),
        wr=np.ascontiguousarray(np.concatenate([inputs["w_router_group"][0], inputs["w_router_expert"][0]], axis=1)),
        br=np.ascontiguousarray(np.broadcast_to(np.concatenate([inputs["b_router_group"][0], inputs["b_router_expert"][0]])[None, :], (128, 20))).astype(np.float32),
        wg=_rows(inputs["w_exp_gate"][0], 8), wu=_rows(inputs["w_exp_up"][0], 8), wd=_rows(inputs["w_exp_down"][0], 4),
        t128=np.ascontiguousarray(np.broadcast_to((128.0 * np.arange(80, dtype=np.float32))[None, :], (128, 80))),
        piota=np.arange(128, dtype=np.float32).reshape(128, 1),
        ident=np.eye(128, dtype=np.float32), cst=cst, masks=masks, invf=invf,
    )
    maps = []
    for c in range(8):
        d = dict(shared)
        d["x"] = np.ascontiguousarray(x[2 * c:2 * c + 2])
        d["mem"] = np.ascontiguousarray(mem[2 * c:2 * c + 2])
        p = pos[2 * c:2 * c + 2].astype(np.int32).reshape(2, NT, 128).transpose(0, 2, 1)
        d["pos"] = np.ascontiguousarray(p)
        maps.append(d)
    return maps


def kernel(**inputs):
    nc = build("full")
    maps = make_in_maps(inputs)
    res = run_bass_kernel_spmd(nc, maps, core_ids=list(range(8)))
    out = np.concatenate([np.asarray(r["out"]) for r in res.results], axis=0)
    return out.astype(np.float32)
```

```python
import numpy as np
from contextlib import ExitStack
import concourse.bass as bass
import concourse.mybir as mybir
from concourse.alu_op_type import AluOpType as ALU
from concourse.bass_utils import run_bass_kernel_spmd

F32 = mybir.dt.float32
BF16 = mybir.dt.bfloat16
I32 = mybir.dt.int32
AF = mybir.ActivationFunctionType
AX = mybir.AxisListType

ENGS = ("pe", "act", "dve", "pool", "sp")
SEM_LIMIT = 30000
NEG = -1.0e30

D = 1024
S = 2048
NT = 16
NSEQ = 2
OFF_B = 4608
OFF_M = 6144
OFF_G = 6656
EPS = 1e-6
NTILE = 80
NSLOT = NTILE * 128
BIGIDX = 1.0e6


class Prog:
    def __init__(self, nc, stack):
        self.nc = nc
        self.stack = stack
        self.ops = []
        self.last_writer = {}
        self.readers = {}
        self.dma_last = {}
        self.out_dmas = []
        self.last_on = {}
        self.pending = {e: set() for e in ENGS}
        self.all_dmas = []
        self.alias = {}

    def barrier(self):
        s = set(self.last_on.values()) | set(self.all_dmas)
        for e in ENGS:
            self.pending[e] |= s
        self.all_dmas = []

    def op(self, eng, fn, reads=(), writes=(), dma_key=None, is_out=False):
        idx = len(self.ops)
        deps = set()
        if self.alias:
            r2 = []
            for k in reads:
                r2.extend(self.alias.get(k, (k,)))
            reads = r2
        for k in reads:
            w = self.last_writer.get(k)
            if w is not None:
                deps.add(w)
        for k in writes:
            w = self.last_writer.get(k)
            if w is not None:
                deps.add(w)
            for r in self.readers.get(k, {}).values():
                deps.add(r)
        if dma_key is not None:
            p = self.dma_last.get(dma_key)
            if p is not None:
                deps.add(p)
            self.dma_last[dma_key] = idx
            self.all_dmas.append(idx)
        deps |= self.pending[eng]
        self.pending[eng] = set()
        deps.discard(idx)
        self.ops.append(dict(eng=eng, fn=fn, deps=deps, dma_key=dma_key))
        for k in reads:
            d = self.readers.setdefault(k, {})
            rk = eng if dma_key is None else ("dma", dma_key)
            d[rk] = idx
        for k in writes:
            self.last_writer[k] = idx
            self.readers[k] = {}
        if dma_key is None:
            self.last_on[eng] = idx
        if is_out:
            self.out_dmas.append(idx)
        return idx

    def emit(self):
        nc = self.nc
        ops = self.ops
        ops.append(dict(eng="sp", fn=None, deps=set(self.out_dmas), dma_key=None))
        has_dep = [False] * len(ops)
        for o in ops:
            for d in o["deps"]:
                has_dep[d] = True
        eng_sem, eng_cnt, dma_sem, dma_cnt = {}, {}, {}, {}
        nsem = [0]

        def new_sem(name):
            nsem[0] += 1
            return self.stack.enter_context(nc.semaphore(f"{name}_{nsem[0]}"))

        for e in ENGS:
            eng_sem[e] = new_sem(f"s_{e}")
            eng_cnt[e] = 0
        events = [None] * len(ops)
        incs = [None] * len(ops)
        for i, o in enumerate(ops):
            if o["dma_key"] is not None:
                k = o["dma_key"]
                if k not in dma_sem or dma_cnt[k] + 16 > SEM_LIMIT:
                    dma_sem[k] = new_sem("d")
                    dma_cnt[k] = 0
                dma_cnt[k] += 16
                events[i] = (dma_sem[k], dma_cnt[k])
                incs[i] = (dma_sem[k], 16)
            elif has_dep[i]:
                e = o["eng"]
                if eng_cnt[e] + 1 > SEM_LIMIT:
                    eng_sem[e] = new_sem(f"s_{e}")
                    eng_cnt[e] = 0
                eng_cnt[e] += 1
                events[i] = (eng_sem[e], eng_cnt[e])
                incs[i] = (eng_sem[e], 1)
        streams = {e: [] for e in ENGS}
        for i, o in enumerate(ops):
            streams[o["eng"]].append(i)

        def run_stream(e, engobj):
            waited = {}
            for i in streams[e]:
                o = ops[i]
                for d in sorted(o["deps"]):
                    od = ops[d]
                    if od["eng"] == "pe" and e == "pe" and od["dma_key"] is None:
                        continue
                    sem, val = events[d]
                    key = id(sem)
                    if waited.get(key, 0) >= val:
                        continue
                    engobj.wait_ge(sem, val)
                    waited[key] = val
                if o["fn"] is None:
                    continue
                ins = o["fn"](engobj)
                if incs[i] is not None:
                    ins.then_inc(incs[i][0], incs[i][1])

        with nc.Block() as block:

            @block.tensor
            def _(eng):
                run_stream("pe", eng)

            @block.scalar
            def _(eng):
                run_stream("act", eng)

            @block.vector
            def _(eng):
                run_stream("dve", eng)

            @block.gpsimd
            def _(eng):
                run_stream("pool", eng)

            @block.sync
            def _(eng):
                run_stream("sp", eng)


def build(stage="full", nseq=NSEQ):
    nc = bass.Bass("TRN2", target_bir_lowering=False)

    def din(name, shape, dt=F32):
        return nc.dram_tensor(name, list(shape), dt, kind="ExternalInput").ap()

    x_d = din("x", [NSEQ, S, D])
    mem_d = din("mem", [NSEQ, 256, D])
    pos_d = din("pos", [NSEQ, 128, NT], I32)
    w_in_d = din("w_in", [D, 9728])
    nmix_d = din("nmix", [128, 8])
    nmem_d = din("nmem", [128, 8])
    nffn_d = din("nffn", [128, 8])
    bgate_d = din("bgate", [128, 24])
    qk4_d = din("qk4", [128, 4, 64])
    qnm_d = din("qnm", [128, 128])
    knm_d = din("knm", [128, 128])
    wkv_d = din("wkv", [D, 1024])
    wo_d = din("wo", [3, 512, D])
    wout_d = din("wout", [D, D])
    wr_d = din("wr", [D, 20])
    br_d = din("br", [128, 20])
    wg_d = din("wg", [2048, 4096])
    wu_d = din("wu", [2048, 4096])
    wd_d = din("wd", [2048, 4096])
    t128_d = din("t128", [128, 80])
    piota_d = din("piota", [128, 1])
    XN_d = nc.dram_tensor("xn_scratch", [NSEQ * S, D], BF16, kind="Internal").ap()
    XS_d = nc.dram_tensor("xs_scratch", [NSLOT, D], BF16, kind="Internal").ap()
    YS_d = nc.dram_tensor("ys_scratch", [NSLOT, D], F32, kind="Internal").ap()
    WBF_d = [nc.dram_tensor(f"wbf_scratch{m}", [2048, 4096], BF16, kind="Internal").ap() for m in range(3)]
    ident_d = din("ident", [128, 128])
    cst_d = din("cst", [128, 6, 128])
    mask_d = din("masks", [128, 23, 128])
    invf_d = din("invf", [128, 32])
    out_d = nc.dram_tensor("out", [NSEQ, S, D], F32, kind="ExternalOutput").ap()
    dbg_d = None
    if stage != "full":
        dbg_d = nc.dram_tensor("dbg", [128, 4, 2048], F32, kind="ExternalOutput").ap()

    with ExitStack() as st:
        def sb(name, shape, dt):
            return st.enter_context(nc.sbuf_tensor("sb_" + name, list(shape), dt))

        def ps(name, shape, dt):
            return st.enter_context(nc.psum_tensor("ps_" + name, list(shape), dt))

        P = Prog(nc, st)

        identf = sb("identf", [128, 128], F32)
        identb = sb("identb", [128, 128], BF16)
        cstb = sb("cstb", [128, 6, 128], BF16)
        maskb = sb("maskb", [128, 23, 128], BF16)
        invf = sb("invf", [128, 32], F32)
        nmix = sb("nmix", [128, 8], F32)
        nmem = sb("nmem", [128, 8], F32)
        nffn = sb("nffn", [128, 8], F32)
        bgate = sb("bgate", [128, 24], F32)
        qk4 = sb("qk4", [128, 4, 64], F32)
        qnm = sb("qnm", [128, 128], F32)
        knm = sb("knm", [128, 128], F32)
        brt = sb("brt", [128, 20], F32)
        wrb = sb("wrb", [128, 8, 20], BF16)
        t128 = sb("t128", [128, 80], F32)
        piota = sb("piota", [128, 1], F32)

        def ld(dst, src, key, eng="sp"):
            P.op(eng, lambda e: e.dma_start(out=dst, in_=src), writes=[key], dma_key="c_" + key)

        ld(identf[:], ident_d, "identf")
        ld(invf[:], invf_d, "invf")
        ld(nmix[:], nmix_d, "nmix")
        ld(nmem[:], nmem_d, "nmem")
        ld(nffn[:], nffn_d, "nffn")
        ld(bgate[:], bgate_d, "bgate")
        ld(qk4[:], qk4_d, "qk4")
        ld(qnm[:], qnm_d, "qnm")
        ld(knm[:], knm_d, "knm")
        ld(brt[:], br_d, "brt")
        ld(t128[:], t128_d, "t128")
        ld(piota[:], piota_d, "piota")
        ld(cstb[:], cst_d, "cstb", eng="pool")
        ld(maskb[:], mask_d, "maskb", eng="pool")
        ld(wrb[:], wr_d.rearrange("(c p) n -> p c n", p=128), "wrb", eng="pool")
        P.op("dve", lambda e: e.tensor_copy(out=identb[:], in_=identf[:]), reads=["identf"], writes=["identb"])
        NTRI = cstb[:, 0, :]
        NONES = cstb[:, 1, :]
        NMSTRICT = cstb[:, 2, :]
        ZEROS = cstb[:, 3, :]
        ONESB = cstb[:, 4, :]

        A_H = sb("A_H", [128, 16384], BF16)
        hT = A_H[:, :].rearrange("p (c t) -> p c t", t=S)
        A_O = sb("A_O", [128, 16384], F32)
        A_Ob = A_O[:, :].bitcast(BF16)
        oT_sb = A_Ob[:, 0:8192].rearrange("p (a t) -> p a t", t=S)
        oT_mem = A_Ob[:, 8192:16384].rearrange("p (a t) -> p a t", t=S)
        oT_dil = A_Ob[:, 16384:24576].rearrange("p (a t) -> p a t", t=S)
        AOs_f = A_O[:, 12288:16384]
        AOs_b = A_Ob[:, 24576:32768]
        acc = A_O[:, :].rearrange("p (j n) -> p j n", n=D)
        AO_KEYS = ["oT_sb", "oT_mem", "oT_dil", "AOs"]
        A_1 = sb("A_1", [128, 12288], F32)
        A_1b = A_1[:, :].bitcast(BF16)
        QT = A_1b[:, 0:8192].rearrange("p (a t) -> p a t", t=S)
        KT = A_1b[:, 8192:16384].rearrange("p (a t) -> p a t", t=S)
        Vt = A_1b[:, 16384:24576].rearrange("p (j n) -> p j n", n=512)
        A1_KEYS = ["QT", "KT", "Vt"]
        wbuf = [sb(f"wbuf{i}", [128, 8, 512], BF16) for i in range(2)]
        xt = [sb(f"xt{i}", [128, D], F32) for i in range(2)]
        xn = [sb(f"xn{i}", [128, D], BF16) for i in range(2)]
        stat = sb("stat", [128, 64], F32)
        st2 = sb("st2", [128, 128], F32)
        hm = [sb(f"hm{i}", [128, 4, 512], BF16) for i in range(2)]
        Lall = sb("Lall", [128, NSEQ * NT, 20], F32)
        rsm = sb("rsm", [128, 128], F32)
        sli = sb("sli", [128, 64], I32)
        idxw = sb("idxw", [128, 80], I32)
        pb = [ps(f"pb{i}", [128, 512], F32) for i in range(8)]

        wctr = [0]

        def load_w(src_aps):
            if not isinstance(src_aps, (list, tuple)):
                src_aps = [src_aps]
            i = wctr[0] % 2
            wctr[0] += 1
            o = 0
            for gi, sap in enumerate(src_aps):
                shp = list(sap.shape)
                dst = wbuf[i][:, 0:shp[1], o:o + shp[2]]
                o += shp[2]
                P.op("pool", lambda e, dst=dst, sap=sap: e.dma_start(out=dst, in_=sap), writes=[f"wbuf{i}" if gi == 0 else f"wbuf{i}g{gi}"],
                     dma_key=f"w{i}_{gi}")
            WK[f"wbuf{i}"] = [f"wbuf{i}"] + [f"wbuf{i}g{gi}" for gi in range(1, 3)]
            return wbuf[i], f"wbuf{i}"

        WK = P.alias
        w_in_r = w_in_d.rearrange("(c p) n -> p c n", p=128)

        def rmsnorm_T(src_dram_tile, gain, gkey, dstT, dst_key, j, nsl, junk=None):
            b = nsl % 2
            P.op("sp", lambda e: e.dma_start(out=xt[b][:], in_=src_dram_tile), writes=[f"xt{b}"], dma_key=f"x{b}")
            norm_tile_T(xt[b][:], f"xt{b}", gain, gkey, dstT, dst_key, j, b, junk)

        def norm_tile_T(src, src_key, gain, gkey, dstT, dst_key, j, b, junk=None):
            ss = stat[:, b:b + 1]
            rs = stat[:, 2 + b:3 + b]
            jout, jkey = (xn[b][:], f"xn{b}") if junk is None else junk
            P.op("act", lambda e: e.activation(out=jout, in_=src, func=AF.Square, accum_out=ss),
                 reads=[src_key], writes=[jkey, f"ss{b}"])
            P.op("act", lambda e: e.activation(out=rs, in_=ss, func=AF.Ln, scale=1.0 / D, bias=EPS),
                 reads=[f"ss{b}"], writes=[f"rs{b}"])
            P.op("act", lambda e: e.activation(out=rs, in_=rs, func=AF.Exp, scale=-0.5),
                 reads=[f"rs{b}"], writes=[f"rs{b}"])
            P.op("dve", lambda e: e.tensor_scalar(out=xn[b][:], in0=src, scalar1=rs, scalar2=None, op0=ALU.mult),
                 reads=[src_key, f"rs{b}"], writes=[f"xn{b}"])
            pT = pb[6 + b][:, 0:512].bitcast(BF16).rearrange("p (c t) -> p c t", t=128)
            for c in range(8):
                P.op("pe", lambda e, c=c: e.transpose(out=pT[:, c, :], in_=xn[b][:, c * 128:(c + 1) * 128], identity=identb[:]),
                     reads=[f"xn{b}", "identb"], writes=[f"pb{6 + b}"])
            P.op("dve", lambda e: e.tensor_tensor(out=dstT[:, :, j * 128:(j + 1) * 128], in0=pT,
                                                  in1=gain[:].unsqueeze(2).to_broadcast([128, 8, 128]), op=ALU.mult),
                 reads=[f"pb{6 + b}", gkey], writes=[dst_key])

        SBTMP = dict(
            eT=[sb(f"sb_e{i}", [128, 512], F32) for i in range(2)],
            spT=[sb(f"sb_sp{i}", [128, 512], BF16) for i in range(2)],
            aT=[sb(f"sb_a{i}", [128, 512], BF16) for i in range(2)],
            spsum=[sb(f"sb_sum{i}", [128, 512], BF16) for i in range(2)],
        )
        eT, spT, aT, spsum_ = SBTMP["eT"], SBTMP["spT"], SBTMP["aT"], SBTMP["spsum"]
        spx = sb("sb_spx", [128, 512], BF16)
        aTx = sb("sb_ax", [128, 512], BF16)

        def headnorm_rstd(ps_ap, ps_key, nh, hd, tmpf, tmpkey, ss_ap, sskey):
            n = nh * hd
            P.op("act", lambda e: e.activation(out=tmpf[:, 0:n], in_=ps_ap, func=AF.Square), reads=[ps_key], writes=[tmpkey])
            P.op("dve", lambda e: e.tensor_reduce(out=ss_ap, in_=tmpf[:, 0:n].rearrange("p (h d) -> p h d", d=hd), axis=AX.X, op=ALU.add),
                 reads=[tmpkey], writes=[sskey])
            P.op("act", lambda e: e.activation(out=ss_ap, in_=ss_ap, func=AF.Ln, scale=1.0 / hd, bias=EPS), reads=[sskey], writes=[sskey])
            P.op("act", lambda e: e.activation(out=ss_ap, in_=ss_ap, func=AF.Exp, scale=-0.5), reads=[sskey], writes=[sskey])

        KTz1 = AOs_b.rearrange("p (a t) -> p a t", t=S)

        def sb_proj():
            P.op("pool", lambda e: e.memset(KT[64:128, :, :], 0.0), writes=["KT"])
            P.op("pool", lambda e: e.memset(KTz1[0:64, :, :], 0.0), writes=["AOs"])
            for qk in range(2):
                dstT = QT if qk == 0 else KT
                dkey = "QT" if qk == 0 else "KT"
                wv, wkey = load_w(w_in_r[:, :, OFF_B + qk * 512: OFF_B + (qk + 1) * 512])
                for p in range(4):
                    for c in range(4):
                        bk = (p * 4 + c) % 4
                        for k in range(8):
                            P.op("pe", lambda e, k=k, p=p, c=c, bk=bk, wv=wv: e.matmul(
                                pb[bk][:, :], lhsT=wv[:, k, p * 128:(p + 1) * 128], rhs=hT[:, k, c * 512:(c + 1) * 512],
                                start=(k == 0), stop=(k == 7)), reads=["hT", wkey], writes=[f"pb{bk}"])
                        if qk == 0:
                            P.op("act", lambda e, p=p, c=c, bk=bk: e.activation(
                                out=QT[:, p, c * 512:(c + 1) * 512], in_=pb[bk][:, :], func=AF.Copy, scale=0.125),
                                reads=[f"pb{bk}"], writes=["QT"])
                        else:
                            P.op("act", lambda e, p=p, c=c, bk=bk: e.activation(
                                out=KT[0:64, p, c * 512:(c + 1) * 512], in_=pb[bk][0:64, :], func=AF.Copy),
                                reads=[f"pb{bk}"], writes=["KT"])
                            P.op("dve", lambda e, p=p, c=c, bk=bk: e.tensor_copy(
                                out=KTz1[64:128, p, c * 512:(c + 1) * 512], in_=pb[bk][64:128, :]),
                                reads=[f"pb{bk}"], writes=["AOs"])
            wv, wkey = load_w(w_in_r[:, :, OFF_B + 1024: OFF_B + 1536])
            for j in range(NT):
                bk = j % 4
                for k in range(8):
                    P.op("pe", lambda e, k=k, j=j, bk=bk, wv=wv: e.matmul(
                        pb[bk][:, :], lhsT=hT[:, k, j * 128:(j + 1) * 128], rhs=wv[:, k, :],
                        start=(k == 0), stop=(k == 7)), reads=["hT", wkey], writes=[f"pb{bk}"])
                P.op("dve", lambda e, j=j, bk=bk: e.tensor_copy(out=Vt[:, j, :], in_=pb[bk][:, :]),
                     reads=[f"pb{bk}"], writes=["Vt"])

        MEMQ = []

        def mem_items(s):
            memT2 = hm[0][:, :, :].rearrange("p a (b c) -> p (a b) c", c=256)
            KTm2 = hm[1][:, 0:2, :].rearrange("p a (b c) -> p (a b) c", c=256)
            Vm2 = hm[1][:, 2:4, :]
            QTm = oT_dil
            items = []

            def norm_mem(mt):
                def f():
                    b = mt % 2
                    P.op("sp", lambda e: e.dma_start(out=xt[b][:], in_=mem_d[s, mt * 128:(mt + 1) * 128, :]), writes=[f"xt{b}"], dma_key=f"x{b}")
                    norm_tile_T(xt[b][:], f"xt{b}", nmem, "nmem", memT2, "hm0", mt, b)
                return f
            items += [norm_mem(0), norm_mem(1)]
            wref = {}

            def loadw(name, src_ap):
                def f():
                    wref[name] = load_w(src_ap)
                return f

            def hn_stats(ti, bank):
                tmpf, tk = xt[ti % 2][:, 0:512], f"xt{ti % 2}"
                ssap = st2[:, (ti % 2) * 4:(ti % 2) * 4 + 4]
                headnorm_rstd(pb[bank][:, :], f"pb{bank}", 4, 128, xt[ti % 2], tk, ssap, f"st2m{ti % 2}")

            def hn_apply(ti, bank, gain_ap, gkey):
                tmpf, tk = xt[ti % 2][:, 0:512], f"xt{ti % 2}"
                ssap, sk = st2[:, (ti % 2) * 4:(ti % 2) * 4 + 4], f"st2m{ti % 2}"
                ob, ok = xn[ti % 2][:, 0:512], f"xn{ti % 2}"
                P.op("dve", lambda e: e.tensor_tensor(out=tmpf.rearrange("p (h d) -> p h d", d=128), in0=pb[bank][:, :].rearrange("p (h d) -> p h d", d=128),
                                                      in1=ssap.unsqueeze(2).to_broadcast([128, 4, 128]), op=ALU.mult), reads=[f"pb{bank}", sk], writes=[tk])
                P.op("dve", lambda e: e.tensor_tensor(out=ob.rearrange("p (h d) -> p h d", d=128), in0=tmpf.rearrange("p (h d) -> p h d", d=128),
                                                      in1=gain_ap.unsqueeze(1).to_broadcast([128, 4, 128]), op=ALU.mult), reads=[tk, gkey], writes=[ok])

            def hn_T(ti, dstT, dkey, col0):
                ob, ok = xn[ti % 2][:, 0:512], f"xn{ti % 2}"
                pT = pb[7][:, 0:256].bitcast(BF16).rearrange("p (h t) -> p h t", t=128)
                for h in range(4):
                    P.op("pe", lambda e, h=h: e.transpose(out=pT[:, h, :], in_=ob[:, h * 128:(h + 1) * 128], identity=identb[:]), reads=[ok, "identb"], writes=["pb7"])
                P.op("act", lambda e: e.activation(out=dstT[:, :, col0:col0 + 128], in_=pT, func=AF.Copy), reads=["pb7"], writes=[dkey])

            items.append(loadw("k", wkv_r[:, :, 0:512]))
            items.append(loadw("v", wkv_r[:, :, 512:1024]))
            for mt in range(2):
                def kproj(mt=mt):
                    wk, wkk = wref["k"]
                    for k in range(8):
                        P.op("pe", lambda e, k=k: e.matmul(pb[6][:, :], lhsT=memT2[:, k, mt * 128:(mt + 1) * 128], rhs=wk[:, k, :], start=(k == 0), stop=(k == 7)),
                             reads=["hm0", wkk], writes=["pb6"])
                    hn_stats(mt, 6)
                items.append(kproj)
                items.append(lambda mt=mt: hn_apply(mt, 6, knm[:, :], "knm"))
                items.append(lambda mt=mt: hn_T(mt, KTm2, "hm1", mt * 128))

                def vproj(mt=mt):
                    wv, wvk = wref["v"]
                    for k in range(8):
                        P.op("pe", lambda e, k=k: e.matmul(pb[6][:, :], lhsT=memT2[:, k, mt * 128:(mt + 1) * 128], rhs=wv[:, k, :], start=(k == 0), stop=(k == 7)),
                             reads=["hm0", wvk], writes=["pb6"])
                    P.op("dve", lambda e: e.tensor_copy(out=Vm2[:, mt, :], in_=pb[6][:, :]), reads=["pb6"], writes=["hm1"])
                items.append(vproj)
            items.append(loadw("q", w_in_r[:, :, OFF_M:OFF_M + 512]))
            for j in range(NT):
                def qproj(j=j):
                    wq, wqk = wref["q"]
                    for k in range(8):
                        P.op("pe", lambda e, k=k: e.matmul(pb[6][:, :], lhsT=hT[:, k, j * 128:(j + 1) * 128], rhs=wq[:, k, :], start=(k == 0), stop=(k == 7)),
                             reads=["hT", wqk], writes=["pb6"])
                    hn_stats(j, 6)
                items.append(qproj)
                items.append(lambda j=j: hn_apply(j, 6, qnm[:, :], "qnm"))
                items.append(lambda j=j: hn_T(j, QTm, "oT_dil", j * 128))
            n = 0
            for c8 in range(8):
                for h in range(4):
                    pm, pk = xn[n % 2][:, 512:1024], f"xn{n % 2}"
                    rc, rk = xt[n % 2][:, 512:768], f"xt{n % 2}"
                    n += 1
                    q0 = c8 * 256

                    def att_a(h=h, q0=q0, pm=pm, pk=pk):
                        for mb in range(2):
                            P.op("pe", lambda e, mb=mb: e.matmul(pb[6][:, mb * 256:(mb + 1) * 256], lhsT=KTm2[:, h, mb * 128:(mb + 1) * 128], rhs=QTm[:, h, q0:q0 + 256],
                                                                 start=True, stop=True), reads=["hm1", "oT_dil"], writes=["pb6"])
                        P.op("act", lambda e: e.activation(out=pm, in_=pb[6][:, :], func=AF.Exp, scale=128.0 ** -0.5), reads=["pb6"], writes=[pk])

                    def att_b(h=h, q0=q0, pm=pm, pk=pk, rc=rc, rk=rk):
                        for mb in range(2):
                            P.op("pe", lambda e, mb=mb: e.matmul(pb[7][:, 0:256], lhsT=Vm2[:, mb, h * 128:(h + 1) * 128], rhs=pm[:, mb * 256:(mb + 1) * 256],
                                                                 start=(mb == 0), stop=(mb == 1)), reads=["hm1", pk], writes=["pb7"])
                        for mb in range(2):
                            P.op("pe", lambda e, mb=mb: e.matmul(pb[7][:, 256:512], lhsT=ONESB, rhs=pm[:, mb * 256:(mb + 1) * 256],
                                                                 start=(mb == 0), stop=(mb == 1)), reads=["cstb", pk], writes=["pb7"])
                        P.op("dve", lambda e: e.reciprocal(out=rc, in_=pb[7][:, 256:512]), reads=["pb7"], writes=[rk])
                        P.op("dve", lambda e: e.tensor_tensor(out=oT_mem[:, h, q0:q0 + 256], in0=pb[7][:, 0:256], in1=rc, op=ALU.mult),
                             reads=["pb7", rk], writes=["oT_mem"])
                    items.append(att_a)
                    items.append(att_b)
            return items

        def mem_drain(k=None):
            cnt = 0
            while MEMQ and (k is None or cnt < k):
                MEMQ.pop(0)()
                cnt += 1

        CONV = [(m_, e_x) for e_x in range(16) for m_ in range(3)]

        def sb_attention():
            its = []
            hcount = 0
            for c in range(4):
                for p in range(4):
                    for hh in range(2):
                        nkb = 4 * c + 4
                        for kb in range(nkb - 1, -1, -1):
                            its.append((c, p, hh, kb, nkb, hcount))
                        hcount += 1
            spT3 = spT + [spx]
            aT3 = aT + [aTx]

            def geom(n):
                c, p, hh, kb, nkb, hc = its[n]
                P0 = 64 * hh
                off = max(0, kb - 4 * c) * 128
                diag = kb >= 4 * c
                ob = 4 + hc % 2
                sm = hc % 2
                return c, p, hh, kb, nkb, hc, P0, off, diag, ob, sm

            def st_a(n):
                if CONV and n % 5 == 0:
                    m_, e_x = CONV.pop(0)
                    wsrc_ = (wg_d, wu_d, wd_d)[m_]
                    P.op("pool", lambda e: e.dma_start(out=WBF_d[m_][e_x * 128:(e_x + 1) * 128, :].rearrange("p (a n) -> p a n", n=2048),
                                                       in_=wsrc_[e_x * 128:(e_x + 1) * 128, :].rearrange("p (a n) -> p a n", n=2048)),
                         writes=["WBFd"], dma_key=f"cv{len(CONV) % 4}")
                if n % 5 in (1, 3):
                    mem_drain(1)
                c, p, hh, kb, nkb, hc, P0, off, diag, ob, sm = geom(n)
                h = 2 * p + hh
                i = n % 2
                zb = i
                e_, s_ = eT[i], spT3[n % 3]
                psO = pb[ob][:, :]
                qs = QT[:, p, c * 512:(c + 1) * 512]
                ks = (KT if hh == 0 else KTz1)[:, p, kb * 128:(kb + 1) * 128]
                if kb == nkb - 1:
                    P.op("pe", lambda e: e.matmul(psO, lhsT=ZEROS, rhs=qs, start=True, stop=False),
                         reads=["cstb", "QT"], writes=[f"pb{ob}"])
                    P.op("pool", lambda e: e.memset(spsum_[sm][:], 0.0), writes=[f"spsum{sm}"])
                P.op("pe", lambda e: e.matmul(pb[zb][:, off:512], lhsT=ks, rhs=qs[:, off:512], start=True, stop=not diag),
                     reads=["KT", "QT", "AOs"], writes=[f"pb{zb}"])
                if diag:
                    P.op("pe", lambda e: e.matmul(pb[zb][:, off:off + 128], lhsT=identb[:], rhs=NMSTRICT, start=False, stop=True),
                         reads=["identb", "cstb"], writes=[f"pb{zb}"])
                P.op("act", lambda e: e.activation(out=e_[:, off:512], in_=pb[zb][:, off:512], func=AF.Exp), reads=[f"pb{zb}"], writes=[f"eT{i}"])
                P.op("act", lambda e: e.activation(out=s_[:, off:512], in_=e_[:, off:512], func=AF.Ln, bias=1.0), reads=[f"eT{i}"], writes=[f"spT{n % 3}"])

            def st_b(n):
                c, p, hh, kb, nkb, hc, P0, off, diag, ob, sm = geom(n)
                cb = 2 + n % 2
                s_, a_ = spT3[n % 3], aT3[n % 3]
                qs = QT[:, p, c * 512:(c + 1) * 512]
                ks = (KT if hh == 0 else KTz1)[:, p, kb * 128:(kb + 1) * 128]
                spsum = spsum_[sm]
                P.op("pe", lambda e: e.matmul(pb[cb][:, off:512], lhsT=ks, rhs=qs[:, off:512], start=True, stop=False),
                     reads=["KT", "QT", "AOs"], writes=[f"pb{cb}"])
                if diag:
                    P.op("pe", lambda e: e.matmul(pb[cb][:, off:off + 128], lhsT=identb[:], rhs=NMSTRICT, start=False, stop=False),
                         reads=["identb", "cstb"], writes=[f"pb{cb}"])
                off2 = off + 128 if diag else 0
                last_is_tri = not (kb < nkb - 1 and off2 < 512)
                P.op("pe", lambda e: e.matmul(pb[cb][:, off:512], lhsT=NTRI, rhs=s_[:, off:512], start=False, stop=last_is_tri),
                     reads=["cstb", f"spT{n % 3}"], writes=[f"pb{cb}"])
                if not last_is_tri:
                    P.op("pe", lambda e: e.matmul(pb[cb][:, off2:512], lhsT=NONES, rhs=spsum[:, off2:512], start=False, stop=True),
                         reads=["cstb", f"spsum{sm}"], writes=[f"pb{cb}"])
                P.op("act", lambda e: e.activation(out=a_[:, off:512], in_=pb[cb][:, off:512], func=AF.Exp), reads=[f"pb{cb}"], writes=[f"aT{n % 3}"])
                if kb > 0:
                    P.op("pool", lambda e: e.tensor_tensor(out=spsum[:, off:512], in0=spsum[:, off:512], in1=s_[:, off:512], op=ALU.add),
                         reads=[f"spT{n % 3}", f"spsum{sm}"], writes=[f"spsum{sm}"])

            def st_c(n):
                c, p, hh, kb, nkb, hc, P0, off, diag, ob, sm = geom(n)
                h = 2 * p + hh
                a_ = aT3[n % 3]
                psO = pb[ob][:, :]
                P.op("pe", lambda e: e.matmul(psO[:, off:512], lhsT=Vt[:, kb, p * 128:(p + 1) * 128], rhs=a_[:, off:512], start=False, stop=(kb == 0)),
                     reads=["Vt", f"aT{n % 3}"], writes=[f"pb{ob}"])
                if kb == 0:
                    P.op("dve", lambda e: e.tensor_copy(out=oT_sb[P0:P0 + 64, p, c * 512:(c + 1) * 512], in_=psO[P0:P0 + 64, :]), reads=[f"pb{ob}"], writes=["oT_sb"])

            NI = len(its)
            for n in range(NI + 2):
                if n < NI:
                    st_a(n)
                if 0 <= n - 1 < NI:
                    st_b(n - 1)
                if 0 <= n - 2 < NI:
                    st_c(n - 2)

        memT = A_1b[:, 8192:8192 + 2048].rearrange("p (c t) -> p c t", t=256)
        KTm = A_1b[:, 8192 + 2048:8192 + 3072].rearrange("p (h t) -> p h t", t=256)
        Vm = A_1b[:, 8192 + 3072:8192 + 4096].rearrange("p (m n) -> p m n", n=512)
        wkv_r = wkv_d.rearrange("(c p) n -> p c n", p=128)

        def headnorm_to_T(ps_ap, ps_key, gain_ap, gkey, dstT, dkey, col0, ti):
            tmpf, tk = eT[ti % 2], f"eT{ti % 2}"
            ssap = st2[:, (ti % 2) * 4:(ti % 2) * 4 + 4]
            sk = f"st2m{ti % 2}"
            headnorm_rstd(ps_ap, ps_key, 4, 128, tmpf, tk, ssap, sk)
            P.op("dve", lambda e: e.tensor_tensor(out=tmpf[:, :].rearrange("p (h d) -> p h d", d=128),
                                                  in0=ps_ap.rearrange("p (h d) -> p h d", d=128),
                                                  in1=ssap.unsqueeze(2).to_broadcast([128, 4, 128]), op=ALU.mult),
                 reads=[ps_key, sk], writes=[tk])
            ob, ok = aT[ti % 2], f"aT{ti % 2}"
            P.op("dve", lambda e: e.tensor_tensor(out=ob[:, :].rearrange("p (h d) -> p h d", d=128),
                                                  in0=tmpf[:, :].rearrange("p (h d) -> p h d", d=128),
                                                  in1=gain_ap.unsqueeze(1).to_broadcast([128, 4, 128]), op=ALU.mult),
                 reads=[tk, gkey], writes=[ok])
            tb = 6 + ti % 2
            pT = pb[tb][:, 0:256].bitcast(BF16).rearrange("p (h t) -> p h t", t=128)
            for h in range(4):
                P.op("pe", lambda e, h=h: e.transpose(out=pT[:, h, :], in_=ob[:, h * 128:(h + 1) * 128], identity=identb[:]),
                     reads=[ok, "identb"], writes=[f"pb{tb}"])
            P.op("act", lambda e: e.activation(out=dstT[:, :, col0:col0 + 128], in_=pT, func=AF.Copy), reads=[f"pb{tb}"], writes=[dkey])

        def mem_mixer(s):
            for mt in range(2):
                rmsnorm_T(mem_d[s, mt * 128:(mt + 1) * 128, :], nmem, "nmem", memT, "KT", mt, mt)
            wk, wkk = load_w(wkv_r[:, :, 0:512])
            wv, wvk = load_w(wkv_r[:, :, 512:1024])
            for mt in range(2):
                bk = mt
                for k in range(8):
                    P.op("pe", lambda e, k=k, mt=mt, bk=bk: e.matmul(pb[bk][:, :], lhsT=memT[:, k, mt * 128:(mt + 1) * 128], rhs=wk[:, k, :],
                                                                      start=(k == 0), stop=(k == 7)), reads=["KT", wkk], writes=[f"pb{bk}"])
                headnorm_to_T(pb[bk][:, :], f"pb{bk}", knm[:, :], "knm", KTm, "KT", mt * 128, mt)
                bv = 2 + mt
                for k in range(8):
                    P.op("pe", lambda e, k=k, mt=mt, bv=bv: e.matmul(pb[bv][:, :], lhsT=memT[:, k, mt * 128:(mt + 1) * 128], rhs=wv[:, k, :],
                                                                      start=(k == 0), stop=(k == 7)), reads=["KT", wvk], writes=[f"pb{bv}"])
                P.op("dve", lambda e, mt=mt, bv=bv: e.tensor_copy(out=Vm[:, mt, :], in_=pb[bv][:, :]), reads=[f"pb{bv}"], writes=["KT"])
            wq, wqk = load_w(w_in_r[:, :, OFF_M:OFF_M + 512])
            def mq_mm(j):
                bk = j % 4
                for k in range(8):
                    P.op("pe", lambda e, k=k, j=j, bk=bk: e.matmul(pb[bk][:, :], lhsT=hT[:, k, j * 128:(j + 1) * 128], rhs=wq[:, k, :],
                                                                    start=(k == 0), stop=(k == 7)), reads=["hT", wqk], writes=[f"pb{bk}"])
            mq_mm(0)
            mq_mm(1)
            for j in range(NT):
                if j + 2 < NT:
                    mq_mm(j + 2)
                bk = j % 4
                headnorm_to_T(pb[bk][:, :], f"pb{bk}", qnm[:, :], "qnm", QT, "QT", j * 128, j)
            n = 0
            for c in range(4):
                for h in range(4):
                    nb_, db_ = 4, 5
                    for mb in range(2):
                        sbk = n % 2
                        pm, pk = spT[n % 2], f"spT{n % 2}"
                        n += 1
                        P.op("pe", lambda e, h=h, c=c, mb=mb, sbk=sbk: e.matmul(pb[sbk][:, :], lhsT=KTm[:, h, mb * 128:(mb + 1) * 128],
                                                                                 rhs=QT[:, h, c * 512:(c + 1) * 512], start=True, stop=True),
                             reads=["KT", "QT"], writes=[f"pb{sbk}"])
                        P.op("act", lambda e, sbk=sbk, pm=pm: e.activation(out=pm[:, :], in_=pb[sbk][:, :], func=AF.Exp, scale=128.0 ** -0.5),
                             reads=[f"pb{sbk}"], writes=[pk])
                        P.op("pe", lambda e, h=h, mb=mb, pm=pm: e.matmul(pb[4][:, :], lhsT=Vm[:, mb, h * 128:(h + 1) * 128], rhs=pm[:, :],
                                                                          start=(mb == 0), stop=(mb == 1)), reads=["KT", pk], writes=["pb4"])
                        P.op("pe", lambda e, mb=mb, pm=pm: e.matmul(pb[5][:, :], lhsT=ONESB, rhs=pm[:, :],
                                                                     start=(mb == 0), stop=(mb == 1)), reads=["cstb", pk], writes=["pb5"])
                    rc, rk = eT[(c * 4 + h) % 2], f"eT{(c * 4 + h) % 2}"
                    P.op("dve", lambda e, rc=rc: e.reciprocal(out=rc[:, :], in_=pb[5][:, :]), reads=["pb5"], writes=[rk])
                    P.op("dve", lambda e, rc=rc, h=h, c=c: e.tensor_tensor(out=oT_mem[:, h, c * 512:(c + 1) * 512], in0=pb[4][:, :], in1=rc[:, :], op=ALU.mult),
                         reads=["pb4", rk], writes=["oT_mem"])

        QKTA = A_1b[:, 0:18432].rearrange("p (a t) -> p a t", t=S)
        VA = A_1b[:, 18432:18432 + 6144].rearrange("p (j g n) -> p j g n", g=3, n=128)
        ropeT = AOs_f.rearrange("p (a j d) -> p a j d", a=4, d=64)
        TWO_PI = float(2 * np.pi)

        def rope_tables(s):
            posi = st2[:, 16:32].bitcast(I32)
            posf = st2[:, 32:48]
            P.op("sp", lambda e: e.dma_start(out=posi, in_=pos_d[s]), writes=["posi"], dma_key="pos")
            P.op("dve", lambda e: e.tensor_copy(out=posf, in_=posi), reads=["posi"], writes=["posf"])
            ang = eT[0][:, :].rearrange("p (j d) -> p j d", d=32)
            P.op("dve", lambda e: e.tensor_tensor(out=ang, in0=posf.unsqueeze(2).to_broadcast([128, NT, 32]),
                                                  in1=invf[:, :].unsqueeze(1).to_broadcast([128, NT, 32]), op=ALU.mult),
                 reads=["posf", "invf"], writes=["eT0"])
            red = eT[1][:, :]
            ki = xt[0][:, 0:512].bitcast(I32)
            kf = xt[0][:, 512:1024]
            for which in range(2):
                shift = 0.0 if which == 0 else float(np.pi / 2)
                P.op("dve", lambda e, shift=shift: e.tensor_scalar(out=red, in0=eT[0][:, :], scalar1=shift, scalar2=None, op0=ALU.add),
                     reads=["eT0"], writes=["eT1"])
                P.op("dve", lambda e: e.tensor_scalar(out=ki, in0=red, scalar1=1.0 / TWO_PI, scalar2=None, op0=ALU.mult), reads=["eT1"], writes=["xt0"])
                P.op("dve", lambda e: e.tensor_copy(out=kf, in_=ki), reads=["xt0"], writes=["xt0"])
                P.op("dve", lambda e: e.scalar_tensor_tensor(out=red, in0=kf, scalar=-TWO_PI, in1=red, op0=ALU.mult, op1=ALU.add),
                     reads=["xt0", "eT1"], writes=["eT1"])
                P.op("dve", lambda e: e.tensor_scalar(out=kf, in0=red, scalar1=float(np.pi), scalar2=None, op0=ALU.is_gt), reads=["eT1"], writes=["xt0"])
                P.op("dve", lambda e: e.scalar_tensor_tensor(out=red, in0=kf, scalar=-TWO_PI, in1=red, op0=ALU.mult, op1=ALU.add),
                     reads=["xt0", "eT1"], writes=["eT1"])
                P.op("dve", lambda e: e.tensor_scalar(out=kf, in0=red, scalar1=float(-np.pi), scalar2=None, op0=ALU.is_lt), reads=["eT1"], writes=["xt0"])
                P.op("dve", lambda e: e.scalar_tensor_tensor(out=red, in0=kf, scalar=TWO_PI, in1=red, op0=ALU.mult, op1=ALU.add),
                     reads=["xt0", "eT1"], writes=["eT1"])
                P.op("dve", lambda e: e.tensor_scalar(out=red, in0=red, scalar1=3.1415925, scalar2=-3.1415925, op0=ALU.min, op1=ALU.max),
                     reads=["eT1"], writes=["eT1"])
                trig = xt[1][:, which * 512:(which + 1) * 512]
                P.op("act", lambda e, trig=trig: e.activation(out=trig, in_=red, func=AF.Sin), reads=["eT1"], writes=["xt1"])
            sin3 = xt[1][:, 0:512].rearrange("p (j d) -> p j d", d=32)
            cos3 = xt[1][:, 512:1024].rearrange("p (j d) -> p j d", d=32)
            for qk in range(2):
                Ct = ropeT[:, 2 * qk, :, :].rearrange("p j (u d) -> p j u d", d=32)
                St = ropeT[:, 2 * qk + 1, :, :].rearrange("p j (u d) -> p j u d", d=32)
                gn = qk4[:, 2 * qk, :].rearrange("p (u d) -> p u d", d=32)
                gs = qk4[:, 2 * qk + 1, :].rearrange("p (u d) -> p u d", d=32)
                P.op("dve", lambda e, Ct=Ct, gn=gn: e.tensor_tensor(out=Ct, in0=cos3.unsqueeze(2).to_broadcast([128, NT, 2, 32]),
                                                                  in1=gn.unsqueeze(1).to_broadcast([128, NT, 2, 32]), op=ALU.mult),
                     reads=["xt1", "qk4"], writes=["AOs"])
                P.op("dve", lambda e, St=St, gs=gs: e.tensor_tensor(out=St, in0=sin3.unsqueeze(2).to_broadcast([128, NT, 2, 32]),
                                                                  in1=gs.unsqueeze(1).to_broadcast([128, NT, 2, 32]), op=ALU.mult),
                     reads=["xt1", "qk4"], writes=["AOs"])
                P.op("dve", lambda e, St=St: e.tensor_scalar(out=St[:, :, 0, :], in0=St[:, :, 0, :], scalar1=-1.0, scalar2=None, op0=ALU.mult),
                     reads=["AOs"], writes=["AOs"])

        def dil_mixer(s):
            rope_tables(s)
            P.op("pool", lambda e: e.memset(QKTA[64:128, 3:6, :], 0.0), writes=["QT", "KT", "Vt"])
            P.op("pool", lambda e: e.memset(QKTA[0:64, 6:9, :], 0.0), writes=["QT", "KT", "Vt"])
            ucount = [0]
            for p in range(4):
                wts = []
                for t in range(2):
                    w_, wk_ = load_w([w_in_r[:, :, t * 1536 + g * 512 + p * 128: t * 1536 + g * 512 + (p + 1) * 128] for g in range(3)])
                    wts.append((w_, wk_))
                def pj_mm(j, wts=wts):
                    for t in range(2):
                        w_, wk_ = wts[t]
                        bk = 2 * (j % 3) + t
                        for k in range(8):
                            P.op("pe", lambda e, k=k, j=j, bk=bk, w_=w_: e.matmul(pb[bk][:, 0:384], lhsT=hT[:, k, j * 128:(j + 1) * 128], rhs=w_[:, k, 0:384],
                                                                                   start=(k == 0), stop=(k == 7)), reads=["hT", wk_], writes=[f"pb{bk}"])

                PEND = []
                pj_mm(0)
                pj_mm(1)
                for j in range(NT):
                    if j + 2 < NT:
                        pj_mm(j + 2)
                    ssq = st2[:, 48 + 12 * (j % 2):48 + 12 * (j % 2) + 12]
                    ssk = f"st2a{j % 2}"
                    obuf, obk = (spT[j % 2], f"spT{j % 2}")
                    for t in range(2):
                        bk = 2 * (j % 3) + t
                        psv = pb[bk][:, 0:384]
                        tf, tfk = eT[t], f"eT{t}"
                        headnorm_rstd(psv, f"pb{bk}", 6, 64, tf, tfk, ssq[:, 6 * t:6 * t + 6], ssk + str(t))
                        Cj = ropeT[:, 2 * t, j, :]
                        Sj = ropeT[:, 2 * t + 1, j, :]
                        ps3 = psv.rearrange("p (a d) -> p a d", d=64)
                        tA = xt[t][:, 0:384].rearrange("p (a d) -> p a d", d=64)
                        tB = xt[t][:, 384:768].rearrange("p (a d) -> p a d", d=64)
                        xk = f"xt{t}"
                        P.op("dve", lambda e, tA=tA, ps3=ps3, Cj=Cj: e.tensor_tensor(out=tA, in0=ps3, in1=Cj.unsqueeze(1).to_broadcast([128, 6, 64]), op=ALU.mult),
                             reads=[f"pb{bk}", "AOs"], writes=[xk])
                        P.op("dve", lambda e, tB=tB, ps3=ps3, Sj=Sj: e.tensor_tensor(out=tB[:, :, 0:32], in0=ps3[:, :, 32:64],
                                                                                      in1=Sj[:, 0:32].unsqueeze(1).to_broadcast([128, 6, 32]), op=ALU.mult),
                             reads=[f"pb{bk}", "AOs"], writes=[xk])
                        P.op("dve", lambda e, tB=tB, ps3=ps3, Sj=Sj: e.tensor_tensor(out=tB[:, :, 32:64], in0=ps3[:, :, 0:32],
                                                                                      in1=Sj[:, 32:64].unsqueeze(1).to_broadcast([128, 6, 32]), op=ALU.mult),
                             reads=[f"pb{bk}", "AOs"], writes=[xk])
                        P.op("pool", lambda e, tA=tA, tB=tB: e.tensor_tensor(out=tA, in0=tA, in1=tB, op=ALU.add), reads=[xk], writes=[xk])
                        rs_ = ssq[:, 6 * t:6 * t + 6]
                        dstb = obuf if t == 0 else aT[j % 2]
                        dstk = obk if t == 0 else f"aT{j % 2}"
                        P.op("pool", lambda e, tA=tA, rs_=rs_, dstb=dstb: e.tensor_tensor(
                            out=dstb[:, 0:384].rearrange("p (a d) -> p a d", d=64),
                            in0=tA, in1=rs_.unsqueeze(2).to_broadcast([128, 6, 64]), op=ALU.mult),
                            reads=[xk, ssk + str(t)], writes=[dstk])
                    tb = 6 + j % 2
                    pT = pb[tb][:, 0:384].bitcast(BF16).rearrange("p (a t) -> p a t", t=128)
                    for t in range(2):
                        srcb = obuf if t == 0 else aT[j % 2]
                        srck = obk if t == 0 else f"aT{j % 2}"
                        for g in range(3):
                            P.op("pe", lambda e, t=t, g=g, srcb=srcb, pT=pT: e.transpose(out=pT[:, 3 * t + g, :], in_=srcb[:, g * 128:(g + 1) * 128], identity=identb[:]),
                                 reads=[srck, "identb"], writes=[f"pb{tb}"])
                    def evac(j=j, pT=pT, tb=tb):
                        P.op("act", lambda e: e.activation(out=QKTA[:, 0:3, j * 128:(j + 1) * 128], in_=pT[:, 0:3, :], func=AF.Copy),
                             reads=[f"pb{tb}"], writes=["QT", "KT", "Vt"])
                        P.op("act", lambda e: e.activation(out=QKTA[0:64, 3:6, j * 128:(j + 1) * 128], in_=pT[0:64, 3:6, :], func=AF.Copy),
                             reads=[f"pb{tb}"], writes=["QT", "KT", "Vt"])
                        P.op("act", lambda e: e.activation(out=QKTA[64:128, 6:9, j * 128:(j + 1) * 128], in_=pT[64:128, 3:6, :], func=AF.Copy),
                             reads=[f"pb{tb}"], writes=["QT", "KT", "Vt"])
                    if PEND:
                        PEND.pop(0)()
                    PEND.append(evac)
                while PEND:
                    PEND.pop(0)()
                w_, wk_ = load_w([w_in_r[:, :, 2 * 1536 + g * 512 + p * 128: 2 * 1536 + g * 512 + (p + 1) * 128] for g in range(3)])
                for j in range(NT):
                    bk = 4 + j % 2
                    for k in range(8):
                        P.op("pe", lambda e, k=k, j=j, bk=bk, w_=w_: e.matmul(pb[bk][:, 0:384], lhsT=hT[:, k, j * 128:(j + 1) * 128], rhs=w_[:, k, 0:384],
                                                                               start=(k == 0), stop=(k == 7)), reads=["hT", wk_], writes=[f"pb{bk}"])
                    P.op("act", lambda e, j=j, bk=bk: e.activation(out=VA[:, j, :, :], in_=pb[bk][:, 0:384].rearrange("p (g n) -> p g n", g=3),
                                                                   func=AF.Copy), reads=[f"pb{bk}"], writes=["QT", "KT", "Vt"])
                units_all = []
                for c in range(4):
                    for hh in range(2):
                        ul = []
                        for jj in range(4 * c - 1, 4 * c + 4):
                            if jj < 0:
                                continue
                            t0_, t1_ = max(jj, 4 * c), min(jj + 1, 4 * c + 3)
                            ul.append((0, jj, t0_ - 4 * c, t1_ - 4 * c + 1, 0 + (t0_ - jj)))
                        for jj in range(4 * c - 4, 4 * c + 4):
                            if jj < 0:
                                continue
                            t0_, t1_ = max(jj, 4 * c), min(jj + 4, 4 * c + 3)
                            ul.append((1, jj, t0_ - 4 * c, t1_ - 4 * c + 1, 2 + (t0_ - jj)))
                        for jj in range(0, 4 * c + 4):
                            t0_ = max(jj, 4 * c)
                            ul.append((2, jj, t0_ - 4 * c, 4, 7 if t0_ == jj else 8))
                        for ui, u in enumerate(ul):
                            units_all.append((c, hh, ui, len(ul), u))
                NB = 4 + (p % 2) * 0
                PMB = [hm[i_][:, k_, :] for i_ in range(2) for k_ in range(4)]
                SBK = [0, 1, 2, 7]

                def stage_a(n):
                    c, hh, ui, nu, (g, jj, ta, tb_, mi) = units_all[n]
                    P0 = 64 * hh
                    lo, hi = ta * 128, tb_ * 128
                    sbk = SBK[n % 4]
                    pm, pk = PMB[n % 8], f"hmA{n % 8}"
                    P.op("pe", lambda e: e.matmul(pb[sbk][:, lo:hi], lhsT=QKTA[:, 3 + 3 * hh + g, jj * 128:(jj + 1) * 128],
                                                  rhs=QKTA[:, g, c * 512 + lo:c * 512 + hi], start=True, stop=True),
                         reads=["QT", "KT", "Vt"], writes=[f"pb{sbk}"])
                    P.op("act", lambda e: e.activation(out=pm[:, lo:hi], in_=pb[sbk][:, lo:hi], func=AF.Exp, scale=0.125),
                         reads=[f"pb{sbk}"], writes=[pk])
                    nt_ = tb_ - ta
                    P.op("dve", lambda e: e.tensor_tensor(out=pm[:, lo:hi].rearrange("p (t q) -> p t q", q=128),
                                                          in0=pm[:, lo:hi].rearrange("p (t q) -> p t q", q=128),
                                                          in1=maskb[:, mi:mi + nt_, :], op=ALU.mult),
                         reads=[pk, "maskb"], writes=[pk])

                def stage_b(n, pp=p):
                    c, hh, ui, nu, (g, jj, ta, tb_, mi) = units_all[n]
                    P0 = 64 * hh
                    lo, hi = ta * 128, tb_ * 128
                    pm, pk = PMB[n % 8], f"hmA{n % 8}"
                    hcn = 2 * c + hh
                    nbk, dbk = 3 + 2 * (hcn % 2), 4 + 2 * (hcn % 2)
                    if ui == 0:
                        for bk_ in (nbk, dbk):
                            P.op("pe", lambda e, bk_=bk_: e.matmul(pb[bk_][:, :], lhsT=ZEROS, rhs=QKTA[:, 0, 0:512],
                                                                   start=True, stop=False), reads=["cstb", "QT"], writes=[f"pb{bk_}"])
                    last = (ui == nu - 1)
                    P.op("pe", lambda e: e.matmul(pb[nbk][:, lo:hi], lhsT=VA[:, jj, g, :], rhs=pm[:, lo:hi], start=False, stop=last),
                         reads=[pk, "QT", "KT", "Vt"], writes=[f"pb{nbk}"])
                    P.op("pe", lambda e: e.matmul(pb[dbk][:, lo:hi], lhsT=ONESB, rhs=pm[:, lo:hi], start=False, stop=last),
                         reads=[pk, "cstb"], writes=[f"pb{dbk}"])
                    if last:
                        rc, rk = eT[hcn % 2], f"eT{hcn % 2}"
                        P.op("act", lambda e: e.activation(out=rc[P0:P0 + 64, :], in_=pb[dbk][P0:P0 + 64, :], func=AF.Ln), reads=[f"pb{dbk}"], writes=[rk])
                        P.op("act", lambda e: e.activation(out=rc[P0:P0 + 64, :], in_=rc[P0:P0 + 64, :], func=AF.Exp, scale=-1.0), reads=[rk], writes=[rk])
                        P.op("dve", lambda e: e.tensor_tensor(out=oT_dil[P0:P0 + 64, pp, c * 512:(c + 1) * 512], in0=pb[nbk][P0:P0 + 64, :],
                                                              in1=rc[P0:P0 + 64, :], op=ALU.mult),
                             reads=[f"pb{nbk}", rk], writes=["oT_dil"])

                NU = len(units_all)
                LAG = 3
                for n in range(NU + LAG):
                    if n < NU:
                        stage_a(n)
                    if n - LAG >= 0:
                        stage_b(n - LAG)

        mergedT = A_1b[:, 0:16384].rearrange("p (c t) -> p c t", t=S)
        wo_dil = A_1b[:, 16384:16384 + 4096].rearrange("p (k n) -> p k n", n=D)
        wo_sbm = AOs_b.rearrange("p (b k n) -> p b k n", b=2, n=D)
        wout_b = AOs_b.rearrange("p (k n) -> p k n", n=D)
        wo_r = wo_d.rearrange("b (k p) n -> p b k n", p=128)
        oTs = [oT_sb, oT_mem, oT_dil]
        oTk = ["oT_sb", "oT_mem", "oT_dil"]
        GOFF = [OFF_G + 1 * D, OFF_G + 2 * D, OFF_G + 0 * D]
        GB = [1, 2, 0]

        def merge_phase(s):
            P.op("pool", lambda e: e.dma_start(out=wo_sbm[:, 0], in_=wo_r[:, 0]), writes=["AOs"], dma_key="wo0")
            P.op("pool", lambda e: e.dma_start(out=wo_sbm[:, 1], in_=wo_r[:, 1]), writes=["AOs"], dma_key="wo0")
            P.op("pool", lambda e: e.dma_start(out=wo_dil, in_=wo_r[:, 2]), writes=["Vt"], dma_key="wo1")
            n = 0
            for f in range(8):
                wgt, wgk = load_w([w_in_r[:, :, GOFF[b] + f * 128: GOFF[b] + (f + 1) * 128] for b in range(3)])
                for c in range(4):
                    mt_ = xt[0][:, 0:512]
                    tt_ = xt[0][:, 512:1024]
                    for b in range(3):
                        gbk, bbk = n % 2, 2 + n % 2
                        gs_, gsk = eT[n % 2], f"eT{n % 2}"
                        n += 1
                        for k in range(8):
                            P.op("pe", lambda e, k=k, b=b, c=c, gbk=gbk, wgt=wgt: e.matmul(pb[gbk][:, :], lhsT=wgt[:, k, b * 128:(b + 1) * 128],
                                                                                          rhs=hT[:, k, c * 512:(c + 1) * 512], start=(k == 0), stop=(k == 7)),
                                 reads=[wgk, "hT"], writes=[f"pb{gbk}"])
                        bias_ap = bgate[:, GB[b] * 8 + f: GB[b] * 8 + f + 1]
                        P.op("act", lambda e, gbk=gbk, gs_=gs_, bias_ap=bias_ap: e.activation(out=gs_[:, :], in_=pb[gbk][:, :], func=AF.Sigmoid, bias=bias_ap),
                             reads=[f"pb{gbk}", "bgate"], writes=[gsk])
                        wsrc = wo_sbm[:, b] if b < 2 else wo_dil
                        wkey = "AOs" if b < 2 else "Vt"
                        for k in range(4):
                            P.op("pe", lambda e, k=k, b=b, c=c, f=f, bbk=bbk, wsrc=wsrc: e.matmul(pb[bbk][:, :], lhsT=wsrc[:, k, f * 128:(f + 1) * 128],
                                                                                                 rhs=oTs[b][:, k, c * 512:(c + 1) * 512], start=(k == 0), stop=(k == 3)),
                                 reads=[wkey, oTk[b]], writes=[f"pb{bbk}"])
                        if b == 0:
                            P.op("dve", lambda e, gs_=gs_, bbk=bbk: e.tensor_tensor(out=mt_, in0=pb[bbk][:, :], in1=gs_[:, :], op=ALU.mult),
                                 reads=[f"pb{bbk}", gsk], writes=["xt0"])
                        else:
                            P.op("dve", lambda e, gs_=gs_, bbk=bbk: e.tensor_tensor(out=tt_, in0=pb[bbk][:, :], in1=gs_[:, :], op=ALU.mult),
                                 reads=[f"pb{bbk}", gsk], writes=["xt0"])
                            dst = mt_ if b == 1 else mergedT[:, f, c * 512:(c + 1) * 512]
                            P.op("dve", lambda e, dst=dst: e.tensor_tensor(out=dst, in0=mt_, in1=tt_, op=ALU.add),
                                 reads=["xt0"], writes=["xt0"] if b == 1 else ["QT", "KT"])
            wout_r = wout_d.rearrange("(k p) n -> p k n", p=128)
            P.op("pool", lambda e: e.dma_start(out=wout_b[:, 0:4], in_=wout_r[:, 0:4]), writes=["AOs"], dma_key="wo0")
            P.op("pool", lambda e: e.dma_start(out=wout_b[:, 4:8], in_=wout_r[:, 4:8]), writes=["AOs"], dma_key="wo0")
            def p5_a(j):
                b = j % 2
                P.op("sp", lambda e: e.dma_start(out=xt[b][:], in_=x_d[s, j * 128:(j + 1) * 128, :]), writes=[f"xt{b}"], dma_key=f"x{b}")
                for hf in range(2):
                    bk = 2 * b + hf
                    for k in range(8):
                        P.op("pe", lambda e, k=k, hf=hf, bk=bk: e.matmul(pb[bk][:, :], lhsT=mergedT[:, k, j * 128:(j + 1) * 128],
                                                                          rhs=wout_b[:, k, hf * 512:(hf + 1) * 512], start=(k == 0), stop=(k == 7)),
                             reads=["QT", "KT", "AOs"], writes=[f"pb{bk}"])
                    P.op("dve", lambda e, hf=hf, bk=bk: e.tensor_tensor(out=xt[b][:, hf * 512:(hf + 1) * 512], in0=pb[bk][:, :],
                                                                         in1=xt[b][:, hf * 512:(hf + 1) * 512], op=ALU.add),
                         reads=[f"pb{bk}", f"xt{b}"], writes=[f"xt{b}"])
                P.op("pool", lambda e: e.dma_start(out=out_d[s, j * 128:(j + 1) * 128, :], in_=xt[b][:]), reads=[f"xt{b}"], writes=[f"outd_{s}_{j}"],
                     dma_key=f"x2st{b}")
                ss = stat[:, b:b + 1]
                rs = stat[:, 2 + b:3 + b]
                P.op("act", lambda e: e.activation(out=hm[1][:, 2 * b:2 * b + 2, :].rearrange("p a c -> p (a c)"), in_=xt[b][:], func=AF.Square, accum_out=ss),
                     reads=[f"xt{b}"], writes=[f"hmk{b}", f"ss{b}"])
                P.op("act", lambda e: e.activation(out=rs, in_=ss, func=AF.Ln, scale=1.0 / D, bias=EPS), reads=[f"ss{b}"], writes=[f"rs{b}"])
                P.op("act", lambda e: e.activation(out=rs, in_=rs, func=AF.Exp, scale=-0.5), reads=[f"rs{b}"], writes=[f"rs{b}"])

                def tail():
                    P.op("dve", lambda e: e.tensor_scalar(out=xn[b][:], in0=xt[b][:], scalar1=rs, scalar2=None, op0=ALU.mult),
                         reads=[f"xt{b}", f"rs{b}"], writes=[f"xn{b}"])
                    P.op("pool", lambda e: e.dma_start(out=XN_d[s * S + j * 128: s * S + (j + 1) * 128, :], in_=xn[b][:]), reads=[f"xn{b}"], writes=[f"XNd_{s}_{j}"],
                         dma_key=f"xnst{b}")
                return tail

            def p5_b(j):
                b = j % 2
                pT = pb[6 + b][:, 0:512].bitcast(BF16).rearrange("p (c t) -> p c t", t=128)
                for c in range(8):
                    P.op("pe", lambda e, c=c: e.transpose(out=pT[:, c, :], in_=xn[b][:, c * 128:(c + 1) * 128], identity=identb[:]),
                         reads=[f"xn{b}", "identb"], writes=[f"pb{6 + b}"])
                P.op("dve", lambda e: e.tensor_tensor(out=hT[:, :, j * 128:(j + 1) * 128], in0=pT,
                                                      in1=nffn[:].unsqueeze(2).to_broadcast([128, 8, 128]), op=ALU.mult),
                     reads=[f"pb{6 + b}", "nffn"], writes=["hT"])

            def p5_c(j):
                b = j % 2
                for k in range(8):
                    P.op("pe", lambda e, k=k: e.matmul(pb[4 + b][:, 0:20], lhsT=hT[:, k, j * 128:(j + 1) * 128], rhs=wrb[:, k, :],
                                                       start=(k == 0), stop=(k == 7)), reads=["hT", "wrb"], writes=[f"pb{4 + b}"])
                P.op("dve", lambda e: e.tensor_tensor(out=Lall[:, s * NT + j, :], in0=pb[4 + b][:, 0:20], in1=brt[:, :], op=ALU.add),
                     reads=[f"pb{4 + b}", "brt"], writes=["Lall"])

            for n in range(NT + 2):
                tl = p5_a(n) if n < NT else None
                if 0 <= n - 1 < NT:
                    p5_b(n - 1)
                if 0 <= n - 2 < NT:
                    p5_c(n - 2)
                if tl is not None:
                    tl()

        BREG = {}

        def breg(e, val):
            if val not in BREG:
                BREG[val] = e.to_reg(val)
            return BREG[val]

        def moe_sparse():
            NTT = NSEQ * NT
            P.barrier()
            R_ = A_1[:, :]
            rk = "A1"
            o = [0]

            def T(n, m=NTT):
                v = R_[:, o[0]:o[0] + m * n].rearrange("p (t n) -> p t n", n=n)
                o[0] += m * n
                return v
            G = Lall[:, :, 0:4]
            E = Lall[:, :, 4:20].rearrange("p t (g e) -> p t g e", e=4)
            gmax, ohg, gex, gsum, gtop = T(1), T(4), T(4), T(1), T(1)
            prod, esel, m1, oh1, esel2, m2, oh2 = T(16), T(4), T(1), T(4), T(4), T(1), T(4)
            dd, ed, den = T(1), T(1), T(1)
            selA, selB, sel, cntS, offs, slot, tmp16 = T(16), T(16), T(16), T(16), T(16), T(16), T(16)
            ne, q_, kf, gt_, pc, base = T(1, 16), T(1, 16), T(1, 16), T(1, 16), T(1, 16), T(1, 16)
            ki = T(1, 16).bitcast(I32)
            cmpE = T(16, NTILE)
            Et, neq, idxf = T(1, NTILE), T(1, NTILE), T(1, NTILE)
            selbf = T(8).bitcast(BF16)
            w1 = rsm[:, 0:32].unsqueeze(2)
            w2 = rsm[:, 32:64].unsqueeze(2)
            slotA_f = rsm[:, 64:96]
            slotB_f = rsm[:, 96:128]
            slotA_i = sli[:, 0:32]
            slotB_i = sli[:, 32:64]
            idxW_i = idxw[:, :]

            def V(fn, reads=("Lall",), eng="dve"):
                P.op(eng, fn, reads=list(reads) + [rk, "rsm"], writes=[rk, "rsm"])

            def bc(v, n):
                return v.to_broadcast([128, NTT, n])
            V(lambda e: e.tensor_reduce(out=gmax[:, :, 0], in_=G, axis=AX.X, op=ALU.max))
            V(lambda e: e.tensor_tensor(out=ohg, in0=G, in1=bc(gmax, 4), op=ALU.is_equal))
            V(lambda e: e.tensor_tensor(out=gex, in0=G, in1=bc(gmax, 4), op=ALU.subtract))
            V(lambda e: e.activation(out=gex, in_=gex, func=AF.Exp), eng="act")
            V(lambda e: e.tensor_reduce(out=gsum[:, :, 0], in_=gex, axis=AX.X, op=ALU.add))
            V(lambda e: e.reciprocal(out=gtop, in_=gsum))
            prod4 = prod.rearrange("p t (g e) -> p t g e", e=4)
            V(lambda e: e.tensor_tensor(out=prod4, in0=E, in1=ohg.unsqueeze(3).to_broadcast([128, NTT, 4, 4]), op=ALU.mult))
            V(lambda e: e.tensor_reduce(out=esel, in_=prod.rearrange("p t (g e) -> p t e g", e=4), axis=AX.X, op=ALU.add))
            V(lambda e: e.tensor_reduce(out=m1[:, :, 0], in_=esel, axis=AX.X, op=ALU.max))
            V(lambda e: e.tensor_tensor(out=oh1, in0=esel, in1=bc(m1, 4), op=ALU.is_equal))
            V(lambda e: e.scalar_tensor_tensor(out=esel2, in0=oh1, scalar=NEG, in1=esel, op0=ALU.mult, op1=ALU.add))
            V(lambda e: e.tensor_reduce(out=m2[:, :, 0], in_=esel2, axis=AX.X, op=ALU.max))
            V(lambda e: e.tensor_tensor(out=oh2, in0=esel2, in1=bc(m2, 4), op=ALU.is_equal))
            V(lambda e: e.tensor_tensor(out=dd, in0=m2, in1=m1, op=ALU.subtract))
            V(lambda e: e.activation(out=ed, in_=dd, func=AF.Exp), eng="act")
            V(lambda e: e.tensor_scalar(out=den, in0=ed, scalar1=1.0, scalar2=None, op0=ALU.add))
            V(lambda e: e.reciprocal(out=den, in_=den))
            V(lambda e: e.tensor_tensor(out=w1, in0=gtop, in1=den, op=ALU.mult))
            V(lambda e: e.tensor_tensor(out=w2, in0=w1, in1=ed, op=ALU.mult))
            for sl, oh in ((selA, oh1), (selB, oh2)):
                V(lambda e, sl=sl, oh=oh: e.tensor_tensor(out=sl.rearrange("p t (g e) -> p t g e", e=4),
                                                         in0=ohg.unsqueeze(3).to_broadcast([128, NTT, 4, 4]),
                                                         in1=oh.unsqueeze(2).to_broadcast([128, NTT, 4, 4]), op=ALU.mult))
            V(lambda e: e.tensor_tensor(out=sel, in0=selA, in1=selB, op=ALU.add))
            V(lambda e: e.tensor_copy(out=selbf, in_=sel))
            selbf2 = selbf.rearrange("p t e -> p (t e)")
            P.op("pe", lambda e: e.matmul(pb[0][:, :], lhsT=ONESB, rhs=selbf2, start=True, stop=True), reads=[rk, "cstb"], writes=["pb0"])
            P.op("pe", lambda e: e.matmul(pb[1][:, :], lhsT=cstb[:, 5, :], rhs=selbf2, start=True, stop=True), reads=[rk, "cstb"], writes=["pb1"])
            V(lambda e: e.tensor_copy(out=cntS, in_=pb[0][:, :].rearrange("p (t e) -> p t e", e=16)), reads=("pb0",))
            V(lambda e: e.memset(offs[:, 0, :], 0.0))
            for j in range(1, NTT):
                V(lambda e, j=j: e.tensor_tensor(out=offs[:, j, :], in0=offs[:, j - 1, :], in1=cntS[:, j - 1, :], op=ALU.add))
            ne2, q2, kf2, gt2, pc2, base2 = [v[:, :, 0] for v in (ne, q_, kf, gt_, pc, base)]
            ki2 = ki[:, :, 0]
            V(lambda e: e.tensor_tensor(out=ne2, in0=offs[:, NTT - 1, :], in1=cntS[:, NTT - 1, :], op=ALU.add))
            V(lambda e: e.tensor_scalar(out=q2, in0=ne2, scalar1=127.0, scalar2=1.0 / 128, op0=ALU.add, op1=ALU.mult))
            V(lambda e: e.tensor_copy(out=ki2, in_=q2))
            V(lambda e: e.tensor_copy(out=kf2, in_=ki2))
            V(lambda e: e.tensor_tensor(out=gt2, in0=kf2, in1=q2, op=ALU.is_gt))
            V(lambda e: e.tensor_tensor(out=kf2, in0=kf2, in1=gt2, op=ALU.subtract))
            V(lambda e: e.tensor_scalar(out=pc2, in0=kf2, scalar1=128.0, scalar2=None, op0=ALU.mult))
            V(lambda e: e.memset(base2[:, 0:1], 0.0))
            for ex in range(1, 16):
                V(lambda e, ex=ex: e.tensor_tensor(out=base2[:, ex:ex + 1], in0=base2[:, ex - 1:ex], in1=pc2[:, ex - 1:ex], op=ALU.add))
            V(lambda e: e.tensor_tensor(out=slot, in0=pb[1][:, :].rearrange("p (t e) -> p t e", e=16), in1=offs, op=ALU.add), reads=("pb1",))
            V(lambda e: e.tensor_tensor(out=slot, in0=slot, in1=base2.unsqueeze(1).to_broadcast([128, NTT, 16]), op=ALU.add))
            for sl, dstf, dsti in ((selA, slotA_f, slotA_i), (selB, slotB_f, slotB_i)):
                V(lambda e, sl=sl: e.tensor_tensor(out=tmp16, in0=sl, in1=slot, op=ALU.mult))
                V(lambda e, dstf=dstf: e.tensor_reduce(out=dstf, in_=tmp16, axis=AX.X, op=ALU.add))
                V(lambda e, dstf=dstf, dsti=dsti: e.tensor_copy(out=dsti, in_=dstf))
            V(lambda e: e.tensor_tensor(out=cmpE, in0=base2.unsqueeze(1).to_broadcast([128, NTILE, 16]),
                                        in1=t128[:, :].unsqueeze(2).to_broadcast([128, NTILE, 16]), op=ALU.is_le), reads=("t128",))
            Et2, neq2, idxf2 = Et[:, :, 0], neq[:, :, 0], idxf[:, :, 0]
            V(lambda e: e.tensor_reduce(out=Et2, in_=cmpE, axis=AX.X, op=ALU.add))
            V(lambda e: e.memset(neq2[:, 0:2], 1.0))
            V(lambda e: e.tensor_tensor(out=neq2[:, 2:NTILE], in0=Et2[:, 2:NTILE], in1=Et2[:, 0:NTILE - 2], op=ALU.not_equal))
            V(lambda e: e.tensor_scalar(out=idxf2, in0=Et2, scalar1=128.0, scalar2=-(128.0 + BIGIDX), op0=ALU.mult, op1=ALU.add))
            V(lambda e: e.tensor_scalar(out=idxf2, in0=idxf2, scalar1=piota[:, 0:1], scalar2=None, op0=ALU.add), reads=("piota",))
            V(lambda e: e.tensor_tensor(out=idxf2, in0=idxf2, in1=neq2, op=ALU.mult))
            V(lambda e: e.tensor_scalar(out=idxf2, in0=idxf2, scalar1=BIGIDX, scalar2=None, op0=ALU.add))
            P.op("dve", lambda e: e.tensor_copy(out=idxW_i, in_=idxf2), reads=[rk], writes=["st2i"])
            P.barrier()
            WB = [A_1b[:, i * 12288:(i + 1) * 12288] for i in range(2)]
            for t in range(2):
                for m in range(3):
                    P.op("pool", lambda e, m=m, t=t: e.indirect_dma_start(
                        out=WB[t][:, m * 4096:(m + 1) * 4096], out_offset=None, in_=WBF_d[m][:, :], in_offset=bass.IndirectOffsetOnAxis(ap=idxW_i[:, t:t + 1], axis=0),
                        bounds_check=breg(e, 2047), oob_is_err=False), reads=["st2i"], writes=[f"wb{t}{m}"], dma_key=f"wg{t}{m}")
            xsb = [xn[0][:, :], xn[1][:, :], xt[0][:, :].bitcast(BF16)[:, 0:1024], xt[1][:, :].bitcast(BF16)[:, 0:1024]]
            xsk = ["xn0", "xn1", "xt0", "xt1"]
            for jt in range(NTT):
                b = jt % 4
                P.op("sp", lambda e, jt=jt, b=b: e.dma_start(out=xsb[b], in_=XN_d[jt * 128:(jt + 1) * 128, :]), reads=["XNd"], writes=[xsk[b]], dma_key=f"xnl{b}")
                for si, sl_i in enumerate((slotA_i, slotB_i)):
                    P.op("pool", lambda e, jt=jt, b=b, sl_i=sl_i: e.indirect_dma_start(
                        out=XS_d[:, :], out_offset=bass.IndirectOffsetOnAxis(ap=sl_i[:, jt:jt + 1], axis=0), in_=xsb[b], in_offset=None,
                        bounds_check=breg(e, NSLOT - 1), oob_is_err=False), reads=[xsk[b], "rsm"], writes=[f"XSd_{jt}_{si}"], dma_key=f"xsc{b}{si}")
            P.barrier()
            WB = [A_1b[:, i * 12288:(i + 1) * 12288] for i in range(2)]
            xsT = [hm[i][:, 0:2, :].rearrange("p a (b c) -> p (a b) c", c=128) for i in range(2)]
            hTb = [aT[i][:, :].rearrange("p (a c) -> p a c", c=128) for i in range(2)]

            def et_wload(t, ms):
                b = t % 2
                for m in ms:
                    P.op("pool", lambda e, m=m: e.indirect_dma_start(
                        out=WB[b][:, m * 4096:(m + 1) * 4096], out_offset=None, in_=WBF_d[m][:, :], in_offset=bass.IndirectOffsetOnAxis(ap=idxW_i[:, t:t + 1], axis=0),
                        bounds_check=breg(e, 2047), oob_is_err=False), reads=["st2i"], writes=[f"wb{b}{m}"], dma_key=f"wg{b}{m}")

            def et_xload(t):
                b = t % 2
                P.op("sp", lambda e: e.dma_start(out=xn[b][:], in_=XS_d[t * 128:(t + 1) * 128, :]), writes=[f"xn{b}"], dma_key=f"xsl{b}")

            SG = [spT[0], spT[1]]
            HB = [spx, aTx]

            def et_T(t):
                b = t % 2
                pT = pb[6 + b][:, 0:512].bitcast(BF16).rearrange("p (c t) -> p c t", t=128)
                for c in range(8):
                    P.op("pe", lambda e, c=c: e.transpose(out=pT[:, c, :], in_=xn[b][:, c * 128:(c + 1) * 128], identity=identb[:]),
                         reads=[f"xn{b}", "identb"], writes=[f"pb{6 + b}"])
                P.op("dve", lambda e: e.tensor_tensor(out=xsT[b], in0=pT, in1=nffn[:].unsqueeze(2).to_broadcast([128, 8, 128]), op=ALU.mult),
                     reads=[f"pb{6 + b}", "nffn"], writes=[f"xsT{b}"])

            def et_GU(t):
                b = t % 2
                wgb = WB[b][:, 0:4096].rearrange("p (k n) -> p k n", n=512)
                wub = WB[b][:, 4096:8192].rearrange("p (k n) -> p k n", n=512)
                for k in range(8):
                    P.op("pe", lambda e, k=k: e.matmul(pb[0][:, :], lhsT=xsT[b][:, k, :], rhs=wgb[:, k, :], start=(k == 0), stop=(k == 7)),
                         reads=[f"xsT{b}", f"wb{b}0"], writes=["pb0"])
                for k in range(8):
                    P.op("pe", lambda e, k=k: e.matmul(pb[1][:, :], lhsT=xsT[b][:, k, :], rhs=wub[:, k, :], start=(k == 0), stop=(k == 7)),
                         reads=[f"xsT{b}", f"wb{b}1"], writes=["pb1"])
                P.op("act", lambda e: e.activation(out=SG[b][:, :], in_=pb[0][:, :], func=AF.Silu), reads=["pb0"], writes=[f"etsg{b}"])
                P.op("dve", lambda e: e.tensor_tensor(out=HB[b][:, :], in0=pb[1][:, :], in1=SG[b][:, :], op=ALU.mult), reads=["pb1", f"etsg{b}"], writes=[f"ethb{b}"])

            def et_s2(t):
                b = t % 2
                wdb = WB[b][:, 8192:12288].rearrange("p (k n) -> p k n", n=D)
                pH = pb[2][:, 0:256].bitcast(BF16).rearrange("p (c t) -> p c t", t=128)
                for c in range(4):
                    P.op("pe", lambda e, c=c: e.transpose(out=pH[:, c, :], in_=HB[b][:, c * 128:(c + 1) * 128], identity=identb[:]),
                         reads=[f"ethb{b}", "identb"], writes=["pb2"])
                P.op("act", lambda e: e.activation(out=hTb[b], in_=pH, func=AF.Copy), reads=["pb2"], writes=[f"hTb{b}"])
                for hf in range(2):
                    yb = 4 + hf
                    for hc in range(4):
                        P.op("pe", lambda e, hc=hc, hf=hf, yb=yb: e.matmul(pb[yb][:, :], lhsT=hTb[b][:, hc, :], rhs=wdb[:, hc, hf * 512:(hf + 1) * 512],
                                                                           start=(hc == 0), stop=(hc == 3)), reads=[f"hTb{b}", f"wb{b}2"], writes=[f"pb{yb}"])
                    if hf == 0:
                        P.op("act", lambda e: e.activation(out=xt[b][:, 0:512], in_=pb[4][:, :], func=AF.Copy), reads=["pb4"], writes=[f"xt{b}"])
                    else:
                        P.op("dve", lambda e: e.tensor_copy(out=xt[b][:, 512:1024], in_=pb[5][:, :]), reads=["pb5"], writes=[f"xt{b}"])
                P.op("sp", lambda e: e.dma_start(out=YS_d[t * 128:(t + 1) * 128, :], in_=xt[b][:]), reads=[f"xt{b}"], writes=[f"YSd_{t}"], dma_key=f"yst{b}")

            et_xload(0)
            et_xload(1)
            for n in range(NTILE + 3):
                if n < NTILE:
                    et_T(n)
                if 0 <= n - 3 < NTILE:
                    et_s2(n - 3)
                if 2 <= n - 1 < NTILE:
                    et_wload(n - 1, (2,))
                if 0 <= n - 1 < NTILE:
                    et_GU(n - 1)
                if 2 <= n + 1 < NTILE:
                    et_wload(n + 1, (0, 1))
                if 2 <= n + 2 < NTILE:
                    et_xload(n + 2)
            P.barrier()
            yAB = [[A_O[:, (2 * i + k) * 1024:(2 * i + k + 1) * 1024] for k in range(2)] for i in range(2)]
            x2b = [A_O[:, (4 + i) * 1024:(5 + i) * 1024] for i in range(2)]
            for jt in range(NTT):
                b = jt % 2
                s_, j_ = jt // NT, jt % NT
                rows = out_d[s_, j_ * 128:(j_ + 1) * 128, :]
                P.op("sp", lambda e, rows=rows, b=b: e.dma_start(out=x2b[b], in_=rows), reads=["outd"], writes=[f"x2b{b}"], dma_key=f"x2l{b}")
                for k, sl_i in enumerate((slotA_i, slotB_i)):
                    P.op("pool", lambda e, jt=jt, b=b, k=k, sl_i=sl_i: e.indirect_dma_start(
                        out=yAB[b][k], out_offset=None, in_=YS_d[:, :], in_offset=bass.IndirectOffsetOnAxis(ap=sl_i[:, jt:jt + 1], axis=0),
                        bounds_check=breg(e, NSLOT - 1), oob_is_err=False), reads=["rsm", "YSd"], writes=[f"yab{b}{k}"], dma_key=f"yg{b}{k}")
                for k, wv_ in enumerate((rsm[:, 0:32], rsm[:, 32:64])):
                    P.op("dve", lambda e, jt=jt, b=b, k=k, wv_=wv_: e.scalar_tensor_tensor(out=x2b[b], in0=yAB[b][k], scalar=wv_[:, jt:jt + 1], in1=x2b[b],
                                                                                       op0=ALU.mult, op1=ALU.add),
                         reads=[f"yab{b}{k}", f"x2b{b}", "rsm"], writes=[f"x2b{b}"])
                P.op("act", lambda e, rows=rows, b=b: e.dma_start(out=rows, in_=x2b[b]), reads=[f"x2b{b}"], writes=[f"outd2_{jt}"], dma_key=f"ost{b}", is_out=True)

        for s in range(nseq):
            for j in range(NT):
                rmsnorm_T(x_d[s, j * 128:(j + 1) * 128, :], nmix, "nmix", hT, "hT", j, j,
                          junk=(hm[0][:, 2 * (j % 2):2 * (j % 2) + 2, :].rearrange("p a b -> p (a b)"), f"hmj{j % 2}"))
            if stage in ("full", "sb", "sbproj", "merge"):
                sb_proj()
                if stage == "sbproj":
                    break
                if stage in ("full", "merge"):
                    MEMQ.extend(mem_items(s))
                sb_attention()
                mem_drain()
                if stage == "sb":
                    break
            if stage == "mem":
                mem_mixer(s)
                break
            if stage in ("full", "dil", "merge"):
                dil_mixer(s)
                if stage == "dil":
                    break
            merge_phase(s)
            if stage == "merge":
                break

        if stage == "full":
            moe_sparse()

        dsrc = {"sbproj": (QT, ["QT"]), "sb": (oT_sb, ["oT_sb"]), "mem": (oT_mem, ["oT_mem"]), "dil": (oT_dil, ["oT_dil"]),
                "merge": (mergedT, ["QT", "KT"])}.get(stage)
        if dsrc is not None:
            n = 0
            for a in range(4):
                for hf in range(2):
                    b = n % 2
                    n += 1
                    P.op("dve", lambda e, a=a, hf=hf, b=b: e.tensor_copy(out=xt[b][:], in_=dsrc[0][:, a, hf * 1024:(hf + 1) * 1024]),
                         reads=dsrc[1], writes=[f"xt{b}"])
                    P.op("sp", lambda e, a=a, hf=hf, b=b: e.dma_start(out=dbg_d[:, a, hf * 1024:(hf + 1) * 1024], in_=xt[b][:]),
                         reads=[f"xt{b}"], dma_key=f"dbg{b}", is_out=True)
        P.emit()
    return nc


def _consts():
    j = np.arange(128)[:, None]
    s_ = np.arange(128)[None, :]
    cst = np.zeros((128, 6, 128), np.float32)
    cst[:, 0, :] = -(j >= s_).astype(np.float32)
    cst[:, 1, :] = -1.0
    cst[:, 2, :] = np.where(j < s_, 0.0, NEG)
    cst[:, 3, :] = 0.0
    cst[:, 4, :] = 1.0
    cst[:, 5, :] = (j < s_).astype(np.float32)
    k = np.arange(128)[:, None]
    q = np.arange(128)[None, :]
    masks = np.zeros((128, 23, 128), np.float32)
    masks[:, 0, :] = (k <= q)
    masks[:, 1, :] = (q <= k)
    same4 = (k % 4) == (q % 4)
    for off in range(5):
        ok = same4.copy()
        if off == 0:
            ok &= (k <= q)
        if off == 4:
            ok &= (q <= k)
        masks[:, 2 + off, :] = ok
    same16 = (k % 16) == (q % 16)
    masks[:, 7, :] = same16 & (k <= q)
    for r in range(8, 12):
        masks[:, r, :] = same16
    invf = (10000.0 ** (-np.arange(0, 64, 2, dtype=np.float32) / 64)).astype(np.float32)
    invf = np.ascontiguousarray(np.broadcast_to(invf[None, :], (128, 32))).astype(np.float32)
    return cst, masks, invf


def _pc(v):
    return np.ascontiguousarray(np.asarray(v, np.float32).reshape(-1, 128).T)


def _rows(w, k):
    w = np.asarray(w, np.float32)
    e, kp, n = w.shape
    return np.ascontiguousarray(w.reshape(e, k, 128, n).transpose(0, 2, 1, 3).reshape(e * 128, k * n))


def make_in_maps(inputs):
    f = lambda a: np.ascontiguousarray(np.asarray(a))
    x = f(inputs["x"]); mem = f(inputs["mem"]); pos = f(inputs["positions"])
    cst, masks, invf = _consts()
    qn = np.asarray(inputs["qn_dil"], np.float32)[0]
    kn = np.asarray(inputs["kn_dil"], np.float32)[0]
    qk4 = np.zeros((128, 4, 64), np.float32)
    qk4[:, 0, :] = qn[None, :]
    qk4[:, 1, :] = np.concatenate([qn[32:], qn[:32]])[None, :]
    qk4[:, 2, :] = kn[None, :]
    qk4[:, 3, :] = np.concatenate([kn[32:], kn[:32]])[None, :]
    shared = dict(
        w_in=f(inputs["w_in"][0]),
        nmix=_pc(inputs["norm_mix"][0]), nmem=_pc(inputs["norm_mem"][0]), nffn=_pc(inputs["norm_ffn"][0]),
        bgate=_pc(inputs["b_gate"][0]),
        qk4=qk4,
        qnm=np.ascontiguousarray(np.broadcast_to(np.asarray(inputs["qn_mem"], np.float32)[0][None, :], (128, 128))),
        knm=np.ascontiguousarray(np.broadcast_to(np.asarray(inputs["kn_mem"], np.float32)[0][None, :], (128, 128))),
        wkv=f(inputs["w_mem_kv"][0]),
        wo=np.ascontiguousarray(np.stack([inputs["w_o_sb"][0], inputs["w_o_mem"][0], inputs["w_o_dil"][0]])),
        wout=f(inputs["w_out"][0]),
        wr=np.ascontiguousarray(np.concatenate([inputs["w_router_group"][0], inputs["w_router_expert"][0]], axis=1)),
        br=np.ascontiguousarray(np.broadcast_to(np.concatenate([inputs["b_router_group"][0], inputs["b_router_expert"][0]])[None, :], (128, 20))).astype(np.float32),
        wg=_rows(inputs["w_exp_gate"][0], 8), wu=_rows(inputs["w_exp_up"][0], 8), wd=_rows(inputs["w_exp_down"][0], 4),
        t128=np.ascontiguousarray(np.broadcast_to((128.0 * np.arange(80, dtype=np.float32))[None, :], (128, 80))),
        piota=np.arange(128, dtype=np.float32).reshape(128, 1),
        ident=np.eye(128, dtype=np.float32), cst=cst, masks=masks, invf=invf,
    )
    maps = []
    for c in range(8):
        d = dict(shared)
        d["x"] = np.ascontiguousarray(x[2 * c:2 * c + 2])
        d["mem"] = np.ascontiguousarray(mem[2 * c:2 * c + 2])
        p = pos[2 * c:2 * c + 2].astype(np.int32).reshape(2, NT, 128).transpose(0, 2, 1)
        d["pos"] = np.ascontiguousarray(p)
        maps.append(d)
    return maps


def kernel(**inputs):
    nc = build("full")
    maps = make_in_maps(inputs)
    res = run_bass_kernel_spmd(nc, maps, core_ids=list(range(8)))
    out = np.concatenate([np.asarray(r["out"]) for r in res.results], axis=0)
    return out.astype(np.float32)
```

```python
import numpy as np
from contextlib import ExitStack
import concourse.bass as bass
import concourse.mybir as mybir
from concourse.alu_op_type import AluOpType as ALU
from concourse.bass_utils import run_bass_kernel_spmd

F32 = mybir.dt.float32
BF16 = mybir.dt.bfloat16
I32 = mybir.dt.int32
AF = mybir.ActivationFunctionType
AX = mybir.AxisListType

ENGS = ("pe", "act", "dve", "pool", "sp")
SEM_LIMIT = 30000
NEG = -1.0e30

D = 1024
S = 2048
NT = 16
NSEQ = 2
OFF_B = 4608
OFF_M = 6144
OFF_G = 6656
EPS = 1e-6
NTILE = 80
NSLOT = NTILE * 128
BIGIDX = 1.0e6


class Prog:
    def __init__(self, nc, stack):
        self.nc = nc
        self.stack = stack
        self.ops = []
        self.last_writer = {}
        self.readers = {}
        self.dma_last = {}
        self.out_dmas = []
        self.last_on = {}
        self.pending = {e: set() for e in ENGS}
        self.all_dmas = []
        self.alias = {}

    def barrier(self):
        s = set(self.last_on.values()) | set(self.all_dmas)
        for e in ENGS:
            self.pending[e] |= s
        self.all_dmas = []

    def op(self, eng, fn, reads=(), writes=(), dma_key=None, is_out=False):
        idx = len(self.ops)
        deps = set()
        if self.alias:
            r2 = []
            for k in reads:
                r2.extend(self.alias.get(k, (k,)))
            reads = r2
        for k in reads:
            w = self.last_writer.get(k)
            if w is not None:
                deps.add(w)
        for k in writes:
            w = self.last_writer.get(k)
            if w is not None:
                deps.add(w)
            for r in self.readers.get(k, {}).values():
                deps.add(r)
        if dma_key is not None:
            p = self.dma_last.get(dma_key)
            if p is not None:
                deps.add(p)
            self.dma_last[dma_key] = idx
            self.all_dmas.append(idx)
        deps |= self.pending[eng]
        self.pending[eng] = set()
        deps.discard(idx)
        self.ops.append(dict(eng=eng, fn=fn, deps=deps, dma_key=dma_key))
        for k in reads:
            d = self.readers.setdefault(k, {})
            rk = eng if dma_key is None else ("dma", dma_key)
            d[rk] = idx
        for k in writes:
            self.last_writer[k] = idx
            self.readers[k] = {}
        if dma_key is None:
            self.last_on[eng] = idx
        if is_out:
            self.out_dmas.append(idx)
        return idx

    def emit(self):
        nc = self.nc
        ops = self.ops
        ops.append(dict(eng="sp", fn=None, deps=set(self.out_dmas), dma_key=None))
        has_dep = [False] * len(ops)
        for o in ops:
            for d in o["deps"]:
                has_dep[d] = True
        eng_sem, eng_cnt, dma_sem, dma_cnt = {}, {}, {}, {}
        nsem = [0]

        def new_sem(name):
            nsem[0] += 1
            return self.stack.enter_context(nc.semaphore(f"{name}_{nsem[0]}"))

        for e in ENGS:
            eng_sem[e] = new_sem(f"s_{e}")
            eng_cnt[e] = 0
        events = [None] * len(ops)
        incs = [None] * len(ops)
        for i, o in enumerate(ops):
            if o["dma_key"] is not None:
                k = o["dma_key"]
                if k not in dma_sem or dma_cnt[k] + 16 > SEM_LIMIT:
                    dma_sem[k] = new_sem("d")
                    dma_cnt[k] = 0
                dma_cnt[k] += 16
                events[i] = (dma_sem[k], dma_cnt[k])
                incs[i] = (dma_sem[k], 16)
            elif has_dep[i]:
                e = o["eng"]
                if eng_cnt[e] + 1 > SEM_LIMIT:
                    eng_sem[e] = new_sem(f"s_{e}")
                    eng_cnt[e] = 0
                eng_cnt[e] += 1
                events[i] = (eng_sem[e], eng_cnt[e])
                incs[i] = (eng_sem[e], 1)
        streams = {e: [] for e in ENGS}
        for i, o in enumerate(ops):
            streams[o["eng"]].append(i)

        def run_stream(e, engobj):
            waited = {}
            for i in streams[e]:
                o = ops[i]
                for d in sorted(o["deps"]):
                    od = ops[d]
                    if od["eng"] == "pe" and e == "pe" and od["dma_key"] is None:
                        continue
                    sem, val = events[d]
                    key = id(sem)
                    if waited.get(key, 0) >= val:
                        continue
                    engobj.wait_ge(sem, val)
                    waited[key] = val
                if o["fn"] is None:
                    continue
                ins = o["fn"](engobj)
                if incs[i] is not None:
                    ins.then_inc(incs[i][0], incs[i][1])

        with nc.Block() as block:

            @block.tensor
            def _(eng):
                run_stream("pe", eng)

            @block.scalar
            def _(eng):
                run_stream("act", eng)

            @block.vector
            def _(eng):
                run_stream("dve", eng)

            @block.gpsimd
            def _(eng):
                run_stream("pool", eng)

            @block.sync
            def _(eng):
                run_stream("sp", eng)


def build(stage="full", nseq=NSEQ):
    nc = bass.Bass("TRN2", target_bir_lowering=False)

    def din(name, shape, dt=F32):
        return nc.dram_tensor(name, list(shape), dt, kind="ExternalInput").ap()

    x_d = din("x", [NSEQ, S, D])
    mem_d = din("mem", [NSEQ, 256, D])
    pos_d = din("pos", [NSEQ, 128, NT], I32)
    w_in_d = din("w_in", [D, 9728])
    nmix_d = din("nmix", [128, 8])
    nmem_d = din("nmem", [128, 8])
    nffn_d = din("nffn", [128, 8])
    bgate_d = din("bgate", [128, 24])
    qk4_d = din("qk4", [128, 4, 64])
    qnm_d = din("qnm", [128, 128])
    knm_d = din("knm", [128, 128])
    wkv_d = din("wkv", [D, 1024])
    wo_d = din("wo", [3, 512, D])
    wout_d = din("wout", [D, D])
    wr_d = din("wr", [D, 20])
    br_d = din("br", [128, 20])
    wg_d = din("wg", [2048, 4096])
    wu_d = din("wu", [2048, 4096])
    wd_d = din("wd", [2048, 4096])
    t128_d = din("t128", [128, 80])
    piota_d = din("piota", [128, 1])
    XN_d = nc.dram_tensor("xn_scratch", [NSEQ * S, D], BF16, kind="Internal").ap()
    XS_d = nc.dram_tensor("xs_scratch", [NSLOT, D], BF16, kind="Internal").ap()
    YS_d = nc.dram_tensor("ys_scratch", [NSLOT, D], F32, kind="Internal").ap()
    WBF_d = [nc.dram_tensor(f"wbf_scratch{m}", [2048, 4096], BF16, kind="Internal").ap() for m in range(3)]
    ident_d = din("ident", [128, 128])
    cst_d = din("cst", [128, 6, 128])
    mask_d = din("masks", [128, 23, 128])
    invf_d = din("invf", [128, 32])
    out_d = nc.dram_tensor("out", [NSEQ, S, D], F32, kind="ExternalOutput").ap()
    dbg_d = None
    if stage != "full":
        dbg_d = nc.dram_tensor("dbg", [128, 4, 2048], F32, kind="ExternalOutput").ap()

    with ExitStack() as st:
        def sb(name, shape, dt):
            return st.enter_context(nc.sbuf_tensor("sb_" + name, list(shape), dt))

        def ps(name, shape, dt):
            return st.enter_context(nc.psum_tensor("ps_" + name, list(shape), dt))

        P = Prog(nc, st)

        identf = sb("identf", [128, 128], F32)
        identb = sb("identb", [128, 128], BF16)
        cstb = sb("cstb", [128, 6, 128], BF16)
        maskb = sb("maskb", [128, 23, 128], BF16)
        invf = sb("invf", [128, 32], F32)
        nmix = sb("nmix", [128, 8], F32)
        nmem = sb("nmem", [128, 8], F32)
        nffn = sb("nffn", [128, 8], F32)
        bgate = sb("bgate", [128, 24], F32)
        qk4 = sb("qk4", [128, 4, 64], F32)
        qnm = sb("qnm", [128, 128], F32)
        knm = sb("knm", [128, 128], F32)
        brt = sb("brt", [128, 20], F32)
        wrb = sb("wrb", [128, 8, 20], BF16)
        t128 = sb("t128", [128, 80], F32)
        piota = sb("piota", [128, 1], F32)

        def ld(dst, src, key, eng="sp"):
            P.op(eng, lambda e: e.dma_start(out=dst, in_=src), writes=[key], dma_key="c_" + key)

        ld(identf[:], ident_d, "identf")
        ld(invf[:], invf_d, "invf")
        ld(nmix[:], nmix_d, "nmix")
        ld(nmem[:], nmem_d, "nmem")
        ld(nffn[:], nffn_d, "nffn")
        ld(bgate[:], bgate_d, "bgate")
        ld(qk4[:], qk4_d, "qk4")
        ld(qnm[:], qnm_d, "qnm")
        ld(knm[:], knm_d, "knm")
        ld(brt[:], br_d, "brt")
        ld(t128[:], t128_d, "t128")
        ld(piota[:], piota_d, "piota")
        ld(cstb[:], cst_d, "cstb", eng="pool")
        ld(maskb[:], mask_d, "maskb", eng="pool")
        ld(wrb[:], wr_d.rearrange("(c p) n -> p c n", p=128), "wrb", eng="pool")
        P.op("dve", lambda e: e.tensor_copy(out=identb[:], in_=identf[:]), reads=["identf"], writes=["identb"])
        NTRI = cstb[:, 0, :]
        NONES = cstb[:, 1, :]
        NMSTRICT = cstb[:, 2, :]
        ZEROS = cstb[:, 3, :]
        ONESB = cstb[:, 4, :]

        A_H = sb("A_H", [128, 16384], BF16)
        hT = A_H[:, :].rearrange("p (c t) -> p c t", t=S)
        A_O = sb("A_O", [128, 16384], F32)
        A_Ob = A_O[:, :].bitcast(BF16)
        oT_sb = A_Ob[:, 0:8192].rearrange("p (a t) -> p a t", t=S)
        oT_mem = A_Ob[:, 8192:16384].rearrange("p (a t) -> p a t", t=S)
        oT_dil = A_Ob[:, 16384:24576].rearrange("p (a t) -> p a t", t=S)
        AOs_f = A_O[:, 12288:16384]
        AOs_b = A_Ob[:, 24576:32768]
        acc = A_O[:, :].rearrange("p (j n) -> p j n", n=D)
        AO_KEYS = ["oT_sb", "oT_mem", "oT_dil", "AOs"]
        A_1 = sb("A_1", [128, 12288], F32)
        A_1b = A_1[:, :].bitcast(BF16)
        QT = A_1b[:, 0:8192].rearrange("p (a t) -> p a t", t=S)
        KT = A_1b[:, 8192:16384].rearrange("p (a t) -> p a t", t=S)
        Vt = A_1b[:, 16384:24576].rearrange("p (j n) -> p j n", n=512)
        A1_KEYS = ["QT", "KT", "Vt"]
        wbuf = [sb(f"wbuf{i}", [128, 8, 512], BF16) for i in range(2)]
        xt = [sb(f"xt{i}", [128, D], F32) for i in range(2)]
        xn = [sb(f"xn{i}", [128, D], BF16) for i in range(2)]
        stat = sb("stat", [128, 64], F32)
        st2 = sb("st2", [128, 128], F32)
        hm = [sb(f"hm{i}", [128, 4, 512], BF16) for i in range(2)]
        Lall = sb("Lall", [128, NSEQ * NT, 20], F32)
        rsm = sb("rsm", [128, 128], F32)
        sli = sb("sli", [128, 64], I32)
        idxw = sb("idxw", [128, 80], I32)
        pb = [ps(f"pb{i}", [128, 512], F32) for i in range(8)]

        wctr = [0]

        def load_w(src_aps):
            if not isinstance(src_aps, (list, tuple)):
                src_aps = [src_aps]
            i = wctr[0] % 2
            wctr[0] += 1
            o = 0
            for gi, sap in enumerate(src_aps):
                shp = list(sap.shape)
                dst = wbuf[i][:, 0:shp[1], o:o + shp[2]]
                o += shp[2]
                P.op("pool", lambda e, dst=dst, sap=sap: e.dma_start(out=dst, in_=sap), writes=[f"wbuf{i}" if gi == 0 else f"wbuf{i}g{gi}"],
                     dma_key=f"w{i}_{gi}")
            WK[f"wbuf{i}"] = [f"wbuf{i}"] + [f"wbuf{i}g{gi}" for gi in range(1, 3)]
            return wbuf[i], f"wbuf{i}"

        WK = P.alias
        w_in_r = w_in_d.rearrange("(c p) n -> p c n", p=128)

        def rmsnorm_T(src_dram_tile, gain, gkey, dstT, dst_key, j, nsl, junk=None):
            b = nsl % 2
            P.op("sp", lambda e: e.dma_start(out=xt[b][:], in_=src_dram_tile), writes=[f"xt{b}"], dma_key=f"x{b}")
            norm_tile_T(xt[b][:], f"xt{b}", gain, gkey, dstT, dst_key, j, b, junk)

        def norm_tile_T(src, src_key, gain, gkey, dstT, dst_key, j, b, junk=None):
            ss = stat[:, b:b + 1]
            rs = stat[:, 2 + b:3 + b]
            jout, jkey = (xn[b][:], f"xn{b}") if junk is None else junk
            P.op("act", lambda e: e.activation(out=jout, in_=src, func=AF.Square, accum_out=ss),
                 reads=[src_key], writes=[jkey, f"ss{b}"])
            P.op("act", lambda e: e.activation(out=rs, in_=ss, func=AF.Ln, scale=1.0 / D, bias=EPS),
                 reads=[f"ss{b}"], writes=[f"rs{b}"])
            P.op("act", lambda e: e.activation(out=rs, in_=rs, func=AF.Exp, scale=-0.5),
                 reads=[f"rs{b}"], writes=[f"rs{b}"])
            P.op("dve", lambda e: e.tensor_scalar(out=xn[b][:], in0=src, scalar1=rs, scalar2=None, op0=ALU.mult),
                 reads=[src_key, f"rs{b}"], writes=[f"xn{b}"])
            pT = pb[6 + b][:, 0:512].bitcast(BF16).rearrange("p (c t) -> p c t", t=128)
            for c in range(8):
                P.op("pe", lambda e, c=c: e.transpose(out=pT[:, c, :], in_=xn[b][:, c * 128:(c + 1) * 128], identity=identb[:]),
                     reads=[f"xn{b}", "identb"], writes=[f"pb{6 + b}"])
            P.op("dve", lambda e: e.tensor_tensor(out=dstT[:, :, j * 128:(j + 1) * 128], in0=pT,
                                                  in1=gain[:].unsqueeze(2).to_broadcast([128, 8, 128]), op=ALU.mult),
                 reads=[f"pb{6 + b}", gkey], writes=[dst_key])

        SBTMP = dict(
            eT=[sb(f"sb_e{i}", [128, 512], F32) for i in range(2)],
            spT=[sb(f"sb_sp{i}", [128, 512], BF16) for i in range(2)],
            aT=[sb(f"sb_a{i}", [128, 512], BF16) for i in range(2)],
            spsum=[sb(f"sb_sum{i}", [128, 512], BF16) for i in range(2)],
        )
        eT, spT, aT, spsum_ = SBTMP["eT"], SBTMP["spT"], SBTMP["aT"], SBTMP["spsum"]
        spx = sb("sb_spx", [128, 512], BF16)
        aTx = sb("sb_ax", [128, 512], BF16)

        def headnorm_rstd(ps_ap, ps_key, nh, hd, tmpf, tmpkey, ss_ap, sskey):
            n = nh * hd
            P.op("act", lambda e: e.activation(out=tmpf[:, 0:n], in_=ps_ap, func=AF.Square), reads=[ps_key], writes=[tmpkey])
            P.op("dve", lambda e: e.tensor_reduce(out=ss_ap, in_=tmpf[:, 0:n].rearrange("p (h d) -> p h d", d=hd), axis=AX.X, op=ALU.add),
                 reads=[tmpkey], writes=[sskey])
            P.op("act", lambda e: e.activation(out=ss_ap, in_=ss_ap, func=AF.Ln, scale=1.0 / hd, bias=EPS), reads=[sskey], writes=[sskey])
            P.op("act", lambda e: e.activation(out=ss_ap, in_=ss_ap, func=AF.Exp, scale=-0.5), reads=[sskey], writes=[sskey])

        KTz1 = AOs_b.rearrange("p (a t) -> p a t", t=S)

        def sb_proj():
            P.op("pool", lambda e: e.memset(KT[64:128, :, :], 0.0), writes=["KT"])
            P.op("pool", lambda e: e.memset(KTz1[0:64, :, :], 0.0), writes=["AOs"])
            for qk in range(2):
                dstT = QT if qk == 0 else KT
                dkey = "QT" if qk == 0 else "KT"
                wv, wkey = load_w(w_in_r[:, :, OFF_B + qk * 512: OFF_B + (qk + 1) * 512])
                for p in range(4):
                    for c in range(4):
                        bk = (p * 4 + c) % 4
                        for k in range(8):
                            P.op("pe", lambda e, k=k, p=p, c=c, bk=bk, wv=wv: e.matmul(
                                pb[bk][:, :], lhsT=wv[:, k, p * 128:(p + 1) * 128], rhs=hT[:, k, c * 512:(c + 1) * 512],
                                start=(k == 0), stop=(k == 7)), reads=["hT", wkey], writes=[f"pb{bk}"])
                        if qk == 0:
                            P.op("act", lambda e, p=p, c=c, bk=bk: e.activation(
                                out=QT[:, p, c * 512:(c + 1) * 512], in_=pb[bk][:, :], func=AF.Copy, scale=0.125),
                                reads=[f"pb{bk}"], writes=["QT"])
                        else:
                            P.op("act", lambda e, p=p, c=c, bk=bk: e.activation(
                                out=KT[0:64, p, c * 512:(c + 1) * 512], in_=pb[bk][0:64, :], func=AF.Copy),
                                reads=[f"pb{bk}"], writes=["KT"])
                            P.op("dve", lambda e, p=p, c=c, bk=bk: e.tensor_copy(
                                out=KTz1[64:128, p, c * 512:(c + 1) * 512], in_=pb[bk][64:128, :]),
                                reads=[f"pb{bk}"], writes=["AOs"])
            wv, wkey = load_w(w_in_r[:, :, OFF_B + 1024: OFF_B + 1536])
            for j in range(NT):
                bk = j % 4
                for k in range(8):
                    P.op("pe", lambda e, k=k, j=j, bk=bk, wv=wv: e.matmul(
                        pb[bk][:, :], lhsT=hT[:, k, j * 128:(j + 1) * 128], rhs=wv[:, k, :],
                        start=(k == 0), stop=(k == 7)), reads=["hT", wkey], writes=[f"pb{bk}"])
                P.op("dve", lambda e, j=j, bk=bk: e.tensor_copy(out=Vt[:, j, :], in_=pb[bk][:, :]),
                     reads=[f"pb{bk}"], writes=["Vt"])

        MEMQ = []

        def mem_items(s):
            memT2 = hm[0][:, :, :].rearrange("p a (b c) -> p (a b) c", c=256)
            KTm2 = hm[1][:, 0:2, :].rearrange("p a (b c) -> p (a b) c", c=256)
            Vm2 = hm[1][:, 2:4, :]
            QTm = oT_dil
            items = []

            def norm_mem(mt):
                def f():
                    b = mt % 2
                    P.op("sp", lambda e: e.dma_start(out=xt[b][:], in_=mem_d[s, mt * 128:(mt + 1) * 128, :]), writes=[f"xt{b}"], dma_key=f"x{b}")
                    norm_tile_T(xt[b][:], f"xt{b}", nmem, "nmem", memT2, "hm0", mt, b)
                return f
            items += [norm_mem(0), norm_mem(1)]
            wref = {}

            def loadw(name, src_ap):
                def f():
                    wref[name] = load_w(src_ap)
                return f

            def hn_stats(ti, bank):
                tmpf, tk = xt[ti % 2][:, 0:512], f"xt{ti % 2}"
                ssap = st2[:, (ti % 2) * 4:(ti % 2) * 4 + 4]
                headnorm_rstd(pb[bank][:, :], f"pb{bank}", 4, 128, xt[ti % 2], tk, ssap, f"st2m{ti % 2}")

            def hn_apply(ti, bank, gain_ap, gkey):
                tmpf, tk = xt[ti % 2][:, 0:512], f"xt{ti % 2}"
                ssap, sk = st2[:, (ti % 2) * 4:(ti % 2) * 4 + 4], f"st2m{ti % 2}"
                ob, ok = xn[ti % 2][:, 0:512], f"xn{ti % 2}"
                P.op("dve", lambda e: e.tensor_tensor(out=tmpf.rearrange("p (h d) -> p h d", d=128), in0=pb[bank][:, :].rearrange("p (h d) -> p h d", d=128),
                                                      in1=ssap.unsqueeze(2).to_broadcast([128, 4, 128]), op=ALU.mult), reads=[f"pb{bank}", sk], writes=[tk])
                P.op("dve", lambda e: e.tensor_tensor(out=ob.rearrange("p (h d) -> p h d", d=128), in0=tmpf.rearrange("p (h d) -> p h d", d=128),
                                                      in1=gain_ap.unsqueeze(1).to_broadcast([128, 4, 128]), op=ALU.mult), reads=[tk, gkey], writes=[ok])

            def hn_T(ti, dstT, dkey, col0):
                ob, ok = xn[ti % 2][:, 0:512], f"xn{ti % 2}"
                pT = pb[7][:, 0:256].bitcast(BF16).rearrange("p (h t) -> p h t", t=128)
                for h in range(4):
                    P.op("pe", lambda e, h=h: e.transpose(out=pT[:, h, :], in_=ob[:, h * 128:(h + 1) * 128], identity=identb[:]), reads=[ok, "identb"], writes=["pb7"])
                P.op("act", lambda e: e.activation(out=dstT[:, :, col0:col0 + 128], in_=pT, func=AF.Copy), reads=["pb7"], writes=[dkey])

            items.append(loadw("k", wkv_r[:, :, 0:512]))
            items.append(loadw("v", wkv_r[:, :, 512:1024]))
            for mt in range(2):
                def kproj(mt=mt):
                    wk, wkk = wref["k"]
                    for k in range(8):
                        P.op("pe", lambda e, k=k: e.matmul(pb[6][:, :], lhsT=memT2[:, k, mt * 128:(mt + 1) * 128], rhs=wk[:, k, :], start=(k == 0), stop=(k == 7)),
                             reads=["hm0", wkk], writes=["pb6"])
                    hn_stats(mt, 6)
                items.append(kproj)
                items.append(lambda mt=mt: hn_apply(mt, 6, knm[:, :], "knm"))
                items.append(lambda mt=mt: hn_T(mt, KTm2, "hm1", mt * 128))

                def vproj(mt=mt):
                    wv, wvk = wref["v"]
                    for k in range(8):
                        P.op("pe", lambda e, k=k: e.matmul(pb[6][:, :], lhsT=memT2[:, k, mt * 128:(mt + 1) * 128], rhs=wv[:, k, :], start=(k == 0), stop=(k == 7)),
                             reads=["hm0", wvk], writes=["pb6"])
                    P.op("dve", lambda e: e.tensor_copy(out=Vm2[:, mt, :], in_=pb[6][:, :]), reads=["pb6"], writes=["hm1"])
                items.append(vproj)
            items.append(loadw("q", w_in_r[:, :, OFF_M:OFF_M + 512]))
            for j in range(NT):
                def qproj(j=j):
                    wq, wqk = wref["q"]
                    for k in range(8):
                        P.op("pe", lambda e, k=k: e.matmul(pb[6][:, :], lhsT=hT[:, k, j * 128:(j + 1) * 128], rhs=wq[:, k, :], start=(k == 0), stop=(k == 7)),
                             reads=["hT", wqk], writes=["pb6"])
                    hn_stats(j, 6)
                items.append(qproj)
                items.append(lambda j=j: hn_apply(j, 6, qnm[:, :], "qnm"))
                items.append(lambda j=j: hn_T(j, QTm, "oT_dil", j * 128))
            n = 0
            for c8 in range(8):
                for h in range(4):
                    pm, pk = xn[n % 2][:, 512:1024], f"xn{n % 2}"
                    rc, rk = xt[n % 2][:, 512:768], f"xt{n % 2}"
                    n += 1
                    q0 = c8 * 256

                    def att_a(h=h, q0=q0, pm=pm, pk=pk):
                        for mb in range(2):
                            P.op("pe", lambda e, mb=mb: e.matmul(pb[6][:, mb * 256:(mb + 1) * 256], lhsT=KTm2[:, h, mb * 128:(mb + 1) * 128], rhs=QTm[:, h, q0:q0 + 256],
                                                                 start=True, stop=True), reads=["hm1", "oT_dil"], writes=["pb6"])
                        P.op("act", lambda e: e.activation(out=pm, in_=pb[6][:, :], func=AF.Exp, scale=128.0 ** -0.5), reads=["pb6"], writes=[pk])

                    def att_b(h=h, q0=q0, pm=pm, pk=pk, rc=rc, rk=rk):
                        for mb in range(2):
                            P.op("pe", lambda e, mb=mb: e.matmul(pb[7][:, 0:256], lhsT=Vm2[:, mb, h * 128:(h + 1) * 128], rhs=pm[:, mb * 256:(mb + 1) * 256],
                                                                 start=(mb == 0), stop=(mb == 1)), reads=["hm1", pk], writes=["pb7"])
                        for mb in range(2):
                            P.op("pe", lambda e, mb=mb: e.matmul(pb[7][:, 256:512], lhsT=ONESB, rhs=pm[:, mb * 256:(mb + 1) * 256],
                                                                 start=(mb == 0), stop=(mb == 1)), reads=["cstb", pk], writes=["pb7"])
                        P.op("dve", lambda e: e.reciprocal(out=rc, in_=pb[7][:, 256:512]), reads=["pb7"], writes=[rk])
                        P.op("dve", lambda e: e.tensor_tensor(out=oT_mem[:, h, q0:q0 + 256], in0=pb[7][:, 0:256], in1=rc, op=ALU.mult),
                             reads=["pb7", rk], writes=["oT_mem"])
                    items.append(att_a)
                    items.append(att_b)
            return items

        def mem_drain(k=None):
            cnt = 0
            while MEMQ and (k is None or cnt < k):
                MEMQ.pop(0)()
                cnt += 1

        CONV = [(m_, e_x) for e_x in range(16) for m_ in range(3)]

        def sb_attention():
            its = []
            hcount = 0
            for c in range(4):
                for p in range(4):
                    for hh in range(2):
                        nkb = 4 * c + 4
                        for kb in range(nkb - 1, -1, -1):
                            its.append((c, p, hh, kb, nkb, hcount))
                        hcount += 1
            spT3 = spT + [spx]
            aT3 = aT + [aTx]

            def geom(n):
                c, p, hh, kb, nkb, hc = its[n]
                P0 = 64 * hh
                off = max(0, kb - 4 * c) * 128
                diag = kb >= 4 * c
                ob = 4 + hc % 2
                sm = hc % 2
                return c, p, hh, kb, nkb, hc, P0, off, diag, ob, sm

            def st_a(n):
                if CONV and n % 5 == 0:
                    m_, e_x = CONV.pop(0)
                    wsrc_ = (wg_d, wu_d, wd_d)[m_]
                    P.op("pool", lambda e: e.dma_start(out=WBF_d[m_][e_x * 128:(e_x + 1) * 128, :].rearrange("p (a n) -> p a n", n=2048),
                                                       in_=wsrc_[e_x * 128:(e_x + 1) * 128, :].rearrange("p (a n) -> p a n", n=2048)),
                         writes=["WBFd"], dma_key=f"cv{len(CONV) % 4}")
                if n % 5 in (1, 3):
                    mem_drain(1)
                c, p, hh, kb, nkb, hc, P0, off, diag, ob, sm = geom(n)
                h = 2 * p + hh
                i = n % 2
                zb = i
                e_, s_ = eT[i], spT3[n % 3]
                psO = pb[ob][:, :]
                qs = QT[:, p, c * 512:(c + 1) * 512]
                ks = (KT if hh == 0 else KTz1)[:, p, kb * 128:(kb + 1) * 128]
                if kb == nkb - 1:
                    P.op("pe", lambda e: e.matmul(psO, lhsT=ZEROS, rhs=qs, start=True, stop=False),
                         reads=["cstb", "QT"], writes=[f"pb{ob}"])
                    P.op("pool", lambda e: e.memset(spsum_[sm][:], 0.0), writes=[f"spsum{sm}"])
                P.op("pe", lambda e: e.matmul(pb[zb][:, off:512], lhsT=ks, rhs=qs[:, off:512], start=True, stop=not diag),
                     reads=["KT", "QT", "AOs"], writes=[f"pb{zb}"])
                if diag:
                    P.op("pe", lambda e: e.matmul(pb[zb][:, off:off + 128], lhsT=identb[:], rhs=NMSTRICT, start=False, stop=True),
                         reads=["identb", "cstb"], writes=[f"pb{zb}"])
                P.op("act", lambda e: e.activation(out=e_[:, off:512], in_=pb[zb][:, off:512], func=AF.Exp), reads=[f"pb{zb}"], writes=[f"eT{i}"])
                P.op("act", lambda e: e.activation(out=s_[:, off:512], in_=e_[:, off:512], func=AF.Ln, bias=1.0), reads=[f"eT{i}"], writes=[f"spT{n % 3}"])

            def st_b(n):
                c, p, hh, kb, nkb, hc, P0, off, diag, ob, sm = geom(n)
                cb = 2 + n % 2
                s_, a_ = spT3[n % 3], aT3[n % 3]
                qs = QT[:, p, c * 512:(c + 1) * 512]
                ks = (KT if hh == 0 else KTz1)[:, p, kb * 128:(kb + 1) * 128]
                spsum = spsum_[sm]
                P.op("pe", lambda e: e.matmul(pb[cb][:, off:512], lhsT=ks, rhs=qs[:, off:512], start=True, stop=False),
                     reads=["KT", "QT", "AOs"], writes=[f"pb{cb}"])
                if diag:
                    P.op("pe", lambda e: e.matmul(pb[cb][:, off:off + 128], lhsT=identb[:], rhs=NMSTRICT, start=False, stop=False),
                         reads=["identb", "cstb"], writes=[f"pb{cb}"])
                off2 = off + 128 if diag else 0
                last_is_tri = not (kb < nkb - 1 and off2 < 512)
                P.op("pe", lambda e: e.matmul(pb[cb][:, off:512], lhsT=NTRI, rhs=s_[:, off:512], start=False, stop=last_is_tri),
                     reads=["cstb", f"spT{n % 3}"], writes=[f"pb{cb}"])
                if not last_is_tri:
                    P.op("pe", lambda e: e.matmul(pb[cb][:, off2:512], lhsT=NONES, rhs=spsum[:, off2:512], start=False, stop=True),
                         reads=["cstb", f"spsum{sm}"], writes=[f"pb{cb}"])
                P.op("act", lambda e: e.activation(out=a_[:, off:512], in_=pb[cb][:, off:512], func=AF.Exp), reads=[f"pb{cb}"], writes=[f"aT{n % 3}"])
                if kb > 0:
                    P.op("pool", lambda e: e.tensor_tensor(out=spsum[:, off:512], in0=spsum[:, off:512], in1=s_[:, off:512], op=ALU.add),
                         reads=[f"spT{n % 3}", f"spsum{sm}"], writes=[f"spsum{sm}"])

            def st_c(n):
                c, p, hh, kb, nkb, hc, P0, off, diag, ob, sm = geom(n)
                h = 2 * p + hh
                a_ = aT3[n % 3]
                psO = pb[ob][:, :]
                P.op("pe", lambda e: e.matmul(psO[:, off:512], lhsT=Vt[:, kb, p * 128:(p + 1) * 128], rhs=a_[:, off:512], start=False, stop=(kb == 0)),
                     reads=["Vt", f"aT{n % 3}"], writes=[f"pb{ob}"])
                if kb == 0:
                    P.op("dve", lambda e: e.tensor_copy(out=oT_sb[P0:P0 + 64, p, c * 512:(c + 1) * 512], in_=psO[P0:P0 + 64, :]), reads=[f"pb{ob}"], writes=["oT_sb"])

            NI = len(its)
            for n in range(NI + 2):
                if n < NI:
                    st_a(n)
                if 0 <= n - 1 < NI:
                    st_b(n - 1)
                if 0 <= n - 2 < NI:
                    st_c(n - 2)

        memT = A_1b[:, 8192:8192 + 2048].rearrange("p (c t) -> p c t", t=256)
        KTm = A_1b[:, 8192 + 2048:8192 + 3072].rearrange("p (h t) -> p h t", t=256)
        Vm = A_1b[:, 8192 + 3072:8192 + 4096].rearrange("p (m n) -> p m n", n=512)
        wkv_r = wkv_d.rearrange("(c p) n -> p c n", p=128)

        def headnorm_to_T(ps_ap, ps_key, gain_ap, gkey, dstT, dkey, col0, ti):
            tmpf, tk = eT[ti % 2], f"eT{ti % 2}"
            ssap = st2[:, (ti % 2) * 4:(ti % 2) * 4 + 4]
            sk = f"st2m{ti % 2}"
            headnorm_rstd(ps_ap, ps_key, 4, 128, tmpf, tk, ssap, sk)
            P.op("dve", lambda e: e.tensor_tensor(out=tmpf[:, :].rearrange("p (h d) -> p h d", d=128),
                                                  in0=ps_ap.rearrange("p (h d) -> p h d", d=128),
                                                  in1=ssap.unsqueeze(2).to_broadcast([128, 4, 128]), op=ALU.mult),
                 reads=[ps_key, sk], writes=[tk])
            ob, ok = aT[ti % 2], f"aT{ti % 2}"
            P.op("dve", lambda e: e.tensor_tensor(out=ob[:, :].rearrange("p (h d) -> p h d", d=128),
                                                  in0=tmpf[:, :].rearrange("p (h d) -> p h d", d=128),
                                                  in1=gain_ap.unsqueeze(1).to_broadcast([128, 4, 128]), op=ALU.mult),
                 reads=[tk, gkey], writes=[ok])
            tb = 6 + ti % 2
            pT = pb[tb][:, 0:256].bitcast(BF16).rearrange("p (h t) -> p h t", t=128)
            for h in range(4):
                P.op("pe", lambda e, h=h: e.transpose(out=pT[:, h, :], in_=ob[:, h * 128:(h + 1) * 128], identity=identb[:]),
                     reads=[ok, "identb"], writes=[f"pb{tb}"])
            P.op("act", lambda e: e.activation(out=dstT[:, :, col0:col0 + 128], in_=pT, func=AF.Copy), reads=[f"pb{tb}"], writes=[dkey])

        def mem_mixer(s):
            for mt in range(2):
                rmsnorm_T(mem_d[s, mt * 128:(mt + 1) * 128, :], nmem, "nmem", memT, "KT", mt, mt)
            wk, wkk = load_w(wkv_r[:, :, 0:512])
            wv, wvk = load_w(wkv_r[:, :, 512:1024])
            for mt in range(2):
                bk = mt
                for k in range(8):
                    P.op("pe", lambda e, k=k, mt=mt, bk=bk: e.matmul(pb[bk][:, :], lhsT=memT[:, k, mt * 128:(mt + 1) * 128], rhs=wk[:, k, :],
                                                                      start=(k == 0), stop=(k == 7)), reads=["KT", wkk], writes=[f"pb{bk}"])
                headnorm_to_T(pb[bk][:, :], f"pb{bk}", knm[:, :], "knm", KTm, "KT", mt * 128, mt)
                bv = 2 + mt
                for k in range(8):
                    P.op("pe", lambda e, k=k, mt=mt, bv=bv: e.matmul(pb[bv][:, :], lhsT=memT[:, k, mt * 128:(mt + 1) * 128], rhs=wv[:, k, :],
                                                                      start=(k == 0), stop=(k == 7)), reads=["KT", wvk], writes=[f"pb{bv}"])
                P.op("dve", lambda e, mt=mt, bv=bv: e.tensor_copy(out=Vm[:, mt, :], in_=pb[bv][:, :]), reads=[f"pb{bv}"], writes=["KT"])
            wq, wqk = load_w(w_in_r[:, :, OFF_M:OFF_M + 512])
            def mq_mm(j):
                bk = j % 4
                for k in range(8):
                    P.op("pe", lambda e, k=k, j=j, bk=bk: e.matmul(pb[bk][:, :], lhsT=hT[:, k, j * 128:(j + 1) * 128], rhs=wq[:, k, :],
                                                                    start=(k == 0), stop=(k == 7)), reads=["hT", wqk], writes=[f"pb{bk}"])
            mq_mm(0)
            mq_mm(1)
            for j in range(NT):
                if j + 2 < NT:
                    mq_mm(j + 2)
                bk = j % 4
                headnorm_to_T(pb[bk][:, :], f"pb{bk}", qnm[:, :], "qnm", QT, "QT", j * 128, j)
            n = 0
            for c in range(4):
                for h in range(4):
                    nb_, db_ = 4, 5
                    for mb in range(2):
                        sbk = n % 2
                        pm, pk = spT[n % 2], f"spT{n % 2}"
                        n += 1
                        P.op("pe", lambda e, h=h, c=c, mb=mb, sbk=sbk: e.matmul(pb[sbk][:, :], lhsT=KTm[:, h, mb * 128:(mb + 1) * 128],
                                                                                 rhs=QT[:, h, c * 512:(c + 1) * 512], start=True, stop=True),
                             reads=["KT", "QT"], writes=[f"pb{sbk}"])
                        P.op("act", lambda e, sbk=sbk, pm=pm: e.activation(out=pm[:, :], in_=pb[sbk][:, :], func=AF.Exp, scale=128.0 ** -0.5),
                             reads=[f"pb{sbk}"], writes=[pk])
                        P.op("pe", lambda e, h=h, mb=mb, pm=pm: e.matmul(pb[4][:, :], lhsT=Vm[:, mb, h * 128:(h + 1) * 128], rhs=pm[:, :],
                                                                          start=(mb == 0), stop=(mb == 1)), reads=["KT", pk], writes=["pb4"])
                        P.op("pe", lambda e, mb=mb, pm=pm: e.matmul(pb[5][:, :], lhsT=ONESB, rhs=pm[:, :],
                                                                     start=(mb == 0), stop=(mb == 1)), reads=["cstb", pk], writes=["pb5"])
                    rc, rk = eT[(c * 4 + h) % 2], f"eT{(c * 4 + h) % 2}"
                    P.op("dve", lambda e, rc=rc: e.reciprocal(out=rc[:, :], in_=pb[5][:, :]), reads=["pb5"], writes=[rk])
                    P.op("dve", lambda e, rc=rc, h=h, c=c: e.tensor_tensor(out=oT_mem[:, h, c * 512:(c + 1) * 512], in0=pb[4][:, :], in1=rc[:, :], op=ALU.mult),
                         reads=["pb4", rk], writes=["oT_mem"])

        QKTA = A_1b[:, 0:18432].rearrange("p (a t) -> p a t", t=S)
        VA = A_1b[:, 18432:18432 + 6144].rearrange("p (j g n) -> p j g n", g=3, n=128)
        ropeT = AOs_f.rearrange("p (a j d) -> p a j d", a=4, d=64)
        TWO_PI = float(2 * np.pi)

        def rope_tables(s):
            posi = st2[:, 16:32].bitcast(I32)
            posf = st2[:, 32:48]
            P.op("sp", lambda e: e.dma_start(out=posi, in_=pos_d[s]), writes=["posi"], dma_key="pos")
            P.op("dve", lambda e: e.tensor_copy(out=posf, in_=posi), reads=["posi"], writes=["posf"])
            ang = eT[0][:, :].rearrange("p (j d) -> p j d", d=32)
            P.op("dve", lambda e: e.tensor_tensor(out=ang, in0=posf.unsqueeze(2).to_broadcast([128, NT, 32]),
                                                  in1=invf[:, :].unsqueeze(1).to_broadcast([128, NT, 32]), op=ALU.mult),
                 reads=["posf", "invf"], writes=["eT0"])
            red = eT[1][:, :]
            ki = xt[0][:, 0:512].bitcast(I32)
            kf = xt[0][:, 512:1024]
            for which in range(2):
                shift = 0.0 if which == 0 else float(np.pi / 2)
                P.op("dve", lambda e, shift=shift: e.tensor_scalar(out=red, in0=eT[0][:, :], scalar1=shift, scalar2=None, op0=ALU.add),
                     reads=["eT0"], writes=["eT1"])
                P.op("dve", lambda e: e.tensor_scalar(out=ki, in0=red, scalar1=1.0 / TWO_PI, scalar2=None, op0=ALU.mult), reads=["eT1"], writes=["xt0"])
                P.op("dve", lambda e: e.tensor_copy(out=kf, in_=ki), reads=["xt0"], writes=["xt0"])
                P.op("dve", lambda e: e.scalar_tensor_tensor(out=red, in0=kf, scalar=-TWO_PI, in1=red, op0=ALU.mult, op1=ALU.add),
                     reads=["xt0", "eT1"], writes=["eT1"])
                P.op("dve", lambda e: e.tensor_scalar(out=kf, in0=red, scalar1=float(np.pi), scalar2=None, op0=ALU.is_gt), reads=["eT1"], writes=["xt0"])
                P.op("dve", lambda e: e.scalar_tensor_tensor(out=red, in0=kf, scalar=-TWO_PI, in1=red, op0=ALU.mult, op1=ALU.add),
                     reads=["xt0", "eT1"], writes=["eT1"])
                P.op("dve", lambda e: e.tensor_scalar(out=kf, in0=red, scalar1=float(-np.pi), scalar2=None, op0=ALU.is_lt), reads=["eT1"], writes=["xt0"])
                P.op("dve", lambda e: e.scalar_tensor_tensor(out=red, in0=kf, scalar=TWO_PI, in1=red, op0=ALU.mult, op1=ALU.add),
                     reads=["xt0", "eT1"], writes=["eT1"])
                P.op("dve", lambda e: e.tensor_scalar(out=red, in0=red, scalar1=3.1415925, scalar2=-3.1415925, op0=ALU.min, op1=ALU.max),
                     reads=["eT1"], writes=["eT1"])
                trig = xt[1][:, which * 512:(which + 1) * 512]
                P.op("act", lambda e, trig=trig: e.activation(out=trig, in_=red, func=AF.Sin), reads=["eT1"], writes=["xt1"])
            sin3 = xt[1][:, 0:512].rearrange("p (j d) -> p j d", d=32)
            cos3 = xt[1][:, 512:1024].rearrange("p (j d) -> p j d", d=32)
            for qk in range(2):
                Ct = ropeT[:, 2 * qk, :, :].rearrange("p j (u d) -> p j u d", d=32)
                St = ropeT[:, 2 * qk + 1, :, :].rearrange("p j (u d) -> p j u d", d=32)
                gn = qk4[:, 2 * qk, :].rearrange("p (u d) -> p u d", d=32)
                gs = qk4[:, 2 * qk + 1, :].rearrange("p (u d) -> p u d", d=32)
                P.op("dve", lambda e, Ct=Ct, gn=gn: e.tensor_tensor(out=Ct, in0=cos3.unsqueeze(2).to_broadcast([128, NT, 2, 32]),
                                                                  in1=gn.unsqueeze(1).to_broadcast([128, NT, 2, 32]), op=ALU.mult),
                     reads=["xt1", "qk4"], writes=["AOs"])
                P.op("dve", lambda e, St=St, gs=gs: e.tensor_tensor(out=St, in0=sin3.unsqueeze(2).to_broadcast([128, NT, 2, 32]),
                                                                  in1=gs.unsqueeze(1).to_broadcast([128, NT, 2, 32]), op=ALU.mult),
                     reads=["xt1", "qk4"], writes=["AOs"])
                P.op("dve", lambda e, St=St: e.tensor_scalar(out=St[:, :, 0, :], in0=St[:, :, 0, :], scalar1=-1.0, scalar2=None, op0=ALU.mult),
                     reads=["AOs"], writes=["AOs"])

        def dil_mixer(s):
            rope_tables(s)
            P.op("pool", lambda e: e.memset(QKTA[64:128, 3:6, :], 0.0), writes=["QT", "KT", "Vt"])
            P.op("pool", lambda e: e.memset(QKTA[0:64, 6:9, :], 0.0), writes=["QT", "KT", "Vt"])
            ucount = [0]
            for p in range(4):
                wts = []
                for t in range(2):
                    w_, wk_ = load_w([w_in_r[:, :, t * 1536 + g * 512 + p * 128: t * 1536 + g * 512 + (p + 1) * 128] for g in range(3)])
                    wts.append((w_, wk_))
                def pj_mm(j, wts=wts):
                    for t in range(2):
                        w_, wk_ = wts[t]
                        bk = 2 * (j % 3) + t
                        for k in range(8):
                            P.op("pe", lambda e, k=k, j=j, bk=bk, w_=w_: e.matmul(pb[bk][:, 0:384], lhsT=hT[:, k, j * 128:(j + 1) * 128], rhs=w_[:, k, 0:384],
                                                                                   start=(k == 0), stop=(k == 7)), reads=["hT", wk_], writes=[f"pb{bk}"])

                PEND = []
                pj_mm(0)
                pj_mm(1)
                for j in range(NT):
                    if j + 2 < NT:
                        pj_mm(j + 2)
                    ssq = st2[:, 48 + 12 * (j % 2):48 + 12 * (j % 2) + 12]
                    ssk = f"st2a{j % 2}"
                    obuf, obk = (spT[j % 2], f"spT{j % 2}")
                    for t in range(2):
                        bk = 2 * (j % 3) + t
                        psv = pb[bk][:, 0:384]
                        tf, tfk = eT[t], f"eT{t}"
                        headnorm_rstd(psv, f"pb{bk}", 6, 64, tf, tfk, ssq[:, 6 * t:6 * t + 6], ssk + str(t))
                        Cj = ropeT[:, 2 * t, j, :]
                        Sj = ropeT[:, 2 * t + 1, j, :]
                        ps3 = psv.rearrange("p (a d) -> p a d", d=64)
                        tA = xt[t][:, 0:384].rearrange("p (a d) -> p a d", d=64)
                        tB = xt[t][:, 384:768].rearrange("p (a d) -> p a d", d=64)
                        xk = f"xt{t}"
                        P.op("dve", lambda e, tA=tA, ps3=ps3, Cj=Cj: e.tensor_tensor(out=tA, in0=ps3, in1=Cj.unsqueeze(1).to_broadcast([128, 6, 64]), op=ALU.mult),
                             reads=[f"pb{bk}", "AOs"], writes=[xk])
                        P.op("dve", lambda e, tB=tB, ps3=ps3, Sj=Sj: e.tensor_tensor(out=tB[:, :, 0:32], in0=ps3[:, :, 32:64],
                                                                                      in1=Sj[:, 0:32].unsqueeze(1).to_broadcast([128, 6, 32]), op=ALU.mult),
                             reads=[f"pb{bk}", "AOs"], writes=[xk])
                        P.op("dve", lambda e, tB=tB, ps3=ps3, Sj=Sj: e.tensor_tensor(out=tB[:, :, 32:64], in0=ps3[:, :, 0:32],
                                                                                      in1=Sj[:, 32:64].unsqueeze(1).to_broadcast([128, 6, 32]), op=ALU.mult),
                             reads=[f"pb{bk}", "AOs"], writes=[xk])
                        P.op("pool", lambda e, tA=tA, tB=tB: e.tensor_tensor(out=tA, in0=tA, in1=tB, op=ALU.add), reads=[xk], writes=[xk])
                        rs_ = ssq[:, 6 * t:6 * t + 6]
                        dstb = obuf if t == 0 else aT[j % 2]
                        dstk = obk if t == 0 else f"aT{j % 2}"
                        P.op("pool", lambda e, tA=tA, rs_=rs_, dstb=dstb: e.tensor_tensor(
                            out=dstb[:, 0:384].rearrange("p (a d) -> p a d", d=64),
                            in0=tA, in1=rs_.unsqueeze(2).to_broadcast([128, 6, 64]), op=ALU.mult),
                            reads=[xk, ssk + str(t)], writes=[dstk])
                    tb = 6 + j % 2
                    pT = pb[tb][:, 0:384].bitcast(BF16).rearrange("p (a t) -> p a t", t=128)
                    for t in range(2):
                        srcb = obuf if t == 0 else aT[j % 2]
                        srck = obk if t == 0 else f"aT{j % 2}"
                        for g in range(3):
                            P.op("pe", lambda e, t=t, g=g, srcb=srcb, pT=pT: e.transpose(out=pT[:, 3 * t + g, :], in_=srcb[:, g * 128:(g + 1) * 128], identity=identb[:]),
                                 reads=[srck, "identb"], writes=[f"pb{tb}"])
                    def evac(j=j, pT=pT, tb=tb):
                        P.op("act", lambda e: e.activation(out=QKTA[:, 0:3, j * 128:(j + 1) * 128], in_=pT[:, 0:3, :], func=AF.Copy),
                             reads=[f"pb{tb}"], writes=["QT", "KT", "Vt"])
                        P.op("act", lambda e: e.activation(out=QKTA[0:64, 3:6, j * 128:(j + 1) * 128], in_=pT[0:64, 3:6, :], func=AF.Copy),
                             reads=[f"pb{tb}"], writes=["QT", "KT", "Vt"])
                        P.op("act", lambda e: e.activation(out=QKTA[64:128, 6:9, j * 128:(j + 1) * 128], in_=pT[64:128, 3:6, :], func=AF.Copy),
                             reads=[f"pb{tb}"], writes=["QT", "KT", "Vt"])
                    if PEND:
                        PEND.pop(0)()
                    PEND.append(evac)
                while PEND:
                    PEND.pop(0)()
                w_, wk_ = load_w([w_in_r[:, :, 2 * 1536 + g * 512 + p * 128: 2 * 1536 + g * 512 + (p + 1) * 128] for g in range(3)])
                for j in range(NT):
                    bk = 4 + j % 2
                    for k in range(8):
                        P.op("pe", lambda e, k=k, j=j, bk=bk, w_=w_: e.matmul(pb[bk][:, 0:384], lhsT=hT[:, k, j * 128:(j + 1) * 128], rhs=w_[:, k, 0:384],
                                                                               start=(k == 0), stop=(k == 7)), reads=["hT", wk_], writes=[f"pb{bk}"])
                    P.op("act", lambda e, j=j, bk=bk: e.activation(out=VA[:, j, :, :], in_=pb[bk][:, 0:384].rearrange("p (g n) -> p g n", g=3),
                                                                   func=AF.Copy), reads=[f"pb{bk}"], writes=["QT", "KT", "Vt"])
                units_all = []
                for c in range(4):
                    for hh in range(2):
                        ul = []
                        for jj in range(4 * c - 1, 4 * c + 4):
                            if jj < 0:
                                continue
                            t0_, t1_ = max(jj, 4 * c), min(jj + 1, 4 * c + 3)
                            ul.append((0, jj, t0_ - 4 * c, t1_ - 4 * c + 1, 0 + (t0_ - jj)))
                        for jj in range(4 * c - 4, 4 * c + 4):
                            if jj < 0:
                                continue
                            t0_, t1_ = max(jj, 4 * c), min(jj + 4, 4 * c + 3)
                            ul.append((1, jj, t0_ - 4 * c, t1_ - 4 * c + 1, 2 + (t0_ - jj)))
                        for jj in range(0, 4 * c + 4):
                            t0_ = max(jj, 4 * c)
                            ul.append((2, jj, t0_ - 4 * c, 4, 7 if t0_ == jj else 8))
                        for ui, u in enumerate(ul):
                            units_all.append((c, hh, ui, len(ul), u))
                NB = 4 + (p % 2) * 0
                PMB = [hm[i_][:, k_, :] for i_ in range(2) for k_ in range(4)]
                SBK = [0, 1, 2, 7]

                def stage_a(n):
                    c, hh, ui, nu, (g, jj, ta, tb_, mi) = units_all[n]
                    P0 = 64 * hh
                    lo, hi = ta * 128, tb_ * 128
                    sbk = SBK[n % 4]
                    pm, pk = PMB[n % 8], f"hmA{n % 8}"
                    P.op("pe", lambda e: e.matmul(pb[sbk][:, lo:hi], lhsT=QKTA[:, 3 + 3 * hh + g, jj * 128:(jj + 1) * 128],
                                                  rhs=QKTA[:, g, c * 512 + lo:c * 512 + hi], start=True, stop=True),
                         reads=["QT", "KT", "Vt"], writes=[f"pb{sbk}"])
                    P.op("act", lambda e: e.activation(out=pm[:, lo:hi], in_=pb[sbk][:, lo:hi], func=AF.Exp, scale=0.125),
                         reads=[f"pb{sbk}"], writes=[pk])
                    nt_ = tb_ - ta
                    P.op("dve", lambda e: e.tensor_tensor(out=pm[:, lo:hi].rearrange("p (t q) -> p t q", q=128),
                                                          in0=pm[:, lo:hi].rearrange("p (t q) -> p t q", q=128),
                                                          in1=maskb[:, mi:mi + nt_, :], op=ALU.mult),
                         reads=[pk, "maskb"], writes=[pk])

                def stage_b(n, pp=p):
                    c, hh, ui, nu, (g, jj, ta, tb_, mi) = units_all[n]
                    P0 = 64 * hh
                    lo, hi = ta * 128, tb_ * 128
                    pm, pk = PMB[n % 8], f"hmA{n % 8}"
                    hcn = 2 * c + hh
                    nbk, dbk = 3 + 2 * (hcn % 2), 4 + 2 * (hcn % 2)
                    if ui == 0:
                        for bk_ in (nbk, dbk):
                            P.op("pe", lambda e, bk_=bk_: e.matmul(pb[bk_][:, :], lhsT=ZEROS, rhs=QKTA[:, 0, 0:512],
                                                                   start=True, stop=False), reads=["cstb", "QT"], writes=[f"pb{bk_}"])
                    last = (ui == nu - 1)
                    P.op("pe", lambda e: e.matmul(pb[nbk][:, lo:hi], lhsT=VA[:, jj, g, :], rhs=pm[:, lo:hi], start=False, stop=last),
                         reads=[pk, "QT", "KT", "Vt"], writes=[f"pb{nbk}"])
                    P.op("pe", lambda e: e.matmul(pb[dbk][:, lo:hi], lhsT=ONESB, rhs=pm[:, lo:hi], start=False, stop=last),
                         reads=[pk, "cstb"], writes=[f"pb{dbk}"])
                    if last:
                        rc, rk = eT[hcn % 2], f"eT{hcn % 2}"
                        P.op("act", lambda e: e.activation(out=rc[P0:P0 + 64, :], in_=pb[dbk][P0:P0 + 64, :], func=AF.Ln), reads=[f"pb{dbk}"], writes=[rk])
                        P.op("act", lambda e: e.activation(out=rc[P0:P0 + 64, :], in_=rc[P0:P0 + 64, :], func=AF.Exp, scale=-1.0), reads=[rk], writes=[rk])
                        P.op("dve", lambda e: e.tensor_tensor(out=oT_dil[P0:P0 + 64, pp, c * 512:(c + 1) * 512], in0=pb[nbk][P0:P0 + 64, :],
                                                              in1=rc[P0:P0 + 64, :], op=ALU.mult),
                             reads=[f"pb{nbk}", rk], writes=["oT_dil"])

                NU = len(units_all)
                LAG = 4
                for n in range(NU + LAG):
                    if n < NU:
                        stage_a(n)
                    if n - LAG >= 0:
                        stage_b(n - LAG)

        mergedT = A_1b[:, 0:16384].rearrange("p (c t) -> p c t", t=S)
        wo_dil = A_1b[:, 16384:16384 + 4096].rearrange("p (k n) -> p k n", n=D)
        wo_sbm = AOs_b.rearrange("p (b k n) -> p b k n", b=2, n=D)
        wout_b = AOs_b.rearrange("p (k n) -> p k n", n=D)
        wo_r = wo_d.rearrange("b (k p) n -> p b k n", p=128)
        oTs = [oT_sb, oT_mem, oT_dil]
        oTk = ["oT_sb", "oT_mem", "oT_dil"]
        GOFF = [OFF_G + 1 * D, OFF_G + 2 * D, OFF_G + 0 * D]
        GB = [1, 2, 0]

        def merge_phase(s):
            P.op("pool", lambda e: e.dma_start(out=wo_sbm[:, 0], in_=wo_r[:, 0]), writes=["AOs"], dma_key="wo0")
            P.op("pool", lambda e: e.dma_start(out=wo_sbm[:, 1], in_=wo_r[:, 1]), writes=["AOs"], dma_key="wo0")
            P.op("pool", lambda e: e.dma_start(out=wo_dil, in_=wo_r[:, 2]), writes=["Vt"], dma_key="wo1")
            n = 0
            for f in range(8):
                wgt, wgk = load_w([w_in_r[:, :, GOFF[b] + f * 128: GOFF[b] + (f + 1) * 128] for b in range(3)])
                for c in range(4):
                    mt_ = xt[0][:, 0:512]
                    tt_ = xt[0][:, 512:1024]
                    for b in range(3):
                        gbk, bbk = n % 2, 2 + n % 2
                        gs_, gsk = eT[n % 2], f"eT{n % 2}"
                        n += 1
                        for k in range(8):
                            P.op("pe", lambda e, k=k, b=b, c=c, gbk=gbk, wgt=wgt: e.matmul(pb[gbk][:, :], lhsT=wgt[:, k, b * 128:(b + 1) * 128],
                                                                                          rhs=hT[:, k, c * 512:(c + 1) * 512], start=(k == 0), stop=(k == 7)),
                                 reads=[wgk, "hT"], writes=[f"pb{gbk}"])
                        bias_ap = bgate[:, GB[b] * 8 + f: GB[b] * 8 + f + 1]
                        P.op("act", lambda e, gbk=gbk, gs_=gs_, bias_ap=bias_ap: e.activation(out=gs_[:, :], in_=pb[gbk][:, :], func=AF.Sigmoid, bias=bias_ap),
                             reads=[f"pb{gbk}", "bgate"], writes=[gsk])
                        wsrc = wo_sbm[:, b] if b < 2 else wo_dil
                        wkey = "AOs" if b < 2 else "Vt"
                        for k in range(4):
                            P.op("pe", lambda e, k=k, b=b, c=c, f=f, bbk=bbk, wsrc=wsrc: e.matmul(pb[bbk][:, :], lhsT=wsrc[:, k, f * 128:(f + 1) * 128],
                                                                                                 rhs=oTs[b][:, k, c * 512:(c + 1) * 512], start=(k == 0), stop=(k == 3)),
                                 reads=[wkey, oTk[b]], writes=[f"pb{bbk}"])
                        if b == 0:
                            P.op("dve", lambda e, gs_=gs_, bbk=bbk: e.tensor_tensor(out=mt_, in0=pb[bbk][:, :], in1=gs_[:, :], op=ALU.mult),
                                 reads=[f"pb{bbk}", gsk], writes=["xt0"])
                        else:
                            P.op("dve", lambda e, gs_=gs_, bbk=bbk: e.tensor_tensor(out=tt_, in0=pb[bbk][:, :], in1=gs_[:, :], op=ALU.mult),
                                 reads=[f"pb{bbk}", gsk], writes=["xt0"])
                            dst = mt_ if b == 1 else mergedT[:, f, c * 512:(c + 1) * 512]
                            P.op("dve", lambda e, dst=dst: e.tensor_tensor(out=dst, in0=mt_, in1=tt_, op=ALU.add),
                                 reads=["xt0"], writes=["xt0"] if b == 1 else ["QT", "KT"])
            wout_r = wout_d.rearrange("(k p) n -> p k n", p=128)
            P.op("pool", lambda e: e.dma_start(out=wout_b[:, 0:4], in_=wout_r[:, 0:4]), writes=["AOs"], dma_key="wo0")
            P.op("pool", lambda e: e.dma_start(out=wout_b[:, 4:8], in_=wout_r[:, 4:8]), writes=["AOs"], dma_key="wo0")
            def p5_a(j):
                b = j % 2
                P.op("sp", lambda e: e.dma_start(out=xt[b][:], in_=x_d[s, j * 128:(j + 1) * 128, :]), writes=[f"xt{b}"], dma_key=f"x{b}")
                for hf in range(2):
                    bk = 2 * b + hf
                    for k in range(8):
                        P.op("pe", lambda e, k=k, hf=hf, bk=bk: e.matmul(pb[bk][:, :], lhsT=mergedT[:, k, j * 128:(j + 1) * 128],
                                                                          rhs=wout_b[:, k, hf * 512:(hf + 1) * 512], start=(k == 0), stop=(k == 7)),
                             reads=["QT", "KT", "AOs"], writes=[f"pb{bk}"])
                    P.op("dve", lambda e, hf=hf, bk=bk: e.tensor_tensor(out=xt[b][:, hf * 512:(hf + 1) * 512], in0=pb[bk][:, :],
                                                                         in1=xt[b][:, hf * 512:(hf + 1) * 512], op=ALU.add),
                         reads=[f"pb{bk}", f"xt{b}"], writes=[f"xt{b}"])
                P.op("pool", lambda e: e.dma_start(out=out_d[s, j * 128:(j + 1) * 128, :], in_=xt[b][:]), reads=[f"xt{b}"], writes=[f"outd_{s}_{j}"],
                     dma_key=f"x2st{b}")
                ss = stat[:, b:b + 1]
                rs = stat[:, 2 + b:3 + b]
                P.op("act", lambda e: e.activation(out=xn[b][:], in_=xt[b][:], func=AF.Square, accum_out=ss), reads=[f"xt{b}"], writes=[f"xn{b}", f"ss{b}"])
                P.op("act", lambda e: e.activation(out=rs, in_=ss, func=AF.Ln, scale=1.0 / D, bias=EPS), reads=[f"ss{b}"], writes=[f"rs{b}"])
                P.op("act", lambda e: e.activation(out=rs, in_=rs, func=AF.Exp, scale=-0.5), reads=[f"rs{b}"], writes=[f"rs{b}"])

                def tail():
                    P.op("dve", lambda e: e.tensor_scalar(out=xn[b][:], in0=xt[b][:], scalar1=rs, scalar2=None, op0=ALU.mult),
                         reads=[f"xt{b}", f"rs{b}"], writes=[f"xn{b}"])
                    P.op("pool", lambda e: e.dma_start(out=XN_d[s * S + j * 128: s * S + (j + 1) * 128, :], in_=xn[b][:]), reads=[f"xn{b}"], writes=[f"XNd_{s}_{j}"],
                         dma_key=f"xnst{b}")
                return tail

            def p5_b(j):
                b = j % 2
                pT = pb[6 + b][:, 0:512].bitcast(BF16).rearrange("p (c t) -> p c t", t=128)
                for c in range(8):
                    P.op("pe", lambda e, c=c: e.transpose(out=pT[:, c, :], in_=xn[b][:, c * 128:(c + 1) * 128], identity=identb[:]),
                         reads=[f"xn{b}", "identb"], writes=[f"pb{6 + b}"])
                P.op("dve", lambda e: e.tensor_tensor(out=hT[:, :, j * 128:(j + 1) * 128], in0=pT,
                                                      in1=nffn[:].unsqueeze(2).to_broadcast([128, 8, 128]), op=ALU.mult),
                     reads=[f"pb{6 + b}", "nffn"], writes=["hT"])

            def p5_c(j):
                b = j % 2
                for k in range(8):
                    P.op("pe", lambda e, k=k: e.matmul(pb[4 + b][:, 0:20], lhsT=hT[:, k, j * 128:(j + 1) * 128], rhs=wrb[:, k, :],
                                                       start=(k == 0), stop=(k == 7)), reads=["hT", "wrb"], writes=[f"pb{4 + b}"])
                P.op("dve", lambda e: e.tensor_tensor(out=Lall[:, s * NT + j, :], in0=pb[4 + b][:, 0:20], in1=brt[:, :], op=ALU.add),
                     reads=[f"pb{4 + b}", "brt"], writes=["Lall"])

            for n in range(NT + 2):
                tl = p5_a(n) if n < NT else None
                if 0 <= n - 1 < NT:
                    p5_b(n - 1)
                if 0 <= n - 2 < NT:
                    p5_c(n - 2)
                if tl is not None:
                    tl()

        BREG = {}

        def breg(e, val):
            if val not in BREG:
                BREG[val] = e.to_reg(val)
            return BREG[val]

        def moe_sparse():
            NTT = NSEQ * NT
            P.barrier()
            R_ = A_1[:, :]
            rk = "A1"
            o = [0]

            def T(n, m=NTT):
                v = R_[:, o[0]:o[0] + m * n].rearrange("p (t n) -> p t n", n=n)
                o[0] += m * n
                return v
            G = Lall[:, :, 0:4]
            E = Lall[:, :, 4:20].rearrange("p t (g e) -> p t g e", e=4)
            gmax, ohg, gex, gsum, gtop = T(1), T(4), T(4), T(1), T(1)
            prod, esel, m1, oh1, esel2, m2, oh2 = T(16), T(4), T(1), T(4), T(4), T(1), T(4)
            dd, ed, den = T(1), T(1), T(1)
            selA, selB, sel, cntS, offs, slot, tmp16 = T(16), T(16), T(16), T(16), T(16), T(16), T(16)
            ne, q_, kf, gt_, pc, base = T(1, 16), T(1, 16), T(1, 16), T(1, 16), T(1, 16), T(1, 16)
            ki = T(1, 16).bitcast(I32)
            cmpE = T(16, NTILE)
            Et, neq, idxf = T(1, NTILE), T(1, NTILE), T(1, NTILE)
            selbf = T(8).bitcast(BF16)
            w1 = rsm[:, 0:32].unsqueeze(2)
            w2 = rsm[:, 32:64].unsqueeze(2)
            slotA_f = rsm[:, 64:96]
            slotB_f = rsm[:, 96:128]
            slotA_i = sli[:, 0:32]
            slotB_i = sli[:, 32:64]
            idxW_i = idxw[:, :]

            def V(fn, reads=("Lall",), eng="dve"):
                P.op(eng, fn, reads=list(reads) + [rk, "rsm"], writes=[rk, "rsm"])

            def bc(v, n):
                return v.to_broadcast([128, NTT, n])
            V(lambda e: e.tensor_reduce(out=gmax[:, :, 0], in_=G, axis=AX.X, op=ALU.max))
            V(lambda e: e.tensor_tensor(out=ohg, in0=G, in1=bc(gmax, 4), op=ALU.is_equal))
            V(lambda e: e.tensor_tensor(out=gex, in0=G, in1=bc(gmax, 4), op=ALU.subtract))
            V(lambda e: e.activation(out=gex, in_=gex, func=AF.Exp), eng="act")
            V(lambda e: e.tensor_reduce(out=gsum[:, :, 0], in_=gex, axis=AX.X, op=ALU.add))
            V(lambda e: e.reciprocal(out=gtop, in_=gsum))
            prod4 = prod.rearrange("p t (g e) -> p t g e", e=4)
            V(lambda e: e.tensor_tensor(out=prod4, in0=E, in1=ohg.unsqueeze(3).to_broadcast([128, NTT, 4, 4]), op=ALU.mult))
            V(lambda e: e.tensor_reduce(out=esel, in_=prod.rearrange("p t (g e) -> p t e g", e=4), axis=AX.X, op=ALU.add))
            V(lambda e: e.tensor_reduce(out=m1[:, :, 0], in_=esel, axis=AX.X, op=ALU.max))
            V(lambda e: e.tensor_tensor(out=oh1, in0=esel, in1=bc(m1, 4), op=ALU.is_equal))
            V(lambda e: e.scalar_tensor_tensor(out=esel2, in0=oh1, scalar=NEG, in1=esel, op0=ALU.mult, op1=ALU.add))
            V(lambda e: e.tensor_reduce(out=m2[:, :, 0], in_=esel2, axis=AX.X, op=ALU.max))
            V(lambda e: e.tensor_tensor(out=oh2, in0=esel2, in1=bc(m2, 4), op=ALU.is_equal))
            V(lambda e: e.tensor_tensor(out=dd, in0=m2, in1=m1, op=ALU.subtract))
            V(lambda e: e.activation(out=ed, in_=dd, func=AF.Exp), eng="act")
            V(lambda e: e.tensor_scalar(out=den, in0=ed, scalar1=1.0, scalar2=None, op0=ALU.add))
            V(lambda e: e.reciprocal(out=den, in_=den))
            V(lambda e: e.tensor_tensor(out=w1, in0=gtop, in1=den, op=ALU.mult))
            V(lambda e: e.tensor_tensor(out=w2, in0=w1, in1=ed, op=ALU.mult))
            for sl, oh in ((selA, oh1), (selB, oh2)):
                V(lambda e, sl=sl, oh=oh: e.tensor_tensor(out=sl.rearrange("p t (g e) -> p t g e", e=4),
                                                         in0=ohg.unsqueeze(3).to_broadcast([128, NTT, 4, 4]),
                                                         in1=oh.unsqueeze(2).to_broadcast([128, NTT, 4, 4]), op=ALU.mult))
            V(lambda e: e.tensor_tensor(out=sel, in0=selA, in1=selB, op=ALU.add))
            V(lambda e: e.tensor_copy(out=selbf, in_=sel))
            selbf2 = selbf.rearrange("p t e -> p (t e)")
            P.op("pe", lambda e: e.matmul(pb[0][:, :], lhsT=ONESB, rhs=selbf2, start=True, stop=True), reads=[rk, "cstb"], writes=["pb0"])
            P.op("pe", lambda e: e.matmul(pb[1][:, :], lhsT=cstb[:, 5, :], rhs=selbf2, start=True, stop=True), reads=[rk, "cstb"], writes=["pb1"])
            V(lambda e: e.tensor_copy(out=cntS, in_=pb[0][:, :].rearrange("p (t e) -> p t e", e=16)), reads=("pb0",))
            V(lambda e: e.memset(offs[:, 0, :], 0.0))
            for j in range(1, NTT):
                V(lambda e, j=j: e.tensor_tensor(out=offs[:, j, :], in0=offs[:, j - 1, :], in1=cntS[:, j - 1, :], op=ALU.add))
            ne2, q2, kf2, gt2, pc2, base2 = [v[:, :, 0] for v in (ne, q_, kf, gt_, pc, base)]
            ki2 = ki[:, :, 0]
            V(lambda e: e.tensor_tensor(out=ne2, in0=offs[:, NTT - 1, :], in1=cntS[:, NTT - 1, :], op=ALU.add))
            V(lambda e: e.tensor_scalar(out=q2, in0=ne2, scalar1=127.0, scalar2=1.0 / 128, op0=ALU.add, op1=ALU.mult))
            V(lambda e: e.tensor_copy(out=ki2, in_=q2))
            V(lambda e: e.tensor_copy(out=kf2, in_=ki2))
            V(lambda e: e.tensor_tensor(out=gt2, in0=kf2, in1=q2, op=ALU.is_gt))
            V(lambda e: e.tensor_tensor(out=kf2, in0=kf2, in1=gt2, op=ALU.subtract))
            V(lambda e: e.tensor_scalar(out=pc2, in0=kf2, scalar1=128.0, scalar2=None, op0=ALU.mult))
            V(lambda e: e.memset(base2[:, 0:1], 0.0))
            for ex in range(1, 16):
                V(lambda e, ex=ex: e.tensor_tensor(out=base2[:, ex:ex + 1], in0=base2[:, ex - 1:ex], in1=pc2[:, ex - 1:ex], op=ALU.add))
            V(lambda e: e.tensor_tensor(out=slot, in0=pb[1][:, :].rearrange("p (t e) -> p t e", e=16), in1=offs, op=ALU.add), reads=("pb1",))
            V(lambda e: e.tensor_tensor(out=slot, in0=slot, in1=base2.unsqueeze(1).to_broadcast([128, NTT, 16]), op=ALU.add))
            for sl, dstf, dsti in ((selA, slotA_f, slotA_i), (selB, slotB_f, slotB_i)):
                V(lambda e, sl=sl: e.tensor_tensor(out=tmp16, in0=sl, in1=slot, op=ALU.mult))
                V(lambda e, dstf=dstf: e.tensor_reduce(out=dstf, in_=tmp16, axis=AX.X, op=ALU.add))
                V(lambda e, dstf=dstf, dsti=dsti: e.tensor_copy(out=dsti, in_=dstf))
            V(lambda e: e.tensor_tensor(out=cmpE, in0=base2.unsqueeze(1).to_broadcast([128, NTILE, 16]),
                                        in1=t128[:, :].unsqueeze(2).to_broadcast([128, NTILE, 16]), op=ALU.is_le), reads=("t128",))
            Et2, neq2, idxf2 = Et[:, :, 0], neq[:, :, 0], idxf[:, :, 0]
            V(lambda e: e.tensor_reduce(out=Et2, in_=cmpE, axis=AX.X, op=ALU.add))
            V(lambda e: e.memset(neq2[:, 0:2], 1.0))
            V(lambda e: e.tensor_tensor(out=neq2[:, 2:NTILE], in0=Et2[:, 2:NTILE], in1=Et2[:, 0:NTILE - 2], op=ALU.not_equal))
            V(lambda e: e.tensor_scalar(out=idxf2, in0=Et2, scalar1=128.0, scalar2=-(128.0 + BIGIDX), op0=ALU.mult, op1=ALU.add))
            V(lambda e: e.tensor_scalar(out=idxf2, in0=idxf2, scalar1=piota[:, 0:1], scalar2=None, op0=ALU.add), reads=("piota",))
            V(lambda e: e.tensor_tensor(out=idxf2, in0=idxf2, in1=neq2, op=ALU.mult))
            V(lambda e: e.tensor_scalar(out=idxf2, in0=idxf2, scalar1=BIGIDX, scalar2=None, op0=ALU.add))
            P.op("dve", lambda e: e.tensor_copy(out=idxW_i, in_=idxf2), reads=[rk], writes=["st2i"])
            P.barrier()
            WB = [A_1b[:, i * 12288:(i + 1) * 12288] for i in range(2)]
            for t in range(2):
                for m in range(3):
                    P.op("pool", lambda e, m=m, t=t: e.indirect_dma_start(
                        out=WB[t][:, m * 4096:(m + 1) * 4096], out_offset=None, in_=WBF_d[m][:, :], in_offset=bass.IndirectOffsetOnAxis(ap=idxW_i[:, t:t + 1], axis=0),
                        bounds_check=breg(e, 2047), oob_is_err=False), reads=["st2i"], writes=[f"wb{t}{m}"], dma_key=f"wg{t}{m}")
            xsb = [xn[0][:, :], xn[1][:, :], xt[0][:, :].bitcast(BF16)[:, 0:1024], xt[1][:, :].bitcast(BF16)[:, 0:1024]]
            xsk = ["xn0", "xn1", "xt0", "xt1"]
            for jt in range(NTT):
                b = jt % 4
                P.op("sp", lambda e, jt=jt, b=b: e.dma_start(out=xsb[b], in_=XN_d[jt * 128:(jt + 1) * 128, :]), reads=["XNd"], writes=[xsk[b]], dma_key=f"xnl{b}")
                for si, sl_i in enumerate((slotA_i, slotB_i)):
                    P.op("pool", lambda e, jt=jt, b=b, sl_i=sl_i: e.indirect_dma_start(
                        out=XS_d[:, :], out_offset=bass.IndirectOffsetOnAxis(ap=sl_i[:, jt:jt + 1], axis=0), in_=xsb[b], in_offset=None,
                        bounds_check=breg(e, NSLOT - 1), oob_is_err=False), reads=[xsk[b], "rsm"], writes=[f"XSd_{jt}_{si}"], dma_key=f"xsc{b}{si}")
            P.barrier()
            WB = [A_1b[:, i * 12288:(i + 1) * 12288] for i in range(2)]
            xsT = [hm[i][:, 0:2, :].rearrange("p a (b c) -> p (a b) c", c=128) for i in range(2)]
            hTb = [aT[i][:, :].rearrange("p (a c) -> p a c", c=128) for i in range(2)]

            def et_wload(t, ms):
                b = t % 2
                for m in ms:
                    P.op("pool", lambda e, m=m: e.indirect_dma_start(
                        out=WB[b][:, m * 4096:(m + 1) * 4096], out_offset=None, in_=WBF_d[m][:, :], in_offset=bass.IndirectOffsetOnAxis(ap=idxW_i[:, t:t + 1], axis=0),
                        bounds_check=breg(e, 2047), oob_is_err=False), reads=["st2i"], writes=[f"wb{b}{m}"], dma_key=f"wg{b}{m}")

            def et_xload(t):
                b = t % 2
                P.op("sp", lambda e: e.dma_start(out=xn[b][:], in_=XS_d[t * 128:(t + 1) * 128, :]), writes=[f"xn{b}"], dma_key=f"xsl{b}")

            SG = [spT[0], spT[1]]
            HB = [spx, aTx]

            def et_T(t):
                b = t % 2
                pT = pb[6 + b][:, 0:512].bitcast(BF16).rearrange("p (c t) -> p c t", t=128)
                for c in range(8):
                    P.op("pe", lambda e, c=c: e.transpose(out=pT[:, c, :], in_=xn[b][:, c * 128:(c + 1) * 128], identity=identb[:]),
                         reads=[f"xn{b}", "identb"], writes=[f"pb{6 + b}"])
                P.op("dve", lambda e: e.tensor_tensor(out=xsT[b], in0=pT, in1=nffn[:].unsqueeze(2).to_broadcast([128, 8, 128]), op=ALU.mult),
                     reads=[f"pb{6 + b}", "nffn"], writes=[f"xsT{b}"])

            def et_GU(t):
                b = t % 2
                wgb = WB[b][:, 0:4096].rearrange("p (k n) -> p k n", n=512)
                wub = WB[b][:, 4096:8192].rearrange("p (k n) -> p k n", n=512)
                for k in range(8):
                    P.op("pe", lambda e, k=k: e.matmul(pb[0][:, :], lhsT=xsT[b][:, k, :], rhs=wgb[:, k, :], start=(k == 0), stop=(k == 7)),
                         reads=[f"xsT{b}", f"wb{b}0"], writes=["pb0"])
                for k in range(8):
                    P.op("pe", lambda e, k=k: e.matmul(pb[1][:, :], lhsT=xsT[b][:, k, :], rhs=wub[:, k, :], start=(k == 0), stop=(k == 7)),
                         reads=[f"xsT{b}", f"wb{b}1"], writes=["pb1"])
                P.op("act", lambda e: e.activation(out=SG[b][:, :], in_=pb[0][:, :], func=AF.Silu), reads=["pb0"], writes=[f"etsg{b}"])
                P.op("dve", lambda e: e.tensor_tensor(out=HB[b][:, :], in0=pb[1][:, :], in1=SG[b][:, :], op=ALU.mult), reads=["pb1", f"etsg{b}"], writes=[f"ethb{b}"])

            def et_s2(t):
                b = t % 2
                wdb = WB[b][:, 8192:12288].rearrange("p (k n) -> p k n", n=D)
                pH = pb[2][:, 0:256].bitcast(BF16).rearrange("p (c t) -> p c t", t=128)
                for c in range(4):
                    P.op("pe", lambda e, c=c: e.transpose(out=pH[:, c, :], in_=HB[b][:, c * 128:(c + 1) * 128], identity=identb[:]),
                         reads=[f"ethb{b}", "identb"], writes=["pb2"])
                P.op("act", lambda e: e.activation(out=hTb[b], in_=pH, func=AF.Copy), reads=["pb2"], writes=[f"hTb{b}"])
                for hf in range(2):
                    yb = 4 + hf
                    for hc in range(4):
                        P.op("pe", lambda e, hc=hc, hf=hf, yb=yb: e.matmul(pb[yb][:, :], lhsT=hTb[b][:, hc, :], rhs=wdb[:, hc, hf * 512:(hf + 1) * 512],
                                                                           start=(hc == 0), stop=(hc == 3)), reads=[f"hTb{b}", f"wb{b}2"], writes=[f"pb{yb}"])
                    if hf == 0:
                        P.op("act", lambda e: e.activation(out=xt[b][:, 0:512], in_=pb[4][:, :], func=AF.Copy), reads=["pb4"], writes=[f"xt{b}"])
                    else:
                        P.op("dve", lambda e: e.tensor_copy(out=xt[b][:, 512:1024], in_=pb[5][:, :]), reads=["pb5"], writes=[f"xt{b}"])
                P.op("sp", lambda e: e.dma_start(out=YS_d[t * 128:(t + 1) * 128, :], in_=xt[b][:]), reads=[f"xt{b}"], writes=[f"YSd_{t}"], dma_key=f"yst{b}")

            et_xload(0)
            et_xload(1)
            for n in range(NTILE + 3):
                if n < NTILE:
                    et_T(n)
                if 0 <= n - 3 < NTILE:
                    et_s2(n - 3)
                if 2 <= n - 1 < NTILE:
                    et_wload(n - 1, (2,))
                if 0 <= n - 1 < NTILE:
                    et_GU(n - 1)
                if 2 <= n + 1 < NTILE:
                    et_wload(n + 1, (0, 1))
                if 2 <= n + 2 < NTILE:
                    et_xload(n + 2)
            P.barrier()
            yAB = [[A_O[:, (2 * i + k) * 1024:(2 * i + k + 1) * 1024] for k in range(2)] for i in range(2)]
            x2b = [A_O[:, (4 + i) * 1024:(5 + i) * 1024] for i in range(2)]
            for jt in range(NTT):
                b = jt % 2
                s_, j_ = jt // NT, jt % NT
                rows = out_d[s_, j_ * 128:(j_ + 1) * 128, :]
                P.op("sp", lambda e, rows=rows, b=b: e.dma_start(out=x2b[b], in_=rows), reads=["outd"], writes=[f"x2b{b}"], dma_key=f"x2l{b}")
                for k, sl_i in enumerate((slotA_i, slotB_i)):
                    P.op("pool", lambda e, jt=jt, b=b, k=k, sl_i=sl_i: e.indirect_dma_start(
                        out=yAB[b][k], out_offset=None, in_=YS_d[:, :], in_offset=bass.IndirectOffsetOnAxis(ap=sl_i[:, jt:jt + 1], axis=0),
                        bounds_check=breg(e, NSLOT - 1), oob_is_err=False), reads=["rsm", "YSd"], writes=[f"yab{b}{k}"], dma_key=f"yg{b}{k}")
                for k, wv_ in enumerate((rsm[:, 0:32], rsm[:, 32:64])):
                    P.op("dve", lambda e, jt=jt, b=b, k=k, wv_=wv_: e.scalar_tensor_tensor(out=x2b[b], in0=yAB[b][k], scalar=wv_[:, jt:jt + 1], in1=x2b[b],
                                                                                       op0=ALU.mult, op1=ALU.add),
                         reads=[f"yab{b}{k}", f"x2b{b}", "rsm"], writes=[f"x2b{b}"])
                P.op("act", lambda e, rows=rows, b=b: e.dma_start(out=rows, in_=x2b[b]), reads=[f"x2b{b}"], writes=[f"outd2_{jt}"], dma_key=f"ost{b}", is_out=True)

        for s in range(nseq):
            for j in range(NT):
                rmsnorm_T(x_d[s, j * 128:(j + 1) * 128, :], nmix, "nmix", hT, "hT", j, j,
                          junk=(hm[0][:, 2 * (j % 2):2 * (j % 2) + 2, :].rearrange("p a b -> p (a b)"), f"hmj{j % 2}"))
            if stage in ("full", "sb", "sbproj", "merge"):
                sb_proj()
                if stage == "sbproj":
                    break
                if stage in ("full", "merge"):
                    MEMQ.extend(mem_items(s))
                sb_attention()
                mem_drain()
                if stage == "sb":
                    break
            if stage == "mem":
                mem_mixer(s)
                break
            if stage in ("full", "dil", "merge"):
                dil_mixer(s)
                if stage == "dil":
                    break
            merge_phase(s)
            if stage == "merge":
                break

        if stage == "full":
            moe_sparse()

        dsrc = {"sbproj": (QT, ["QT"]), "sb": (oT_sb, ["oT_sb"]), "mem": (oT_mem, ["oT_mem"]), "dil": (oT_dil, ["oT_dil"]),
                "merge": (mergedT, ["QT", "KT"])}.get(stage)
        if dsrc is not None:
            n = 0
            for a in range(4):
                for hf in range(2):
                    b = n % 2
                    n += 1
                    P.op("dve", lambda e, a=a, hf=hf, b=b: e.tensor_copy(out=xt[b][:], in_=dsrc[0][:, a, hf * 1024:(hf + 1) * 1024]),
                         reads=dsrc[1], writes=[f"xt{b}"])
                    P.op("sp", lambda e, a=a, hf=hf, b=b: e.dma_start(out=dbg_d[:, a, hf * 1024:(hf + 1) * 1024], in_=xt[b][:]),
                         reads=[f"xt{b}"], dma_key=f"dbg{b}", is_out=True)
        P.emit()
    return nc


def _consts():
    j = np.arange(128)[:, None]
    s_ = np.arange(128)[None, :]
    cst = np.zeros((128, 6, 128), np.float32)
    cst[:, 0, :] = -(j >= s_).astype(np.float32)
    cst[:, 1, :] = -1.0
    cst[:, 2, :] = np.where(j < s_, 0.0, NEG)
    cst[:, 3, :] = 0.0
    cst[:, 4, :] = 1.0
    cst[:, 5, :] = (j < s_).astype(np.float32)
    k = np.arange(128)[:, None]
    q = np.arange(128)[None, :]
    masks = np.zeros((128, 23, 128), np.float32)
    masks[:, 0, :] = (k <= q)
    masks[:, 1, :] = (q <= k)
    same4 = (k % 4) == (q % 4)
    for off in range(5):
        ok = same4.copy()
        if off == 0:
            ok &= (k <= q)
        if off == 4:
            ok &= (q <= k)
        masks[:, 2 + off, :] = ok
    same16 = (k % 16) == (q % 16)
    masks[:, 7, :] = same16 & (k <= q)
    for r in range(8, 12):
        masks[:, r, :] = same16
    invf = (10000.0 ** (-np.arange(0, 64, 2, dtype=np.float32) / 64)).astype(np.float32)
    invf = np.ascontiguousarray(np.broadcast_to(invf[None, :], (128, 32))).astype(np.float32)
    return cst, masks, invf


def _pc(v):
    return np.ascontiguousarray(np.asarray(v, np.float32).reshape(-1, 128).T)


def _rows(w, k):
    w = np.asarray(w, np.float32)
    e, kp, n = w.shape
    return np.ascontiguousarray(w.reshape(e, k, 128, n).transpose(0, 2, 1, 3).reshape(e * 128, k * n))


def make_in_maps(inputs):
    f = lambda a: np.ascontiguousarray(np.asarray(a))
    x = f(inputs["x"]); mem = f(inputs["mem"]); pos = f(inputs["positions"])
    cst, masks, invf = _consts()
    qn = np.asarray(inputs["qn_dil"], np.float32)[0]
    kn = np.asarray(inputs["kn_dil"], np.float32)[0]
    qk4 = np.zeros((128, 4, 64), np.float32)
    qk4[:, 0, :] = qn[None, :]
    qk4[:, 1, :] = np.concatenate([qn[32:], qn[:32]])[None, :]
    qk4[:, 2, :] = kn[None, :]
    qk4[:, 3, :] = np.concatenate([kn[32:], kn[:32]])[None, :]
    shared = dict(
        w_in=f(inputs["w_in"][0]),
        nmix=_pc(inputs["norm_mix"][0]), nmem=_pc(inputs["norm_mem"][0]), nffn=_pc(inputs["norm_ffn"][0]),
        bgate=_pc(inputs["b_gate"][0]),
        qk4=qk4,
        qnm=np.ascontiguousarray(np.broadcast_to(np.asarray(inputs["qn_mem"], np.float32)[0][None, :], (128, 128))),
        knm=np.ascontiguousarray(np.broadcast_to(np.asarray(inputs["kn_mem"], np.float32)[0][None, :], (128, 128))),
        wkv=f(inputs["w_mem_kv"][0]),
        wo=np.ascontiguousarray(np.stack([inputs["w_o_sb"][0], inputs["w_o_mem"][0], inputs["w_o_dil"][0]])),
        wout=f(inputs["w_out"][0]),
        wr=np.ascontiguousarray(np.concatenate([inputs["w_router_group"][0], inputs["w_router_expert"][0]], axis=1)),
        br=np.ascontiguousarray(np.broadcast_to(np.concatenate([inputs["b_router_group"][0], inputs["b_router_expert"][0]])[None, :], (128, 20))).astype(np.float32),
        wg=_rows(inputs["w_exp_gate"][0], 8), wu=_rows(inputs["w_exp_up"][0], 8), wd=_rows(inputs["w_exp_down"][0], 4),
        t128=np.ascontiguousarray(np.broadcast_to((128.0 * np.arange(80, dtype=np.float32))[None, :], (128, 80))),
        piota=np.arange(128, dtype=np.float32).reshape(128, 1),
        ident=np.eye(128, dtype=np.float32), cst=cst, masks=masks, invf=invf,
    )
    maps = []
    for c in range(8):
        d = dict(shared)
        d["x"] = np.ascontiguousarray(x[2 * c:2 * c + 2])
        d["mem"] = np.ascontiguousarray(mem[2 * c:2 * c + 2])
        p = pos[2 * c:2 * c + 2].astype(np.int32).reshape(2, NT, 128).transpose(0, 2, 1)
        d["pos"] = np.ascontiguousarray(p)
        maps.append(d)
    return maps


def kernel(**inputs):
    nc = build("full")
    maps = make_in_maps(inputs)
    res = run_bass_kernel_spmd(nc, maps, core_ids=list(range(8)))
    out = np.concatenate([np.asarray(r["out"]) for r in res.results], axis=0)
    return out.astype(np.float32)
```
